# Optimizing a Trainium2 kernel written in Bass

```python
import math
import jax, jax.numpy as jnp
from jax import lax
import numpy as np

D_MODEL = 1024
BATCH = 32
SEQ = 256
DEPTH = 4
DEC_BATCH = 8
DEC_SEQ = 2048
PAST_LEN = 512

GRID_W = 64
N_BRANCH = 4
BRANCH_W = D_MODEL // N_BRANCH
HEAD_DIM = 64
N_HEADS = BRANCH_W // HEAD_DIM
RW_DECAY_RANK = 32
RW_A_RANK = 32
RW_GATE_RANK = 64
RW_COLS = 3 * BRANCH_W + RW_DECAY_RANK + RW_A_RANK + RW_GATE_RANK
HY_ORDER = 2
HY_COLS = (HY_ORDER + 1) * BRANCH_W
HY_BANDS = 16
HY_EMB = 2 * HY_BANDS + 1
HY_HID = 64
SHORT_W = 3
RET_COLS = 4 * BRANCH_W
RET_CHUNK = 128
HG_EXPAND = HEAD_DIM
HG_COLS = 5 * BRANCH_W
HG_CHUNK = 32
MG_COLS = N_BRANCH * D_MODEL
N_IN = RW_COLS + HY_COLS + RET_COLS + HG_COLS + MG_COLS
N_EXPERTS = 16
EC_FACTOR = 2
EXPERT_FF = 2 * D_MODEL
ROPE_BASE = 10000.0
NORM_EPS = 1e-6
RW_GN_EPS = 64e-5
GN_EPS = 1e-5
LB_FLOOR = 1e-30

kernel_name = "hybrid_flow_prefix_trunk_step"


def rms_norm(x, g):
    xf = x.astype(jnp.float32)
    y = xf * lax.rsqrt(jnp.mean(xf * xf, axis=-1, keepdims=True) + NORM_EPS)
    return (y * g.astype(jnp.float32)).astype(x.dtype)


def split_heads(t):
    return t.reshape(t.shape[:-1] + (N_HEADS, HEAD_DIM))


def merge_heads(t):
    return t.reshape(t.shape[:-2] + (BRANCH_W,))


def head_layer_norm(y, gain, bias, eps):
    mu = jnp.mean(y, axis=-1, keepdims=True)
    var = jnp.mean(jnp.square(y - mu), axis=-1, keepdims=True)
    return merge_heads((y - mu) * lax.rsqrt(var + eps)) * gain + bias


def centred_taps(x, w_prev, w_mid, w_next):
    xp = jnp.pad(x, ((0, 0), (1, 1), (0, 0)))
    return w_prev * xp[:, :-2] + w_mid * xp[:, 1:-1] + w_next * xp[:, 2:]


def to_dir_heads(fw, bw):
    B, T, _ = fw.shape
    st = jnp.stack([fw, bw[:, ::-1]])
    return st.reshape(2, B, T, N_HEADS, HEAD_DIM).transpose(0, 1, 3, 2, 4)


def bidir(t):
    return to_dir_heads(t, t)


def from_dir_heads(y):
    return jnp.transpose(y[0] + y[1][:, :, ::-1], (0, 2, 1, 3))


def rope_tables(row, col):
    half = HEAD_DIM // 2
    nf = half // 2
    inv = ROPE_BASE ** (-jnp.arange(nf, dtype=jnp.float32) / nf)
    ang = jnp.concatenate([row[:, None] * inv, col[:, None] * inv], axis=-1)
    return jnp.cos(ang)[:, None, :], jnp.sin(ang)[:, None, :]


def apply_rope(x, rope):
    cos, sin = rope
    half = HEAD_DIM // 2
    x1, x2 = x[..., :half], x[..., half:]
    return jnp.concatenate([x1 * cos - x2 * sin, x1 * sin + x2 * cos], axis=-1)


def rwkv7_scan(r, w, kk, kka, v, k, s0):
    def step(S, inp):
        r_t, w_t, kk_t, kka_t, v_t, k_t = inp
        S = (S * w_t[..., None, :]
             - jnp.einsum('dbhvk,dbhk->dbhv', S, kk_t)[..., None] * kka_t[..., None, :]
             + v_t[..., None] * k_t[..., None, :])
        return S, jnp.einsum('dbhvk,dbhk->dbhv', S, r_t)
    xs = tuple(jnp.moveaxis(a, 3, 0) for a in (r, w, kk, kka, v, k))
    S, ys = lax.scan(step, s0, xs)
    return jnp.moveaxis(ys, 0, 3), S


def retention_chunks(q, k, v, log_gamma, s0):
    d, B, H, T, N = q.shape
    C = RET_CHUNK
    nc = T // C

    def chunked(a):
        return jnp.moveaxis(a.reshape(d, B, H, nc, C, a.shape[-1]), 3, 0)

    n = jnp.arange(C, dtype=jnp.float32)
    lg = log_gamma[:, None, :, None, None]
    rel = n[:, None] - n[None, :]
    dmask = jnp.exp(jnp.where(rel >= 0, rel * lg, -jnp.inf))
    q_dec = jnp.exp((n[:, None] + 1.0) * lg)
    k_dec = jnp.exp((C - 1.0 - n[:, None]) * lg)
    c_dec = jnp.exp(C * lg)

    def step(S, inp):
        qc, kc, vc = inp
        att = jnp.einsum('dbhnk,dbhmk->dbhnm', qc, kc) * dmask
        y = (jnp.einsum('dbhnm,dbhmv->dbhnv', att, vc)
             + jnp.einsum('dbhnk,dbhkv->dbhnv', qc * q_dec, S))
        S = S * c_dec + jnp.einsum('dbhmk,dbhmv->dbhkv', kc * k_dec, vc)
        return S, y
    S, ys = lax.scan(step, s0, (chunked(q), chunked(k), chunked(v)))
    return jnp.moveaxis(ys, 0, 3).reshape(d, B, H, T, N), S


def hgrn2_chunks(q, k, log_f, v, s0):
    d, B, H, T, K = q.shape
    V = v.shape[-1]
    C = HG_CHUNK
    nc = T // C

    def chunked(a):
        return jnp.moveaxis(a.reshape(d, B, H, nc, C, a.shape[-1]), 3, 0)

    causal = jnp.tril(jnp.ones((C, C), dtype=bool))[..., None]

    def step(S, inp):
        qc, kc, gc, vc = inp
        b = jnp.cumsum(gc, axis=-2)
        diff = jnp.where(causal, b[..., :, None, :] - b[..., None, :, :], -jnp.inf)
        att = jnp.einsum('dbhtk,dbhsk,dbhtsk->dbhts', qc, kc, jnp.exp(diff))
        y = (jnp.einsum('dbhts,dbhsv->dbhtv', att, vc)
             + jnp.einsum('dbhtk,dbhkv->dbhtv', qc * jnp.exp(b), S))
        b_end = b[..., -1:, :]
        S = (jnp.exp(b_end)[..., 0, :, None] * S
             + jnp.einsum('dbhsk,dbhsv->dbhkv', kc * jnp.exp(b_end - b), vc))
        return S, y
    S, ys = lax.scan(step, s0, (chunked(q), chunked(k), chunked(log_f), chunked(v)))
    return jnp.moveaxis(ys, 0, 3).reshape(d, B, H, T, V), S


def hyena_filter_spectrum(T, lp):
    f32 = jnp.float32
    t = jnp.linspace(0.0, 1.0, T, dtype=f32)[:, None]
    w = (2.0 * math.pi / T) * jnp.arange(T, dtype=f32)[:, None]
    bands = jnp.linspace(1e-4, HY_BANDS - 1.0, HY_BANDS, dtype=f32)[None, :]
    feats = jnp.concatenate([t, jnp.cos(bands * w), -jnp.sin(bands * w)], axis=-1)
    freq = lp['hy_freq'].astype(f32)
    hid = jnp.sin(freq[0] * (feats @ lp['hy_ffn1'].astype(f32) + lp['hy_ffn1_b'].astype(f32)))
    hid = jnp.sin(freq[1] * (hid @ lp['hy_ffn2'].astype(f32) + lp['hy_ffn2_b'].astype(f32)))
    filt = (hid @ lp['hy_ffn3'].astype(f32)) * jnp.exp(-t * jnp.abs(lp['hy_decay'].astype(f32)))
    filt = filt.reshape(T, HY_ORDER, 2, BRANCH_W)
    filt = filt / jnp.sum(jnp.abs(filt), axis=(0, 2), keepdims=True)
    circ = jnp.concatenate([filt[:, :, 0], jnp.zeros((1, HY_ORDER, BRANCH_W), f32),
                            filt[:0:-1, :, 1]], axis=0)
    return jnp.fft.rfft(circ, axis=0)


def fft_long_conv(u, spec):
    T = u.shape[1]
    return jnp.fft.irfft(jnp.fft.rfft(u, n=2 * T, axis=1) * spec, n=2 * T, axis=1)[:, :T]


def token_mix(h, lp, s_rw, s_ret, s_hg, rope):
    B, T, _ = h.shape
    f32 = jnp.float32
    BW = BRANCH_W
    z = jnp.einsum('btd,dn->btn', h, lp['w_in'])
    o1 = RW_COLS
    o2 = o1 + HY_COLS
    o3 = o2 + RET_COLS
    o4 = o3 + HG_COLS
    z_rw, z_hy, z_ret, z_hg, z_mg = jnp.split(z, [o1, o2, o3, o4], axis=-1)

    mu = lp['rw_mu']
    xr = centred_taps(z_rw, mu[0], 1.0 - mu[0] - mu[1], mu[1]).astype(f32)
    r, k, v, xw, xa, xg = jnp.split(
        xr, [BW, 2 * BW, 3 * BW, 3 * BW + RW_DECAY_RANK, 3 * BW + RW_DECAY_RANK + RW_A_RANK], axis=-1)
    wlog = -jax.nn.softplus(-(lp['rw_w0'][:, None, None, :]
                              + jnp.einsum('btr,drc->dbtc', jnp.tanh(xw), lp['rw_w_up']))) - 0.5
    decay = jnp.exp(-jnp.exp(wlog))
    a = jax.nn.sigmoid(lp['rw_a0'] + xa @ lp['rw_a_up'])
    g_rw = jax.nn.sigmoid(xg) @ lp['rw_g_up']
    k_k, k_a, r_k = lp['rw_kvec'][0], lp['rw_kvec'][1], lp['rw_kvec'][2]
    kk = split_heads(k * k_k)
    kk = merge_heads(kk / jnp.maximum(jnp.linalg.norm(kk, axis=-1, keepdims=True), 1e-12))
    k = k * (1.0 + (a - 1.0) * k_a)
    y_dir, s_rw_new = rwkv7_scan(bidir(r), to_dir_heads(decay[0], decay[1]), bidir(kk),
                                 bidir(kk * a), bidir(v), bidir(k), s_rw.astype(f32))
    bonus = jnp.sum(split_heads(r * k * r_k), axis=-1, keepdims=True) * split_heads(v)
    y_rw = (head_layer_norm(from_dir_heads(y_dir), lp['rw_ln'][0], lp['rw_ln'][1], RW_GN_EPS)
            + merge_heads(bonus)) * g_rw

    hc = lp['hy_conv']
    hx = centred_taps(z_hy, hc[0], hc[1], hc[2]).astype(f32)
    u, x1, x2 = jnp.split(hx, 3, axis=-1)
    spec = hyena_filter_spectrum(T, lp)
    skip = lp['hy_skip'].astype(f32)
    u = x1 * (fft_long_conv(u, spec[:, 0]) + skip[0] * u)
    y_hy = x2 * (fft_long_conv(u, spec[:, 1]) + skip[1] * u)

    q, kr, vr, gr = jnp.split(z_ret.astype(f32), 4, axis=-1)
    if rope is not None:
        q = merge_heads(apply_rope(split_heads(q), rope))
        kr = merge_heads(apply_rope(split_heads(kr), rope))
    log_gamma = -jnp.exp(lp['ret_rate'].astype(f32))
    y_dir, s_ret_new = retention_chunks(bidir(q * HEAD_DIM ** -0.5), bidir(kr), bidir(vr),
                                        log_gamma, s_ret.astype(f32))
    y_ret = head_layer_norm(from_dir_heads(y_dir), lp['ret_gn'][0], lp['ret_gn'][1], GN_EPS) * jax.nn.silu(gr)

    qh, zf_fw, zf_bw, ih, gh = jnp.split(z_hg.astype(f32), 5, axis=-1)
    lb = lp['hg_lb'][:, None, None, :]
    zf = jnp.stack([zf_fw, zf_bw])
    log_f = jnp.logaddexp(jnp.log(jnp.maximum(lb, LB_FLOOR)), jnp.log1p(-lb) + jax.nn.log_sigmoid(zf))
    k_in = (1.0 - lb) * jax.nn.sigmoid(-zf)
    y_dir, s_hg_new = hgrn2_chunks(bidir(jax.nn.silu(qh)), to_dir_heads(k_in[0], k_in[1]),
                                   to_dir_heads(log_f[0], log_f[1]), bidir(ih), s_hg.astype(f32))
    yh = from_dir_heads(y_dir)
    yh = yh * lax.rsqrt(jnp.mean(yh * yh, axis=-1, keepdims=True) + NORM_EPS)
    y_hg = merge_heads(yh) * lp['hg_norm'] * jax.nn.silu(gh)

    br = jnp.stack([y_rw, y_hy, y_ret, y_hg], axis=2).astype(h.dtype)
    proj = jnp.einsum('btnc,ncd->btnd', br, lp['br_w'])
    gates = jax.nn.sigmoid(z_mg.reshape(B, T, N_BRANCH, D_MODEL))
    out = jnp.einsum('btd,de->bte', jnp.sum(gates * proj, axis=2), lp['w_out'])
    return out, (s_rw_new, s_ret_new, s_hg_new)


def ec_moe(h, router, w1, w3, w2):
    B, T, D = h.shape
    cap = EC_FACTOR * T // N_EXPERTS
    aff = jax.nn.softmax(jnp.einsum('btd,de->bte', h, router).astype(jnp.float32), axis=-1)
    gate, idx = lax.top_k(jnp.swapaxes(aff, 1, 2), cap)
    xe = jax.vmap(lambda hb, ib: hb[ib])(h, idx)
    hid = jax.nn.silu(jnp.einsum('becd,edf->becf', xe, w1)) * jnp.einsum('becd,edf->becf', xe, w3)
    ye = jnp.einsum('becf,efd->becd', hid, w2) * gate[..., None].astype(h.dtype)
    return jax.vmap(lambda ib, yb: jnp.zeros((T, D), yb.dtype).at[ib.reshape(-1)].add(yb.reshape(-1, D)))(idx, ye)


def trunk_layer(x, cvec, lp, s_rw, s_ret, s_hg, rope):
    mod = jnp.einsum('bd,dm->bm', jax.nn.silu(cvec), lp['ada_w']) + lp['ada_b']
    sh1, sc1, g1, sh2, sc2, g2 = jnp.split(mod[:, None, :], 6, axis=-1)
    h = rms_norm(x, lp['norm1_g']) * (1.0 + sc1) + sh1
    mix, states = token_mix(h, lp, s_rw, s_ret, s_hg, rope)
    x = x + g1 * mix
    h = rms_norm(x, lp['norm2_g']) * (1.0 + sc2) + sh2
    x = x + g2 * ec_moe(h, lp['router'], lp['ex_w1'], lp['ex_w3'], lp['ex_w2'])
    return x, states


def pack_states(states):
    return jnp.stack(states, axis=0).transpose(2, 0, 1, 3, 4, 5)


def setup_inputs(seed: int = 0) -> dict:
    key = jax.random.key(seed)
    keys = jax.random.split(key, 48)
    counter = [0]
    f32 = jnp.float32
    L, D, BW, H, N = DEPTH, D_MODEL, BRANCH_W, N_HEADS, HEAD_DIM

    def nk():
        kk = keys[counter[0]]
        counter[0] += 1
        return kk

    def nrm(shape, scale=1.0):
        return scale * jax.random.normal(nk(), shape, f32)

    inp = {}
    inp['x_prompt'] = nrm((BATCH, SEQ, D))
    inp['x_sample'] = nrm((DEC_BATCH, DEC_SEQ, D))
    inp['state_rwkv'] = nrm((DEC_BATCH, L, 2, H, N, N), 0.5)
    inp['state_ret'] = nrm((DEC_BATCH, L, 2, H, N, N), 0.5)
    inp['state_hgrn'] = nrm((DEC_BATCH, L, 2, H, HG_EXPAND, N), 0.5)
    inp['c'] = nrm((DEC_BATCH, D))
    inp['c_ctx'] = nrm((D,))
    inp['ada_w'] = nrm((L, D, 6 * D), 0.5 * D ** -0.5)
    inp['ada_b'] = nrm((L, 6 * D), 0.02)
    inp['norm1_g'] = 1.0 + nrm((L, D), 0.02)
    inp['norm2_g'] = 1.0 + nrm((L, D), 0.02)
    inp['final_g'] = 1.0 + nrm((D,), 0.02)
    inp['w_in'] = nrm((L, D, N_IN), D ** -0.5)
    inp['rw_mu'] = jax.random.uniform(nk(), (L, 2, RW_COLS), f32, 0.0, 0.4)
    inp['rw_w0'] = jnp.linspace(-6.5, -1.5, BW, dtype=f32)[None, None, :] + nrm((L, 2, BW), 0.1)
    inp['rw_w_up'] = nrm((L, 2, RW_DECAY_RANK, BW), 0.1)
    inp['rw_a0'] = nrm((L, BW), 0.1)
    inp['rw_a_up'] = nrm((L, RW_A_RANK, BW), 0.1)
    inp['rw_g_up'] = nrm((L, RW_GATE_RANK, BW), RW_GATE_RANK ** -0.5)
    inp['rw_kvec'] = jnp.array([0.85, 1.0, -0.04], f32)[None, :, None] + nrm((L, 3, BW), 0.02)
    inp['rw_ln'] = jnp.array([1.0, 0.0], f32)[None, :, None] + nrm((L, 2, BW), 0.02)
    inp['hy_conv'] = nrm((L, SHORT_W, HY_COLS), SHORT_W ** -0.5)
    inp['hy_ffn1'] = nrm((L, HY_EMB, HY_HID), HY_EMB ** -0.5)
    inp['hy_ffn1_b'] = nrm((L, HY_HID), 0.1)
    inp['hy_ffn2'] = nrm((L, HY_HID, HY_HID), HY_HID ** -0.5)
    inp['hy_ffn2_b'] = nrm((L, HY_HID), 0.1)
    inp['hy_ffn3'] = nrm((L, HY_HID, HY_ORDER * 2 * BW), HY_HID ** -0.5)
    inp['hy_freq'] = 1.0 + nrm((L, 2, HY_HID), 0.02)
    inp['hy_decay'] = jnp.tile(jnp.linspace(3.07, 15.35, BW, dtype=f32), HY_ORDER * 2)[None, :] + nrm((L, HY_ORDER * 2 * BW), 0.1)
    inp['hy_skip'] = nrm((L, HY_ORDER, BW))
    ret_base = jnp.log(-jnp.log1p(-(2.0 ** (-5.0 - jnp.arange(H, dtype=f32)))))
    inp['ret_rate'] = ret_base[None, None, :] + nrm((L, 2, H), 0.05)
    inp['ret_gn'] = jnp.array([1.0, 0.0], f32)[None, :, None] + nrm((L, 2, BW), 0.02)
    inp['hg_lb'] = nrm((L, 2, BW), 0.5)
    inp['hg_norm'] = 1.0 + nrm((L, BW), 0.02)
    inp['br_w'] = nrm((L, N_BRANCH, BW, D), BW ** -0.5)
    inp['w_out'] = nrm((L, D, D), D ** -0.5)
    inp['router'] = nrm((L, D, N_EXPERTS), D ** -0.5)
    inp['ex_w1'] = nrm((L, N_EXPERTS, D, EXPERT_FF), D ** -0.5)
    inp['ex_w3'] = nrm((L, N_EXPERTS, D, EXPERT_FF), D ** -0.5)
    inp['ex_w2'] = nrm((L, N_EXPERTS, EXPERT_FF, D), EXPERT_FF ** -0.5)
    return inp


def reference(x_prompt, x_sample, state_rwkv, state_ret, state_hgrn, c, c_ctx, ada_w, ada_b,
              norm1_g, norm2_g, final_g, w_in, rw_mu, rw_w0, rw_w_up, rw_a0, rw_a_up, rw_g_up,
              rw_kvec, rw_ln, hy_conv, hy_ffn1, hy_ffn1_b, hy_ffn2, hy_ffn2_b, hy_ffn3, hy_freq,
              hy_decay, hy_skip, ret_rate, ret_gn, hg_lb, hg_norm, br_w, w_out, router,
              ex_w1, ex_w3, ex_w2):
    f32 = jnp.float32
    n_ctx_req = x_prompt.shape[0]
    n_lat = x_sample.shape[1]
    rows = n_lat // GRID_W
    row = jnp.repeat(jnp.arange(rows, dtype=f32), GRID_W)
    col = jnp.tile(jnp.arange(GRID_W, dtype=f32), rows)
    rope = rope_tables(row, col)
    lb_p = jax.nn.softmax(hg_lb.astype(f32), axis=0)
    lower_bounds = jnp.cumsum(lb_p, axis=0) - lb_p[0]
    zero_rw = jnp.zeros((2, n_ctx_req, N_HEADS, HEAD_DIM, HEAD_DIM), f32)
    zero_ret = jnp.zeros((2, n_ctx_req, N_HEADS, HEAD_DIM, HEAD_DIM), f32)
    zero_hg = jnp.zeros((2, n_ctx_req, N_HEADS, HG_EXPAND, HEAD_DIM), f32)
    c_context = c_ctx[None, :]
    xp, xs = x_prompt, x_sample
    rw_states, ret_states, hg_states = [], [], []
    for l in range(DEPTH):
        lp = {'w_in': w_in[l], 'ada_w': ada_w[l], 'ada_b': ada_b[l], 'norm1_g': norm1_g[l],
              'norm2_g': norm2_g[l], 'rw_mu': rw_mu[l], 'rw_w0': rw_w0[l], 'rw_w_up': rw_w_up[l],
              'rw_a0': rw_a0[l], 'rw_a_up': rw_a_up[l], 'rw_g_up': rw_g_up[l], 'rw_kvec': rw_kvec[l],
              'rw_ln': rw_ln[l], 'hy_conv': hy_conv[l], 'hy_ffn1': hy_ffn1[l], 'hy_ffn1_b': hy_ffn1_b[l],
              'hy_ffn2': hy_ffn2[l], 'hy_ffn2_b': hy_ffn2_b[l], 'hy_ffn3': hy_ffn3[l], 'hy_freq': hy_freq[l],
              'hy_decay': hy_decay[l], 'hy_skip': hy_skip[l], 'ret_rate': ret_rate[l], 'ret_gn': ret_gn[l],
              'hg_lb': lower_bounds[l], 'hg_norm': hg_norm[l], 'br_w': br_w[l], 'w_out': w_out[l],
              'router': router[l], 'ex_w1': ex_w1[l], 'ex_w3': ex_w3[l], 'ex_w2': ex_w2[l]}
        xp, (s_rw, s_ret, s_hg) = trunk_layer(xp, c_context, lp, zero_rw, zero_ret, zero_hg, None)
        rw_states.append(s_rw)
        ret_states.append(s_ret)
        hg_states.append(s_hg)
        xs, _ = trunk_layer(xs, c, lp, jnp.swapaxes(state_rwkv[:, l], 0, 1),
                            jnp.swapaxes(state_ret[:, l], 0, 1), jnp.swapaxes(state_hgrn[:, l], 0, 1), rope)
    y_prompt = rms_norm(xp, final_g)
    y_sample = rms_norm(xs, final_g)
    new_state_rwkv = pack_states(rw_states)
    new_state_ret = pack_states(ret_states)
    new_state_hgrn = pack_states(hg_states)
    return (y_prompt, y_sample, new_state_rwkv, new_state_ret, new_state_hgrn)
```

```python
import contextlib
import numpy as np
import concourse.bass as bass
import concourse.mybir as mybir

F32 = mybir.dt.float32
BF16 = mybir.dt.bfloat16
I32 = mybir.dt.int32
AF = mybir.ActivationFunctionType
ALU = mybir.AluOpType
AX = mybir.AxisListType

WRITE_KEYS = ("out", "accum_out", "ap")
N_DMA_SEMS = 96


class Eng:
    def __init__(self, name, h, sem):
        self.name, self.h, self.sem = name, h, sem
        self.cnt = 0
        self.seen = {}


class DSem:
    def __init__(self, sem):
        self.sem = sem
        self.cnt = 0


class Tile:
    def __init__(self, k, handle, space):
        self.k, self.h, self.space = k, handle, space
        self.w = {}
        self.r = {}
        self.dsem = None

    def _ap(self):
        return self.h.ap() if self.space == "dram" else self.h[:]

    @property
    def v(self):
        return View(self, self._ap())

    def __getitem__(self, idx):
        return View(self, self._ap()[idx])


class View:
    def __init__(self, tile, ap):
        self.tile, self.ap = tile, ap

    def __getitem__(self, idx):
        return View(self.tile, self.ap[idx])

    def re(self, pat, **kw):
        return View(self.tile, self.ap.rearrange(pat, **kw))

    def bc(self, shape):
        return View(self.tile, self.ap.to_broadcast(shape))

    @property
    def shape(self):
        return self.ap.shape


class Ext:
    def __init__(self, ap):
        self.ap = ap

    def __getitem__(self, idx):
        return Ext(self.ap[idx])

    def re(self, pat, **kw):
        return Ext(self.ap.rearrange(pat, **kw))

    def bc(self, shape):
        return Ext(self.ap.to_broadcast(shape))


class K:
    def __init__(self, nc):
        self.nc = nc
        self.root = contextlib.ExitStack()
        self.stacks = [self.root]
        self.scope_tiles = [[]]
        self.eng = {}
        for name, h in (("pe", nc.tensor), ("act", nc.scalar), ("dve", nc.vector),
                        ("pool", nc.gpsimd), ("sp", nc.sync)):
            sem = self.root.enter_context(nc.semaphore("sem_" + name))
            self.eng[name] = Eng(name, h, sem)
        self.dsems = [DSem(self.root.enter_context(nc.semaphore("dsem%d" % i))) for i in range(N_DMA_SEMS)]
        self.free_dsems = list(self.dsems)
        self.psums = []
        self.ps_i = 0
        self.n_ins = 0
        self.uid = 0

    def name(self, n):
        self.uid += 1
        return "%s_%d" % (n, self.uid)

    def sb(self, name, shape, dt=F32):
        h = self.stacks[-1].enter_context(self.nc.sbuf_tensor(self.name(name), list(shape), dt))
        t = Tile(self, h, "sb")
        self.scope_tiles[-1].append(t)
        return t

    def dram(self, name, shape, dt=F32):
        h = self.nc.dram_tensor(self.name(name), list(shape), dt, kind="Internal")
        t = Tile(self, h, "dram")
        self.scope_tiles[0].append(t)
        return t

    def init_psum(self, n=8):
        for i in range(n):
            h = self.root.enter_context(self.nc.psum_tensor("ps%d" % i, [128, 512], F32))
            self.psums.append(Tile(self, h, "ps"))

    def ps(self):
        while True:
            t = self.psums[self.ps_i % len(self.psums)]
            self.ps_i += 1
            if not getattr(t, "reserved", False):
                return t

    def ps_reserve(self, n):
        out = []
        for _ in range(n):
            t = self.ps()
            t.reserved = True
            out.append(t)
        return out

    def ps_release(self, tiles):
        for t in tiles:
            t.reserved = False

    @contextlib.contextmanager
    def scope(self):
        st = contextlib.ExitStack()
        self.stacks.append(st)
        self.scope_tiles.append([])
        try:
            with st:
                yield
                self.barrier()
                for t in self.scope_tiles[-1]:
                    if t.dsem is not None:
                        self.free_dsems.append(t.dsem)
                        t.dsem = None
        finally:
            self.stacks.pop()
            self.scope_tiles.pop()

    def _wait(self, e, deps):
        for d in deps:
            if d is None:
                continue
            semobj, val, owner = d
            if owner is e and e.name in ("pe", "sp"):
                continue
            key = id(semobj)
            if e.seen.get(key, 0) < val:
                e.h.wait_ge(semobj.sem, val)
                e.seen[key] = val

    def barrier(self):
        engs = list(self.eng.values())
        for e in engs:
            deps = [(x, x.cnt, x) for x in engs if x is not e and x.cnt > 0]
            deps += [(d, d.cnt, None) for d in self.dsems if d.cnt > 0]
            self._wait(e, deps)

    @staticmethod
    def _merge(d, tag):
        key = id(tag[0])
        if key not in d or d[key][1] < tag[1]:
            d[key] = tag

    def _deps(self, reads, writes, free):
        deps = []
        for t in reads:
            deps.extend(t.w.values())
        for t in writes:
            if not free:
                deps.extend(t.w.values())
                deps.extend(t.r.values())
        return deps

    def _record(self, reads, writes, tag, free):
        for t in writes:
            if free:
                self._merge(t.w, tag)
            else:
                t.w = {id(tag[0]): tag}
                t.r = {}
        for t in reads:
            if t not in writes:
                self._merge(t.r, tag)

    def op(self, engname, meth, R=(), W=(), free=False, **kw):
        e = self.eng[engname]
        reads, writes = list(R), list(W)
        args = {}
        for key, val in kw.items():
            if isinstance(val, Tile):
                val = val.v
            if isinstance(val, View):
                (writes if key in WRITE_KEYS else reads).append(val.tile)
                args[key] = val.ap
            elif isinstance(val, Ext):
                args[key] = val.ap
            else:
                args[key] = val
        self._wait(e, self._deps(reads, writes, free))
        ins = getattr(e.h, meth)(**args)
        e.cnt += 1
        ins.then_inc(e.sem, 1)
        self._record(reads, writes, (e, e.cnt, e), free)
        self.n_ins += 1
        return ins

    def dma(self, out, in_, q="sp", free=False, **kw):
        e = self.eng[q]
        if isinstance(out, Tile):
            out = out.v
        if isinstance(in_, Tile):
            in_ = in_.v
        reads = [in_.tile] if isinstance(in_, View) else []
        writes = [out.tile] if isinstance(out, View) else []
        tracked = None
        if reads:
            tracked = reads[0]
        if writes and (tracked is None or writes[0].space == "sb"):
            tracked = writes[0]
        if tracked is None:
            raise ValueError("dma needs a tracked side")
        self._wait(e, self._deps(reads, writes, free))
        if tracked.dsem is None:
            if not self.free_dsems:
                raise RuntimeError("out of dma sems")
            tracked.dsem = self.free_dsems.pop(0)
        ds = tracked.dsem
        ins = e.h.dma_start(out=out.ap, in_=in_.ap, **kw)
        ds.cnt += 16
        ins.then_inc(ds.sem, 16)
        self._record(reads, writes, (ds, ds.cnt, None), free)
        self.n_ins += 1
        return ins

    def finish(self):
        e = self.eng["sp"]
        deps = [(d, d.cnt, None) for d in self.dsems if d.cnt > 0]
        deps += [(x, x.cnt, x) for x in self.eng.values() if x is not e and x.cnt > 0]
        self._wait(e, deps)
        self.root.close()

from concourse.bass_utils import run_bass_kernel_spmd

D = 1024
L = 4
NLAYERS_RUN = 4
TS = 2048
TP = 256
NPR = 4
NCORES = 8
NC_RUN = 8
EPS = 1e-6
ST_ELEMS = L * 2 * 4 * 64 * 64
NE = 16
FF = 2048
N_IN = 8064
O_RW, O_HY, O_RET, O_HG, O_MG = 0, 896, 896 + 768, 896 + 768 + 1024, 896 + 768 + 1024 + 1280
ENABLE = dict(rw=True, hy=True, ret=True, hg=True, moe=True)
RW_STOP = [9]
RW_VAR = [0]
GROUPS_RUN = ['s', 'p']


def consts_np():
    c = {}
    c["ident"] = np.eye(128, dtype=np.float32)
    c["iota_f"] = np.tile(np.arange(256, dtype=np.float32)[None, :], (128, 1))
    c["triu"] = np.triu(np.ones((128, 128), dtype=np.float32))
    c["ones"] = np.ones((128, 128), dtype=np.float32)
    up = np.arange(2 * TS - 128, dtype=np.float32)[None, :]
    mp = np.arange(128, dtype=np.float32)[:, None]
    c["xtab"] = (up - (TS - 128) - mp).astype(np.float32)
    c["iota_n"] = np.tile(np.arange(TS, dtype=np.float32)[None, :], (128, 1))
    n = np.arange(TS)
    row = (n // 64).astype(np.float32); col = (n % 64).astype(np.float32)
    inv = (10000.0 ** (-np.arange(16, dtype=np.float32) / 16)).astype(np.float32)
    ang = np.concatenate([row[:, None] * inv, col[:, None] * inv], axis=-1).astype(np.float32)
    cosT, sinT = np.cos(ang).T.astype(np.float32), np.sin(ang).T.astype(np.float32)
    c["cos2"] = np.concatenate([cosT, cosT, cosT, cosT], axis=0)
    c["sin2"] = np.concatenate([-sinT, sinT, -sinT, sinT], axis=0)
    pm = np.zeros((128, 128), dtype=np.float32)
    for p in range(128):
        q = (p // 64) * 64 + ((p % 64) + 32) % 64
        pm[q, p] = 1.0
    c["perm"] = pm
    ev = np.zeros((128, 4), dtype=np.float32)
    for blk in range(2):
        m = blk * 128 + np.arange(128)
        ev[:, blk * 2 + 0] = TP - 1 - m
        ev[:, blk * 2 + 1] = m
    c["evals"] = ev
    t = np.arange(TS)
    c["cmask"] = np.tile((t % 32 != 0).astype(np.float32)[None, :], (128, 1))
    a = np.arange(128)
    same = (a[:, None] // 32) == (a[None, :] // 32)
    c["mk_f"] = (same & (a[:, None] <= a[None, :])).astype(np.float32)
    c["mk_b"] = (same & (a[:, None] >= a[None, :])).astype(np.float32)
    for nm, T in (("feats_s", TS), ("feats_p", TP)):
        tt = np.linspace(0.0, 1.0, T, dtype=np.float32)[:, None]
        w = ((2.0 * np.pi / T) * np.arange(T, dtype=np.float32))[:, None].astype(np.float32)
        bands = np.linspace(1e-4, 15.0, 16, dtype=np.float32)[None, :]
        feats = np.concatenate([tt, np.cos(bands * w), -np.sin(bands * w)], axis=-1).astype(np.float32)
        c[nm] = np.ascontiguousarray(feats.T)
    c["negpi"] = np.full((128, 1), -np.pi, dtype=np.float32)
    c["fcs_s"], c["gcs_s"] = dft_consts(TS)
    c["fcs_p"], c["gcs_p"] = dft_consts(TP)
    c["cmask64"] = np.tile((t % 64 != 0).astype(np.float32)[None, :], (128, 1))
    pp = np.arange(64)[:, None]; ff = np.arange(64)[None, :]
    for nm, m in (("m_lt", pp < ff), ("m_gt", pp > ff), ("m_le", pp <= ff), ("m_ge", pp >= ff)):
        c[nm] = np.ascontiguousarray(m.astype(np.float32))
    c["eye8"] = np.eye(64, dtype=np.float32)
    return c


class Rot:
    def __init__(self, tiles):
        self.t, self.i = tiles, 0

    def next(self):
        x = self.t[self.i % len(self.t)]
        self.i += 1
        return x


def build_program():
    nc = bass.Bass("TRN2", target_bir_lowering=False)
    k = K(nc)
    k.init_psum(8)
    E = {}

    def ein(name, shape, dt=F32):
        E[name] = Ext(nc.dram_tensor(name, list(shape), dt, kind="ExternalInput").ap())
        return E[name]

    def eout(name, shape, dt=F32):
        E[name] = Ext(nc.dram_tensor(name, list(shape), dt, kind="ExternalOutput").ap())
        return E[name]

    ein("xs", [TS, D]); ein("xp", [NPR * TP, D])
    ein("c_s", [1, D]); ein("c_ctx", [1, D])
    ein("final_g", [1, D])
    ein("ada_w", [NLAYERS_RUN, D, 6 * D]); ein("ada_b", [NLAYERS_RUN, 6 * D])
    ein("norm1_g", [NLAYERS_RUN, D]); ein("norm2_g", [NLAYERS_RUN, D])
    ein("w_in", [NLAYERS_RUN, D, N_IN])
    ein("br_w", [NLAYERS_RUN, 4, 256, D]); ein("w_out", [NLAYERS_RUN, D, D])
    ein("router", [NLAYERS_RUN, D, NE])
    if ENABLE["moe"]:
        ein("ex_w1", [NLAYERS_RUN, NE, D, FF]); ein("ex_w3", [NLAYERS_RUN, NE, D, FF]); ein("ex_w2", [NLAYERS_RUN, NE, FF, D])
    for nm, shp in (("ident", [128, 128]), ("iota_f", [128, 256]), ("triu", [128, 128]), ("ones", [128, 128])):
        ein(nm, shp)
    ein("st_ret", [L, 2, 4, 64, 64]); ein("ret_rate", [L, 8]); ein("ret_gn", [L, 2, 256])
    for nm, shp in (("xtab", [128, 2 * TS - 128]), ("iota_n", [128, TS]), ("cos2", [128, TS]), ("sin2", [128, TS]),
                    ("perm", [128, 128]), ("evals", [128, 4])):
        ein(nm, shp)
    ein("st_hg", [L, 2, 4, 64, 64]); ein("hg_lb", [L, 2, 256]); ein("hg_norm", [L, 256])
    for nm, shp in (("cmask", [128, TS]), ("mk_f", [128, 128]), ("mk_b", [128, 128])):
        ein(nm, shp)
    for nm, shp in (("hy_conv", [L, 3, 768]), ("hy_ffn1", [L, 33, 64]), ("hy_ffn1_b", [L, 64]), ("hy_ffn2", [L, 64, 64]),
                    ("hy_ffn2_b", [L, 64]), ("hy_ffn3", [L, 64, 1024]), ("hy_freq", [L, 2, 64]), ("hy_decay", [L, 1024]),
                    ("hy_skip", [L, 2, 256]), ("feats_s", [33, TS]), ("feats_p", [33, TP]), ("negpi", [128, 1]),
                    ("fcs_s", [2, TS // 128 + 1, 128, TS]), ("gcs_s", [2, TS // 128 + 1, 128, TS]),
                    ("fcs_p", [2, TP // 128 + 1, 128, TP]), ("gcs_p", [2, TP // 128 + 1, 128, TP])):
        ein(nm, shp)
    for nm, shp in (("st_rw", [L, 2, 4, 64, 64]), ("rw_mu", [L, 2, 896]), ("rw_w0", [L, 2, 256]), ("rw_w_up", [L, 2, 32, 256]),
                    ("rw_a0", [L, 256]), ("rw_a_up", [L, 32, 256]), ("rw_g_up", [L, 64, 256]), ("rw_kvec", [L, 3, 256]),
                    ("rw_ln", [L, 2, 256]), ("cmask64", [128, TS]), ("m_lt", [64, 64]), ("m_gt", [64, 64]),
                    ("m_le", [64, 64]), ("m_ge", [64, 64]), ("eye8", [64, 64])):
        ein(nm, shp)
    eout("ys", [TS, D]); eout("yp", [NPR * TP, D])
    eout("o_rw", [NPR, ST_ELEMS]); eout("o_ret", [NPR, ST_ELEMS]); eout("o_hg", [NPR, ST_ELEMS])

    C = {}
    for nm, shp in (("ident", [128, 128]), ("iota_f", [128, 256]), ("triu", [128, 128]), ("ones", [128, 128])):
        C[nm] = k.sb(nm, shp)
        k.dma(C[nm], E[nm])
    fg_b = k.sb("fg_b", [128, D])
    k.dma(fg_b, E["final_g"].bc([128, D]))

    with k.scope():
        zt = k.sb("zt", [128, 4096])
        k.op("pool", "memset", ap=zt.v, constant=0.0)
        for nm, en in (("o_rw", "rw"), ("o_ret", "ret"), ("o_hg", "hg")):
            if ENABLE[en] and NLAYERS_RUN == L:
                continue
            ov = E[nm].re("b (p f) -> b p f", p=128)
            for b in range(NPR):
                k.dma(ov[b], zt[:, 0:ST_ELEMS // 128])

    mod_d = k.dram("mod_d", [L, 2, 6 * D])
    adaln(k, E, C, mod_d)

    groups = (
        dict(name="s", xin=E["xs"], yout=E["ys"], NT=TS // 128, seqs=[(0, TS)], cond=0),
        dict(name="p", xin=E["xp"], yout=E["yp"], NT=NPR * TP // 128,
             seqs=[(j * TP, TP) for j in range(NPR)], cond=1),
    )
    for g in groups:
        if g['name'] not in GROUPS_RUN:
            continue
        TG = g["NT"] * 128
        g["TG"] = TG
        zT_d = k.dram("zT_" + g["name"], [N_IN, TG])
        yb_d = k.dram("yb_" + g["name"], [4 * 256, TG])
        with k.scope():
            NT = g["NT"]
            x = k.sb("x", [128, NT, D])
            xv = g["xin"].re("(n p) d -> p n d", p=128)
            yv = g["yout"].re("(n p) d -> p n d", p=128)
            for i in range(NT):
                k.dma(x[:, i, :], xv[:, i, :], free=True)
            for l in range(NLAYERS_RUN):
                stage_proj(k, E, C, g, l, x, mod_d, zT_d)
                stage_mixers(k, E, C, g, l, x, zT_d, yb_d)
                if ENABLE["ret"]:
                    stage_ret(k, E, C, g, l, zT_d, yb_d)
                if ENABLE["hg"]:
                    stage_hg(k, E, C, g, l, zT_d, yb_d)
                if ENABLE["hy"]:
                    stage_hy(k, E, C, g, l, zT_d, yb_d)
                if ENABLE["rw"]:
                    stage_rw(k, E, C, g, l, zT_d, yb_d)
                stage_merge(k, E, C, g, l, x, mod_d, zT_d, yb_d)
                if ENABLE["moe"]:
                    stage_moe(k, E, C, g, l, x, mod_d)
            with k.scope():
                final_norm(k, x, NT, fg_b, yv)
    k.finish()
    print("instructions:", k.n_ins)
    return nc


def adaln(k, E, C, mod_d):
    with k.scope():
        cc = k.sb("cc", [16, 128])
        k.dma(cc[0:8, :], E["c_s"].re("o (kc p) -> (o kc) p", p=128))
        k.dma(cc[8:16, :], E["c_ctx"].re("o (kc p) -> (o kc) p", p=128))
        cs = k.sb("cs", [16, 128])
        k.op("act", "activation", out=cs.v, in_=cc.v, func=AF.Sigmoid)
        k.op("dve", "tensor_tensor", out=cs.v, in0=cs.v, in1=cc.v, op=ALU.mult)
        ps = k.ps()
        k.op("pe", "transpose", out=ps[:, 0:16], in_=cs.v, identity=C["ident"][0:16, 0:16])
        scT = k.sb("scT", [128, 16])
        k.op("dve", "tensor_copy", out=scT.v, in_=ps[:, 0:16])
        scv = scT.v.re("p (b kc) -> p kc b", b=2)
        wb = Rot([k.sb("adaw", [128, 8, 512]) for _ in range(2)])
        bb = Rot([k.sb("adab", [2, 512]) for _ in range(2)])
        ob = Rot([k.sb("adao", [2, 512]) for _ in range(2)])
        for l in range(NLAYERS_RUN):
            for c0 in range(0, 6 * D, 512):
                w, b_, o = wb.next(), bb.next(), ob.next()
                k.dma(w, E["ada_w"][l][:, c0:c0 + 512].re("(kc p) n -> p kc n", p=128))
                k.dma(b_, E["ada_b"][l:l + 1, c0:c0 + 512].bc([2, 512]))
                ps = k.ps()
                for kc in range(8):
                    k.op("pe", "matmul", out=ps[0:2, :], lhsT=scv[:, kc, :], rhs=w[:, kc, :],
                         start=(kc == 0), stop=(kc == 7))
                k.op("dve", "tensor_tensor", out=o.v, in0=ps[0:2, :], in1=b_.v, op=ALU.add)
                k.dma(mod_d[l, :, c0:c0 + 512], o.v, free=True)


def bc_param(k, name, src_row):
    t = k.sb(name, [128, D])
    k.dma(t, src_row.bc([128, D]))
    return t


def mod_params(k, E, mod_d, l, cond, which, gname):
    off = 3 * D * which
    sh = bc_param(k, "sh", mod_d[l, cond:cond + 1, off:off + D])
    sc = bc_param(k, "sc", mod_d[l, cond:cond + 1, off + D:off + 2 * D])
    ng = bc_param(k, "ng", E[gname][l:l + 1, :])
    k.op("dve", "scalar_tensor_tensor", out=sc.v, in0=sc.v, scalar=1.0, in1=ng.v, op0=ALU.add, op1=ALU.mult)
    return sc, sh


class NormMod:
    def __init__(self, k):
        self.k = k
        self.junk = k.sb("junk", [128, D])
        self.ss = Rot([k.sb("ss", [128, 1]) for _ in range(2)])

    def __call__(self, xview, A, B, out):
        k = self.k
        ss = self.ss.next()
        k.op("act", "activation", out=self.junk.v, in_=xview, func=AF.Square, accum_out=ss.v)
        k.op("dve", "tensor_scalar", out=ss.v, in0=ss.v, scalar1=1.0 / D, scalar2=EPS, op0=ALU.mult, op1=ALU.add)
        k.op("act", "activation", out=ss.v, in_=ss.v, func=AF.Ln)
        k.op("act", "activation", out=ss.v, in_=ss.v, func=AF.Exp, scale=-0.5)
        k.op("dve", "scalar_tensor_tensor", out=out, in0=xview, scalar=ss[:, 0:1], in1=A.v,
             op0=ALU.mult, op1=ALU.mult)
        if B is not None:
            k.op("dve", "tensor_tensor", out=out, in0=out, in1=B.v, op=ALU.add)


def to_fm(k, C, h_tm, hT, i, free=True, evi=[0]):
    for j in range(2):
        ps = k.ps()
        for a in range(4):
            kc = 4 * j + a
            k.op("pe", "transpose", out=ps[:, a * 128:(a + 1) * 128], in_=h_tm[:, kc * 128:(kc + 1) * 128],
                 identity=C["ident"].v)
        eng = "act" if (evi[0] % 2) else "dve"
        evi[0] += 1
        dst = hT[:, 4 * j:4 * j + 4, i * 128:(i + 1) * 128]
        src = ps.v.re("p (a t) -> p a t", a=4)
        if eng == "act":
            k.op("act", "copy", out=dst, in_=src, free=free)
        else:
            k.op("dve", "tensor_copy", out=dst, in_=src, free=free)


def linear_fm(k, w_ext, col0, ncols, inT, KC, T, evac, wrot, WB=256, TB=512):
    for c0 in range(col0, col0 + ncols, WB):
        cw = min(WB, col0 + ncols - c0)
        wb = wrot.next()
        k.dma(wb[:, :, 0:cw], w_ext[:, c0:c0 + cw].re("(kc p) n -> p kc n", p=128))
        for n0 in range(0, cw, 128):
            nw = min(128, cw - n0)
            for t0 in range(0, T, TB):
                tw = min(TB, T - t0)
                ps = k.ps()
                for kc in range(KC):
                    k.op("pe", "matmul", out=ps[0:nw, 0:tw], lhsT=wb[:, kc, n0:n0 + nw],
                         rhs=inT[:, kc, t0:t0 + tw], start=(kc == 0), stop=(kc == KC - 1))
                evac(ps, c0 + n0, nw, t0, tw)


def stage_proj(k, E, C, g, l, x, mod_d, zT_d):
    NT, TG = g["NT"], g["TG"]
    with k.scope():
        A, B = mod_params(k, E, mod_d, l, g["cond"], 0, "norm1_g")
        nm = NormMod(k)
        hT = k.sb("hT", [128, 8, TG])
        htm = Rot([k.sb("htm", [128, D]) for _ in range(2)])
        for i in range(NT):
            h = htm.next()
            nm(x[:, i, :], A, B, h.v)
            to_fm(k, C, h, hT, i)
        wrot = Rot([k.sb("wb", [128, 8, 256]) for _ in range(2)])
        stg = Rot([k.sb("stg", [128, 512]) for _ in range(3)])
        cnt = [0]

        def evac(ps, row0, nw, t0, tw):
            s = stg.next()
            if row0 >= O_MG:
                k.op("act", "activation", out=s[0:nw, 0:tw], in_=ps[0:nw, 0:tw], func=AF.Sigmoid)
            elif cnt[0] % 2:
                k.op("act", "copy", out=s[0:nw, 0:tw], in_=ps[0:nw, 0:tw])
            else:
                k.op("dve", "tensor_copy", out=s[0:nw, 0:tw], in_=ps[0:nw, 0:tw])
            cnt[0] += 1
            k.dma(zT_d[row0:row0 + nw, t0:t0 + tw], s[0:nw, 0:tw], free=True)

        ranges = []
        if ENABLE["rw"]:
            ranges.append((O_RW, 896))
        if ENABLE["hy"]:
            ranges.append((O_HY, 768))
        if ENABLE["ret"]:
            ranges.append((O_RET, 1024))
        if ENABLE["hg"]:
            ranges.append((O_HG, 1280))
        ranges.append((O_MG, 4096))
        for (c0, n) in ranges:
            linear_fm(k, E["w_in"][l], c0, n, hT, 8, TG, evac, wrot)


def stage_mixers(k, E, C, g, l, x, zT_d, yb_d):
    TG = g["TG"]
    with k.scope():
        z = k.sb("zz", [128, 2048])
        k.op("pool", "memset", ap=z.v, constant=0.0)
        for b, nm in enumerate(("rw", "hy", "ret", "hg")):
            if not ENABLE[nm]:
                for c in range(2):
                    k.dma(yb_d[b * 256 + c * 128:b * 256 + (c + 1) * 128, :], z[:, 0:TG], free=True)


def stage_ret(k, E, C, g, l, zT_d, yb_d):
    TG, seqs = g["TG"], g["seqs"]
    is_s = g["name"] == "s"
    zq, zk, zv, zg = O_RET, O_RET + 256, O_RET + 512, O_RET + 768
    if is_s:
        with k.scope():
            cos2 = k.sb("cos2", [128, TS]); k.dma(cos2, E["cos2"])
            sin2 = k.sb("sin2", [128, TS]); k.dma(sin2, E["sin2"])
            perm = k.sb("perm", [128, 128]); k.dma(perm, E["perm"])
            tq = Rot([k.sb("rq", [128, TS]) for _ in range(2)])
            to = Rot([k.sb("ro", [128, TS]) for _ in range(2)])
            tmp = Rot([k.sb("rt", [128, 512]) for _ in range(2)])
            for r0 in (zq, zq + 128, zk, zk + 128):
                t, o = tq.next(), to.next()
                k.dma(t, zT_d[r0:r0 + 128, :])
                for nb in range(0, TS, 512):
                    ps = k.ps()
                    k.op("pe", "matmul", out=ps.v, lhsT=perm.v, rhs=t[:, nb:nb + 512], start=True, stop=True)
                    tm = tmp.next()
                    k.op("dve", "tensor_tensor", out=tm.v, in0=ps.v, in1=sin2[:, nb:nb + 512], op=ALU.mult)
                    k.op("pool", "tensor_tensor", out=o[:, nb:nb + 512], in0=t[:, nb:nb + 512],
                         in1=cos2[:, nb:nb + 512], op=ALU.mult)
                    k.op("dve", "tensor_tensor", out=o[:, nb:nb + 512], in0=o[:, nb:nb + 512], in1=tm.v, op=ALU.add)
                k.dma(zT_d[r0:r0 + 128, :], o.v)
    with k.scope():
        lg = k.sb("lg", [128, 8])
        nlg = k.sb("nlg", [128, 8])
        lgT = k.sb("lgT", [128, 8])
        k.dma(lg, E["ret_rate"][l:l + 1, :].bc([128, 8]))
        k.op("act", "activation", out=nlg.v, in_=lg.v, func=AF.Exp)
        k.op("dve", "tensor_scalar", out=lg.v, in0=nlg.v, scalar1=-1.0, scalar2=None, op0=ALU.mult)
        gn4 = k.sb("gn4", [4, 128])
        k.dma(gn4, E["ret_gn"][l].re("g (hp c) -> (g hp) c", c=128))
        psg = k.ps()
        k.op("pe", "transpose", out=psg[:, 0:4], in_=gn4.v, identity=C["ident"][0:4, 0:4])
        gnT = k.sb("gnT", [128, 4])
        k.op("dve", "tensor_copy", out=gnT.v, in_=psg[:, 0:4])
        Tmax = max(T for (_, T) in seqs)
        k.op("dve", "tensor_scalar", out=lgT.v, in0=lg.v, scalar1=float(Tmax), scalar2=None, op0=ALU.mult)
        WW = 2 * Tmax - 128
        xoff = (TS - 128) - (Tmax - 128)
        Xt = k.sb("Xt", [128, WW]); k.dma(Xt, E["xtab"][:, xoff:xoff + WW])
        W = k.sb("W", [128, WW])
        HW = WW // 2
        tmpW = k.sb("tmpW", [128, HW])
        NB = min(512, Tmax)
        nnb = Tmax // NB
        nblk = Tmax // 128
        qTt, kTt, vTt, gTt = (k.sb(nm, [128, Tmax]) for nm in ("qT", "kT", "vT", "gT"))
        v_tm = k.sb("v_tm", [128, nblk, 128])
        yo = k.sb("yo", [128, Tmax])
        atr = Rot([k.sb("at", [128, Tmax]) for _ in range(2)])
        t64 = Rot([k.sb("t64", [128, NB]) for _ in range(6)])
        if is_s:
            iota_n = k.sb("iota_n", [128, TS]); k.dma(iota_n, E["iota_n"])
            s0f = k.sb("s0f", [128, 128]); s0b = k.sb("s0b", [128, 128])
        else:
            k_tm = k.sb("k_tm", [128, nblk, 128])
            evals = k.sb("evals", [128, 4]); k.dma(evals, E["evals"])
            tokdec = k.sb("tokdec", [128, 2, 8])
            for blk in range(2):
                for d in range(2):
                    for h in range(4):
                        k.op("act", "activation", out=tokdec[:, blk, d * 4 + h:d * 4 + h + 1],
                             in_=evals[:, blk * 2 + d:blk * 2 + d + 1], func=AF.Exp, scale=lg[:, d * 4 + h:d * 4 + h + 1])
            kdr = Rot([k.sb("kd", [128, 64]) for _ in range(2)])
            sor = Rot([k.sb("so", [64, 64]) for _ in range(2)])
            o_ret = E["o_ret"].re("b (l d h k v) -> b l d h k v", l=L, d=2, h=4, k=64)
        for sj, (s0, T) in enumerate(seqs):
            for hp in range(2):
                k.dma(qTt, zT_d[zq + hp * 128:zq + (hp + 1) * 128, s0:s0 + T])
                k.dma(kTt, zT_d[zk + hp * 128:zk + (hp + 1) * 128, s0:s0 + T])
                k.dma(vTt, zT_d[zv + hp * 128:zv + (hp + 1) * 128, s0:s0 + T])
                k.dma(gTt, zT_d[zg + hp * 128:zg + (hp + 1) * 128, s0:s0 + T])
                k.op("act", "mul", out=qTt.v, in_=qTt.v, mul=0.125)
                for blk in range(nblk):
                    ps = k.ps()
                    k.op("pe", "transpose", out=ps[:, 0:128], in_=vTt[:, blk * 128:(blk + 1) * 128], identity=C["ident"].v)
                    k.op("act", "copy", out=v_tm[:, blk, :], in_=ps[:, 0:128])
                    if not is_s:
                        ps2 = k.ps()
                        k.op("pe", "transpose", out=ps2[:, 0:128], in_=kTt[:, blk * 128:(blk + 1) * 128],
                             identity=C["ident"].v)
                        k.op("dve", "tensor_copy", out=k_tm[:, blk, :], in_=ps2[:, 0:128])
                if is_s:
                    k.op("pool", "memset", ap=s0f.v, constant=0.0)
                    k.op("pool", "memset", ap=s0b.v, constant=0.0)
                    for hh in range(2):
                        P0 = hh * 64
                        k.dma(s0f[P0:P0 + 64, P0:P0 + 64], E["st_ret"][l, 0, hp * 2 + hh])
                        k.dma(s0b[P0:P0 + 64, P0:P0 + 64], E["st_ret"][l, 1, hp * 2 + hh])
                for hh in range(2):
                    h = hp * 2 + hh
                    P0 = hh * 64
                    sl = slice(P0, P0 + 64)
                    cf, cb = h, 4 + h
                    for half in range(2):
                        c0, c1 = half * HW, (half + 1) * HW
                        k.op("act", "activation", out=tmpW.v, in_=Xt[:, c0:c1], func=AF.Exp, scale=lg[:, cf:cf + 1])
                        k.op("dve", "scalar_tensor_tensor", out=W[:, c0:c1], in0=Xt[:, c0:c1], scalar=0.0, in1=tmpW.v,
                             op0=ALU.is_ge, op1=ALU.mult)
                        k.op("act", "activation", out=tmpW.v, in_=Xt[:, c0:c1], func=AF.Exp, scale=nlg[:, cb:cb + 1])
                        k.op("dve", "scalar_tensor_tensor", out=tmpW.v, in0=Xt[:, c0:c1], scalar=0.0, in1=tmpW.v,
                             op0=ALU.is_le, op1=ALU.mult)
                        k.op("pool", "tensor_tensor", out=W[:, c0:c1], in0=W[:, c0:c1], in1=tmpW.v, op=ALU.add)
                    acc = k.ps_reserve(nnb)
                    if is_s:
                        for nb in range(nnb):
                            cs = slice(nb * NB, (nb + 1) * NB)
                            d1, d2 = t64.next(), t64.next()
                            k.op("act", "activation", out=d1[sl, :], in_=iota_n[sl, cs], func=AF.Exp,
                                 scale=lg[sl, cf:cf + 1], bias=lg[sl, cf:cf + 1])
                            k.op("dve", "tensor_tensor", out=d1[sl, :], in0=d1[sl, :], in1=qTt[sl, cs], op=ALU.mult)
                            k.op("pe", "matmul", out=acc[nb][:, 0:NB], lhsT=s0f[sl, :], rhs=d1[sl, :], start=True, stop=False)
                            k.op("act", "activation", out=d2[sl, :], in_=iota_n[sl, cs], func=AF.Exp,
                                 scale=nlg[sl, cb:cb + 1], bias=lgT[sl, cb:cb + 1])
                            k.op("dve", "tensor_tensor", out=d2[sl, :], in0=d2[sl, :], in1=qTt[sl, cs], op=ALU.mult)
                            k.op("pe", "matmul", out=acc[nb][:, 0:NB], lhsT=s0b[sl, :], rhs=d2[sl, :], start=False, stop=False)
                    for j in range(nblk):
                        at = atr.next()
                        for nb in range(nnb):
                            ps = k.ps()
                            k.op("pe", "matmul", out=ps[:, 0:NB], lhsT=kTt[sl, j * 128:(j + 1) * 128],
                                 rhs=qTt[sl, nb * NB:(nb + 1) * NB], start=True, stop=True)
                            wc = (T - 128 - 128 * j) + nb * NB
                            k.op("dve", "tensor_tensor", out=at[:, nb * NB:(nb + 1) * NB], in0=ps[:, 0:NB],
                                 in1=W[:, wc:wc + NB], op=ALU.mult)
                        for nb in range(nnb):
                            k.op("pe", "matmul", out=acc[nb][:, 0:NB], lhsT=v_tm[:, j, :], rhs=at[:, nb * NB:(nb + 1) * NB],
                                 start=(j == 0 and not is_s), stop=(j == nblk - 1))
                    for nb in range(nnb):
                        cs = slice(nb * NB, (nb + 1) * NB)
                        ysb, cen, sq, rs, sg = (t64.next() for _ in range(5))
                        k.op("act", "copy", out=ysb[sl, :], in_=acc[nb][sl, 0:NB])
                        pm = k.ps()
                        k.op("pe", "matmul", out=pm[:, 0:NB], lhsT=C["ones"][sl, :], rhs=ysb[sl, :], start=True, stop=True)
                        k.op("dve", "scalar_tensor_tensor", out=cen[sl, :], in0=pm[sl, 0:NB], scalar=-1.0 / 64, in1=ysb[sl, :],
                             op0=ALU.mult, op1=ALU.add)
                        k.op("act", "activation", out=sq[sl, :], in_=cen[sl, :], func=AF.Square)
                        pv = k.ps()
                        k.op("pe", "matmul", out=pv[:, 0:NB], lhsT=C["ones"][sl, :], rhs=sq[sl, :], start=True, stop=True)
                        k.op("dve", "tensor_scalar", out=rs[sl, :], in0=pv[sl, 0:NB], scalar1=1.0 / 64, scalar2=1e-5,
                             op0=ALU.mult, op1=ALU.add)
                        k.op("act", "activation", out=rs[sl, :], in_=rs[sl, :], func=AF.Ln)
                        k.op("act", "activation", out=rs[sl, :], in_=rs[sl, :], func=AF.Exp, scale=-0.5)
                        k.op("dve", "tensor_tensor", out=cen[sl, :], in0=cen[sl, :], in1=rs[sl, :], op=ALU.mult)
                        k.op("dve", "tensor_scalar", out=cen[sl, :], in0=cen[sl, :], scalar1=gnT[sl, hp:hp + 1],
                             scalar2=gnT[sl, 2 + hp:3 + hp], op0=ALU.mult, op1=ALU.add)
                        k.op("act", "activation", out=sg[sl, :], in_=gTt[sl, cs], func=AF.Sigmoid)
                        k.op("dve", "tensor_tensor", out=sg[sl, :], in0=sg[sl, :], in1=gTt[sl, cs], op=ALU.mult)
                        k.op("dve", "tensor_tensor", out=yo[sl, cs], in0=cen[sl, :], in1=sg[sl, :], op=ALU.mult)
                    k.ps_release(acc)
                    if not is_s:
                        b = sj
                        for d in range(2):
                            pS = k.ps()
                            for blk in range(nblk):
                                kd = kdr.next()
                                k.op("dve", "tensor_scalar", out=kd.v, in0=k_tm[:, blk, P0:P0 + 64],
                                     scalar1=tokdec[:, blk, d * 4 + h:d * 4 + h + 1], scalar2=None, op0=ALU.mult)
                                k.op("pe", "matmul", out=pS[0:64, 0:64], lhsT=kd.v, rhs=v_tm[:, blk, P0:P0 + 64],
                                     start=(blk == 0), stop=(blk == nblk - 1))
                            so = sor.next()
                            k.op("act", "copy", out=so.v, in_=pS[0:64, 0:64])
                            k.dma(o_ret[b, l, d, h], so.v)
                k.dma(yb_d[512 + hp * 128:512 + (hp + 1) * 128, s0:s0 + T], yo[:, 0:T])


def stage_hg(k, E, C, g, l, zT_d, yb_d):
    TG, seqs = g["TG"], g["seqs"]
    is_s = g["name"] == "s"
    z0 = O_HG
    with k.scope():
        lb16 = k.sb("lb16", [16, 128])
        k.dma(lb16, E["hg_lb"].re("l d (hp c) -> (l d hp) c", c=128))
        ps = k.ps()
        k.op("pe", "transpose", out=ps[:, 0:16], in_=lb16.v, identity=C["ident"][0:16, 0:16])
        lbT = k.sb("lbT", [128, 4, 4])
        k.op("dve", "tensor_copy", out=lbT.v.re("p l j -> p (l j)"), in_=ps[:, 0:16])
        mx = k.sb("mx4", [128, 4]); sm4 = k.sb("sm4", [128, 4])
        k.op("dve", "tensor_tensor", out=mx.v, in0=lbT[:, 0, :], in1=lbT[:, 1, :], op=ALU.max)
        k.op("dve", "tensor_tensor", out=mx.v, in0=mx.v, in1=lbT[:, 2, :], op=ALU.max)
        k.op("dve", "tensor_tensor", out=mx.v, in0=mx.v, in1=lbT[:, 3, :], op=ALU.max)
        for ll in range(4):
            k.op("dve", "tensor_tensor", out=lbT[:, ll, :], in0=lbT[:, ll, :], in1=mx.v, op=ALU.subtract)
        k.op("act", "activation", out=lbT.v, in_=lbT.v, func=AF.Exp)
        k.op("dve", "tensor_tensor", out=sm4.v, in0=lbT[:, 0, :], in1=lbT[:, 1, :], op=ALU.add)
        k.op("dve", "tensor_tensor", out=sm4.v, in0=sm4.v, in1=lbT[:, 2, :], op=ALU.add)
        k.op("dve", "tensor_tensor", out=sm4.v, in0=sm4.v, in1=lbT[:, 3, :], op=ALU.add)
        k.op("dve", "reciprocal", out=sm4.v, in_=sm4.v)
        lbl = k.sb("lbl", [128, 4]); oml = k.sb("oml", [128, 4]); noml = k.sb("noml", [128, 4]); lbf = k.sb("lbf", [128, 4])
        k.op("pool", "memset", ap=lbl.v, constant=0.0)
        for ll in range(1, l + 1):
            k.op("dve", "tensor_tensor", out=mx.v, in0=lbT[:, ll, :], in1=sm4.v, op=ALU.mult)
            k.op("dve", "tensor_tensor", out=lbl.v, in0=lbl.v, in1=mx.v, op=ALU.add)
        k.op("dve", "tensor_scalar", out=oml.v, in0=lbl.v, scalar1=-1.0, scalar2=1.0, op0=ALU.mult, op1=ALU.add)
        k.op("dve", "tensor_scalar", out=noml.v, in0=oml.v, scalar1=-1.0, scalar2=None, op0=ALU.mult)
        k.op("dve", "tensor_scalar", out=lbf.v, in0=lbl.v, scalar1=1e-30, scalar2=None, op0=ALU.max)
        hn2 = k.sb("hn2", [2, 128])
        k.dma(hn2, E["hg_norm"][l:l + 1, :].re("o (hp c) -> (o hp) c", c=128))
        ps = k.ps()
        k.op("pe", "transpose", out=ps[:, 0:2], in_=hn2.v, identity=C["ident"][0:2, 0:2])
        hnT = k.sb("hnT", [128, 2])
        k.op("dve", "tensor_copy", out=hnT.v, in_=ps[:, 0:2])
        Tm = max(T for (_, T) in seqs)
        nblk, nch = Tm // 128, Tm // 32
        NB = min(512, Tm)
        cm = k.sb("cm", [128, Tm]); k.dma(cm, E["cmask"][:, 0:Tm])
        mk = [k.sb("mkf", [128, 128]), k.sb("mkb", [128, 128])]
        k.dma(mk[0], E["mk_f"]); k.dma(mk[1], E["mk_b"])
        qh, zf, vT, gh = (k.sb(nm, [128, Tm]) for nm in ("qh", "zf", "vT", "gh"))
        qt, kt, t1 = (k.sb(nm, [128, Tm]) for nm in ("qt", "kt", "t1"))
        kin, bb = zf, vT
        bend = k.sb("bend", [128, nch]); ebend = k.sb("ebend", [128, nch])
        v_tm = k.sb("v_tm", [128, nblk, 128]); kh_tm = k.sb("kh_tm", [64, 2 * nblk, 128])
        v_t64 = k.sb("v_t64", [64, 2 * nblk, 128])
        yd = [k.sb("yf", [128, Tm]), k.sb("ybw", [128, Tm])]
        S = [[k.sb("S%d%d" % (hh, i), [128, 128]) for i in range(2)] for hh in range(2)]
        atr = [Rot([k.sb("at%d" % hh, [128, 128]) for _ in range(2)]) for hh in range(2)]
        t64 = Rot([k.sb("h64", [128, NB]) for _ in range(4)])
        sor = Rot([k.sb("hso", [128, 64]) for _ in range(2)])
        o_hg = E["o_hg"].re("b (l d h k v) -> b l d h k v", l=L, d=2, h=4, k=64)
        c3 = lambda t: t.v.re("p (c s) -> p c s", s=32)
        for sj, (s0, T) in enumerate(seqs):
            for hp in range(2):
                r = lambda o: zT_d[z0 + o + hp * 128:z0 + o + (hp + 1) * 128, s0:s0 + T]
                k.dma(qh, r(0)); k.dma(vT, r(768)); k.dma(gh, r(1024))
                k.op("act", "activation", out=t1.v, in_=qh.v, func=AF.Sigmoid)
                k.op("dve", "tensor_tensor", out=qh.v, in0=qh.v, in1=t1.v, op=ALU.mult)
                for blk in range(nblk):
                    ps = k.ps()
                    k.op("pe", "transpose", out=ps[:, 0:128], in_=vT[:, blk * 128:(blk + 1) * 128], identity=C["ident"].v)
                    k.op("act", "copy", out=v_tm[:, blk, :], in_=ps[:, 0:128])
                for hb in range(2 * nblk):
                    ps = k.ps()
                    k.op("pe", "transpose", out=ps[0:64, 0:128], in_=vT[:, hb * 64:(hb + 1) * 64], identity=C["ident"].v)
                    k.op("dve", "tensor_copy", out=v_t64[:, hb, :], in_=ps[0:64, 0:128])
                for d in range(2):
                    j = d * 2 + hp
                    k.dma(zf, r(256 + 256 * d))
                    k.op("act", "activation", out=t1.v, in_=zf.v, func=AF.Sigmoid)
                    k.op("dve", "tensor_scalar", out=kin.v, in0=t1.v, scalar1=noml[:, j:j + 1], scalar2=oml[:, j:j + 1],
                         op0=ALU.mult, op1=ALU.add)
                    k.op("dve", "tensor_scalar", out=t1.v, in0=t1.v, scalar1=oml[:, j:j + 1], scalar2=lbf[:, j:j + 1],
                         op0=ALU.mult, op1=ALU.add)
                    k.op("act", "activation", out=t1.v, in_=t1.v, func=AF.Ln)
                    k.op("dve", "tensor_tensor_scan", out=bb.v, data0=cm.v, data1=t1.v, initial=0.0,
                         op0=ALU.mult, op1=ALU.add)
                    k.op("dve", "tensor_copy", out=bend.v, in_=c3(bb)[:, :, 31])
                    if d == 1:
                        k.op("dve", "tensor_tensor", out=c3(bb), in0=bend.v.re("p (c o) -> p c o", o=1).bc([128, nch, 32]),
                             in1=c3(bb), op=ALU.subtract)
                        k.op("dve", "tensor_tensor", out=bb.v, in0=bb.v, in1=t1.v, op=ALU.add)
                    k.op("act", "activation", out=t1.v, in_=bb.v, func=AF.Exp)
                    k.op("dve", "tensor_tensor", out=qt.v, in0=qh.v, in1=t1.v, op=ALU.mult)
                    k.op("act", "activation", out=t1.v, in_=bb.v, func=AF.Exp, scale=-1.0)
                    k.op("dve", "tensor_tensor", out=kt.v, in0=kin.v, in1=t1.v, op=ALU.mult)
                    k.op("act", "activation", out=ebend.v, in_=bend.v, func=AF.Exp)
                    k.op("dve", "tensor_tensor", out=c3(t1), in0=c3(kt),
                         in1=ebend.v.re("p (c o) -> p c o", o=1).bc([128, nch, 32]), op=ALU.mult)
                    for hb in range(2 * nblk):
                        ps = k.ps()
                        k.op("pe", "transpose", out=ps[0:64, 0:128], in_=t1[:, hb * 64:(hb + 1) * 64], identity=C["ident"].v)
                        k.op("act", "copy", out=kh_tm[:, hb, :], in_=ps[0:64, 0:128])
                    cur = [0, 0]
                    for hh in range(2):
                        P0 = hh * 64
                        for i in range(2):
                            k.op("pool", "memset", ap=S[hh][i].v, constant=0.0)
                        if is_s:
                            k.dma(S[hh][0][P0:P0 + 64, P0:P0 + 64], E["st_hg"][l, d, hp * 2 + hh])
                    blks = range(nblk) if d == 0 else range(nblk - 1, -1, -1)
                    chs = range(4) if d == 0 else range(3, -1, -1)
                    for blk in blks:
                        bc_ = slice(blk * 128, (blk + 1) * 128)
                        ats, accs = [], []
                        for hh in range(2):
                            sl = slice(hh * 64, hh * 64 + 64)
                            ps = k.ps()
                            k.op("pe", "matmul", out=ps[:, 0:128], lhsT=kt[sl, bc_], rhs=qt[sl, bc_], start=True, stop=True)
                            at = atr[hh].next()
                            k.op("dve", "tensor_tensor", out=at.v, in0=ps[:, 0:128], in1=mk[d].v, op=ALU.mult)
                            ats.append(at)
                        accs = k.ps_reserve(2)
                        for c in chs:
                            lc = slice(c * 32, (c + 1) * 32)
                            gc = slice(blk * 128 + c * 32, blk * 128 + (c + 1) * 32)
                            ci = blk * 4 + c
                            for hh in range(2):
                                P0 = hh * 64
                                sl = slice(P0, P0 + 64)
                                Sc, Sn = S[hh][cur[hh]], S[hh][1 - cur[hh]]
                                k.op("pe", "matmul", out=accs[hh][:, lc], lhsT=Sc[sl, :], rhs=qt[sl, gc], start=True, stop=False)
                                k.op("pe", "matmul", out=accs[hh][:, lc], lhsT=v_tm[:, blk, :], rhs=ats[hh][:, lc],
                                     start=False, stop=True)
                                pu = k.ps()
                                hb, pb = blk * 2 + c // 2, (c % 2) * 32
                                k.op("pe", "matmul", out=pu[:, 0:64], lhsT=kh_tm[pb:pb + 32, hb, :],
                                     rhs=v_t64[pb:pb + 32, hb, P0:P0 + 64], start=True, stop=True)
                                k.op("dve", "scalar_tensor_tensor", out=Sn[sl, P0:P0 + 64], in0=Sc[sl, P0:P0 + 64],
                                     scalar=ebend[sl, ci:ci + 1], in1=pu[sl, 0:64], op0=ALU.mult, op1=ALU.add)
                                cur[hh] = 1 - cur[hh]
                        for hh in range(2):
                            sl = slice(hh * 64, hh * 64 + 64)
                            k.op("act", "copy", out=yd[d][sl, bc_], in_=accs[hh][sl, 0:128])
                        k.ps_release(accs)
                    if not is_s:
                        for hh in range(2):
                            P0 = hh * 64
                            so = sor.next()
                            k.op("dve", "tensor_copy", out=so[P0:P0 + 64, :], in_=S[hh][cur[hh]][P0:P0 + 64, P0:P0 + 64])
                            k.dma(o_hg[sj, l, d, hp * 2 + hh], so[P0:P0 + 64, :])
                k.op("dve", "tensor_tensor", out=yd[0].v, in0=yd[0].v, in1=yd[1].v, op=ALU.add)
                k.op("act", "activation", out=t1.v, in_=gh.v, func=AF.Sigmoid)
                k.op("dve", "tensor_tensor", out=gh.v, in0=gh.v, in1=t1.v, op=ALU.mult)
                for nb in range(T // NB):
                    cs = slice(nb * NB, (nb + 1) * NB)
                    for hh in range(2):
                        sl = slice(hh * 64, hh * 64 + 64)
                        sq, rs = t64.next(), t64.next()
                        k.op("act", "activation", out=sq[sl, :], in_=yd[0][sl, cs], func=AF.Square)
                        pm = k.ps()
                        k.op("pe", "matmul", out=pm[:, 0:NB], lhsT=C["ones"][sl, :], rhs=sq[sl, :], start=True, stop=True)
                        k.op("dve", "tensor_scalar", out=rs[sl, :], in0=pm[sl, 0:NB], scalar1=1.0 / 64, scalar2=EPS,
                             op0=ALU.mult, op1=ALU.add)
                        k.op("act", "activation", out=rs[sl, :], in_=rs[sl, :], func=AF.Ln)
                        k.op("act", "activation", out=rs[sl, :], in_=rs[sl, :], func=AF.Exp, scale=-0.5)
                        k.op("dve", "scalar_tensor_tensor", out=rs[sl, :], in0=rs[sl, :], scalar=hnT[sl, hp:hp + 1],
                             in1=yd[0][sl, cs], op0=ALU.mult, op1=ALU.mult)
                        k.op("dve", "tensor_tensor", out=yd[1][sl, cs], in0=rs[sl, :], in1=gh[sl, cs], op=ALU.mult)
                k.dma(yb_d[768 + hp * 128:768 + (hp + 1) * 128, s0:s0 + T], yd[1][:, 0:T])


def load_T(k, C, src, n, name):
    st = k.sb(name + "_st", [n, 128])
    k.dma(st, src)
    ps = k.ps()
    k.op("pe", "transpose", out=ps[:, 0:n], in_=st.v, identity=C["ident"][0:n, 0:n])
    t = k.sb(name, [128, n])
    k.op("dve", "tensor_copy", out=t.v, in_=ps[:, 0:n])
    return t


def dft_consts(T):
    nblk, NFC = T // 128, T // 128 + 1
    N2 = 2 * T
    f = np.arange(NFC * 128, dtype=np.float64)
    t = np.arange(T, dtype=np.float64)
    th = 2.0 * np.pi * np.outer(f, t) / N2
    valid = (f <= T)[:, None]
    c = np.where(valid, np.cos(th), 0.0)
    s_ = np.where(valid, np.sin(th), 0.0)
    def fw(m):
        a = m.reshape(NFC, 128, nblk, 128)
        return np.ascontiguousarray(a.transpose(0, 3, 2, 1).reshape(NFC, 128, nblk * 128))
    fcs = np.stack([fw(c), fw(s_)]).astype(np.float32)
    wf = np.where((f == 0) | (f == T), 1.0, 2.0)[:, None] * valid
    gc = (wf * c / N2).reshape(NFC, 128, T)
    gs = (-wf * s_ / N2).reshape(NFC, 128, T)
    gcs = np.stack([gc, gs]).astype(np.float32)
    return fcs, gcs


def stage_hy(k, E, C, g, l, zT_d, yb_d):
    TG, seqs = g["TG"], g["seqs"]
    T = seqs[0][1]
    sfx = "s" if T == TS else "p"
    FCS, GCS = E["fcs_" + sfx], E["gcs_" + sfx]
    nblk, NFC = T // 128, T // 128 + 1
    z0 = O_HY
    PI = float(np.pi)
    NB = min(512, T)
    with k.scope():
        skT = load_T(k, C, E["hy_skip"][l].re("o (c p) -> (o c) p", p=128), 4, "skT")
        hcT = load_T(k, C, E["hy_conv"][l].re("w (c p) -> (w c) p", p=128), 18, "hcT")
        decT = load_T(k, C, E["hy_decay"][l:l + 1, :].re("o (c p) -> (o c) p", p=128), 8, "decT")
        dneg = k.sb("dneg", [128, 8])
        k.op("dve", "tensor_scalar", out=dneg.v, in0=decT.v, scalar1=-1.0, scalar2=None, op0=ALU.mult)
        k.op("dve", "tensor_tensor", out=decT.v, in0=decT.v, in1=dneg.v, op=ALU.max)
        k.op("dve", "tensor_scalar", out=decT.v, in0=decT.v, scalar1=-1.0 / (T - 1), scalar2=None, op0=ALU.mult)
        w3 = k.sb("hw3", [64, 1024]); k.dma(w3, E["hy_ffn3"][l])
        hid2 = k.sb("hid2", [64, T])
        with k.scope():
            featsT = k.sb("featsT", [33, T]); k.dma(featsT, E["feats_s" if T == TS else "feats_p"])
            w1 = k.sb("hw1", [33, 64]); k.dma(w1, E["hy_ffn1"][l])
            w2 = k.sb("hw2", [64, 64]); k.dma(w2, E["hy_ffn2"][l])
            pr = k.sb("hpr", [64, 4])
            k.dma(pr[:, 0:1], E["hy_ffn1_b"][l:l + 1, :].re("o (p q) -> (o p) q", q=1))
            k.dma(pr[:, 1:2], E["hy_ffn2_b"][l:l + 1, :].re("o (p q) -> (o p) q", q=1))
            k.dma(pr[:, 2:3], E["hy_freq"][l, 0:1, :].re("o (p q) -> (o p) q", q=1))
            k.dma(pr[:, 3:4], E["hy_freq"][l, 1:2, :].re("o (p q) -> (o p) q", q=1))
            hid1 = k.sb("hid1", [64, T])
            m1 = k.sb("hm1", [64, NB])

            def sin_layer(wt, rhs, bcol, fcol, dst):
                for n0 in range(0, T, NB):
                    ps = k.ps()
                    k.op("pe", "matmul", out=ps[0:64, 0:NB], lhsT=wt, rhs=rhs[:, n0:n0 + NB], start=True, stop=True)
                    a = dst[:, n0:n0 + NB]
                    k.op("dve", "tensor_scalar", out=a, in0=ps[0:64, 0:NB], scalar1=pr[:, bcol:bcol + 1],
                         scalar2=pr[:, fcol:fcol + 1], op0=ALU.add, op1=ALU.mult)
                    k.op("dve", "tensor_scalar", out=m1.v, in0=a, scalar1=PI, scalar2=-2.0 * PI, op0=ALU.is_ge, op1=ALU.mult)
                    k.op("dve", "tensor_tensor", out=a, in0=a, in1=m1.v, op=ALU.add)
                    k.op("dve", "tensor_scalar", out=m1.v, in0=a, scalar1=-PI, scalar2=2.0 * PI, op0=ALU.is_le, op1=ALU.mult)
                    k.op("dve", "tensor_tensor", out=a, in0=a, in1=m1.v, op=ALU.add)
                    k.op("act", "activation", out=a, in_=a, func=AF.Sin)

            sin_layer(w1.v, featsT, 0, 2, hid1)
            sin_layer(w2.v, hid1, 1, 3, hid2)
        frot = Rot([k.sb("hF", [128, nblk, 128]) for _ in range(2)])
        grot = Rot([k.sb("hG", [128, NB]) for _ in range(3)])
        tmpc = Rot([k.sb("htc", [128, 128]) for _ in range(2)])

        def fwd_dft(x_tm, ncol, sink):
            for fc in range(NFC):
                for part in range(2):
                    F = frot.next()
                    k.dma(F.v.re("p b f -> p (b f)"), FCS[part, fc])
                    ps = k.ps()
                    for blk in range(nblk):
                        k.op("pe", "matmul", out=ps[:, 0:ncol], lhsT=F[:, blk, :], rhs=x_tm[:, blk, 0:ncol],
                             start=(blk == 0), stop=(blk == nblk - 1))
                    sink(part, fc, ps)

        for chh in range(2):
            with k.scope():
                Cre = [k.sb("Cre%d" % o, [128, NFC, 128]) for o in range(2)]
                Cim = [k.sb("Cim%d" % o, [128, NFC, 128]) for o in range(2)]
                with k.scope():
                    iota_n = k.sb("iota_n", [128, T]); k.dma(iota_n, E["iota_n"][:, 0:T])
                    ed = k.sb("hed", [128, T])
                    junk = k.sb("hjunk", [128, T])
                    hfb = [k.sb("hfb%d" % i, [128, T]) for i in range(2)]
                    asum = k.sb("asum", [128, 2])
                    f_tm = k.sb("f_tm", [128, nblk, 256])
                    for o in range(2):
                        for di in range(2):
                            c = o * 4 + di * 2 + chh
                            k.op("act", "activation", out=ed.v, in_=iota_n.v, func=AF.Exp, scale=decT[:, c:c + 1])
                            for n0 in range(0, T, NB):
                                ps = k.ps()
                                k.op("pe", "matmul", out=ps[:, 0:NB], lhsT=w3[:, c * 128:(c + 1) * 128], rhs=hid2[:, n0:n0 + NB],
                                     start=True, stop=True)
                                k.op("dve", "tensor_tensor", out=hfb[di][:, n0:n0 + NB], in0=ps[:, 0:NB], in1=ed[:, n0:n0 + NB],
                                     op=ALU.mult)
                            k.op("dve", "tensor_scalar", out=junk.v, in0=hfb[di].v, scalar1=-1.0, scalar2=None, op0=ALU.mult)
                            k.op("dve", "tensor_tensor", out=junk.v, in0=junk.v, in1=hfb[di].v, op=ALU.max)
                            k.op("dve", "reduce_sum", out=asum[:, di:di + 1], in_=junk.v, axis=AX.X)
                        k.op("dve", "tensor_tensor", out=asum[:, 0:1], in0=asum[:, 0:1], in1=asum[:, 1:2], op=ALU.add)
                        k.op("dve", "reciprocal", out=asum[:, 0:1], in_=asum[:, 0:1])
                        for di in range(2):
                            k.op("dve", "tensor_scalar", out=hfb[di].v, in0=hfb[di].v, scalar1=asum[:, 0:1], scalar2=None, op0=ALU.mult)
                        k.op("pool", "memset", ap=hfb[1][:, 0:1], constant=0.0)
                        for blk in range(nblk):
                            ps = k.ps()
                            for di in range(2):
                                k.op("pe", "transpose", out=ps[:, di * 128:(di + 1) * 128], in_=hfb[di][:, blk * 128:(blk + 1) * 128],
                                     identity=C["ident"].v)
                            k.op("act", "copy", out=f_tm[:, blk, :], in_=ps[:, 0:256])

                        def sink_f(part, fc, ps, o=o):
                            tc_ = tmpc.next()
                            k.op("act", "copy", out=tc_.v, in_=ps[:, 0:128])
                            if part == 0:
                                k.op("dve", "tensor_tensor", out=Cre[o][:, fc, :], in0=ps[:, 128:256], in1=tc_.v, op=ALU.add)
                            else:
                                k.op("dve", "tensor_tensor", out=Cim[o][:, fc, :], in0=ps[:, 128:256], in1=tc_.v, op=ALU.subtract)

                        fwd_dft(f_tm, 256, sink_f)
                hx = [k.sb("hx%d" % i, [128, T]) for i in range(3)]
                Xc, Xs, tA, tB = (k.sb(nm, [128, NFC, 128]) for nm in ("hXc", "hXs", "htA", "htB"))
                cv = k.sb("hcv", [128, T])
                zt = cv
                u_tm = cv.v.re("p (b c) -> p b c", c=128)
                for (s0, T_) in seqs:
                    for part in range(3):
                        cix = part * 2 + chh
                        k.dma(zt, zT_d[z0 + part * 256 + chh * 128:z0 + part * 256 + (chh + 1) * 128, s0:s0 + T])
                        h = hx[part]
                        k.op("dve", "tensor_scalar", out=h.v, in0=zt.v, scalar1=hcT[:, 6 + cix:7 + cix], scalar2=None, op0=ALU.mult)
                        k.op("dve", "scalar_tensor_tensor", out=h[:, 1:T], in0=zt[:, 0:T - 1], scalar=hcT[:, cix:cix + 1],
                             in1=h[:, 1:T], op0=ALU.mult, op1=ALU.add)
                        k.op("dve", "scalar_tensor_tensor", out=h[:, 0:T - 1], in0=zt[:, 1:T], scalar=hcT[:, 12 + cix:13 + cix],
                             in1=h[:, 0:T - 1], op0=ALU.mult, op1=ALU.add)
                    cur = hx[0]
                    for o in range(2):
                        for blk in range(nblk):
                            ps = k.ps()
                            k.op("pe", "transpose", out=ps[:, 0:128], in_=cur[:, blk * 128:(blk + 1) * 128], identity=C["ident"].v)
                            k.op("act", "copy", out=u_tm[:, blk, :], in_=ps[:, 0:128])

                        def sink_x(part, fc, ps):
                            dst = Xc if part == 0 else Xs
                            if fc % 2:
                                k.op("act", "copy", out=dst[:, fc, :], in_=ps[:, 0:128])
                            else:
                                k.op("dve", "tensor_copy", out=dst[:, fc, :], in_=ps[:, 0:128])

                        fwd_dft(u_tm, 128, sink_x)
                        k.op("dve", "tensor_tensor", out=tA.v, in0=Xs.v, in1=Cim[o].v, op=ALU.mult)
                        k.op("pool", "tensor_tensor", out=tB.v, in0=Xs.v, in1=Cre[o].v, op=ALU.mult)
                        k.op("dve", "tensor_tensor", out=Xs.v, in0=Xc.v, in1=Cim[o].v, op=ALU.mult)
                        k.op("dve", "tensor_tensor", out=Xs.v, in0=Xs.v, in1=tB.v, op=ALU.subtract)
                        k.op("pool", "tensor_tensor", out=Xc.v, in0=Xc.v, in1=Cre[o].v, op=ALU.mult)
                        k.op("dve", "tensor_tensor", out=Xc.v, in0=Xc.v, in1=tA.v, op=ALU.add)
                        for tb in range(T // NB):
                            ps = k.ps()
                            n = 0
                            for fc in range(NFC):
                                for part in range(2):
                                    G = grot.next()
                                    k.dma(G, GCS[part, fc][:, tb * NB:(tb + 1) * NB])
                                    k.op("pe", "matmul", out=ps[:, 0:NB], lhsT=(Xc if part == 0 else Xs)[:, fc, :], rhs=G.v,
                                         start=(n == 0), stop=(n == 2 * NFC - 1))
                                    n += 1
                            k.op("dve", "scalar_tensor_tensor", out=cv[:, tb * NB:(tb + 1) * NB], in0=cur[:, tb * NB:(tb + 1) * NB],
                                 scalar=skT[:, o * 2 + chh:o * 2 + chh + 1], in1=ps[:, 0:NB], op0=ALU.mult, op1=ALU.add)
                        k.op("dve", "tensor_tensor", out=hx[o + 1].v, in0=hx[o + 1].v, in1=cv.v, op=ALU.mult)
                        cur = hx[o + 1]
                    k.dma(yb_d[256 + chh * 128:256 + (chh + 1) * 128, s0:s0 + T], hx[2].v)


def stage_rw(k, E, C, g, l, zT_d, yb_d):
    TG, seqs = g["TG"], g["seqs"]
    is_s = g["name"] == "s"
    T = seqs[0][1]
    CH = 64
    nch = T // CH
    G = min(8, nch)
    BW_ = G * CH
    nbat = nch // G
    NB = min(512, T)
    with k.scope():
        muT = load_T(k, C, E["rw_mu"][l].re("w (c p) -> (w c) p", p=128), 14, "muT")
        mid = k.sb("rwmid", [128, 7])
        k.op("dve", "tensor_tensor", out=mid.v, in0=muT[:, 0:7], in1=muT[:, 7:14], op=ALU.add)
        k.op("dve", "tensor_scalar", out=mid.v, in0=mid.v, scalar1=-1.0, scalar2=1.0, op0=ALU.mult, op1=ALU.add)
        w0T = load_T(k, C, E["rw_w0"][l].re("d (c p) -> (d c) p", p=128), 4, "w0T")
        a0T = load_T(k, C, E["rw_a0"][l:l + 1, :].re("o (c p) -> (o c) p", p=128), 2, "a0T")
        kvT = load_T(k, C, E["rw_kvec"][l].re("j (c p) -> (j c) p", p=128), 6, "kvT")
        lnT = load_T(k, C, E["rw_ln"][l].re("j (c p) -> (j c) p", p=128), 4, "lnT")
        omka = k.sb("omka", [128, 2])
        k.op("dve", "tensor_scalar", out=omka.v, in0=kvT[:, 2:4], scalar1=-1.0, scalar2=1.0, op0=ALU.mult, op1=ALU.add)
        lora = k.sb("lora", [128, 4, 256])
        k.dma(lora[0:32, 0, :], E["rw_w_up"][l, 0]); k.dma(lora[0:32, 1, :], E["rw_w_up"][l, 1])
        k.dma(lora[32:64, 2, :], E["rw_a_up"][l]); k.dma(lora[64:128, 3, :], E["rw_g_up"][l])
        cm = zt_alias = None
        MK = {}
        for nm in ("m_lt", "m_gt", "m_le", "m_ge", "eye8"):
            MK[nm] = k.sb(nm, [64, 64]); k.dma(MK[nm], E[nm])
        mb = lambda t_: t_.v.re("p (o c) -> p o c", o=1).bc([64, G, 64])
        p3v = lambda ps_: ps_[0:64, 0:G * 64].re("p (g c) -> p g c", g=G)
        MSK = [(MK["m_lt"], MK["m_le"], MK["m_gt"]), (MK["m_gt"], MK["m_ge"], MK["m_lt"])]
        zt = k.sb("rzt", [128, T])
        r_, kmod, v_, kkn, alpha, x6 = (k.sb(nm, [128, T]) for nm in ("rr", "rkmod", "rv", "rkkn", "ralpha", "rx6"))
        logw, lpi, ysum = (k.sb(nm, [128, T]) for nm in ("rlogw", "rlpi", "rysum"))
        tot = k.sb("rtot", [128, nch]); pC = k.sb("rpC", [128, nch])
        bt = {nm: k.sb("rb_" + nm, [128, BW_]) for nm in ("rt", "at", "kt", "kp", "e")}
        t64 = {nm: k.sb("r64_" + nm, [128 if nm == "v" else 64, G, 128]) for nm in ("v", "a", "k")}
        PP = {nm: k.sb("rP_" + nm, [128 if nm in ("BT", "RAT", "RKT") else 64, G, 64])
              for nm in ("Y0", "Y1", "YT0", "YT1", "P", "BT", "RAT", "RKT")}
        for t_ in (t64["v"], PP["BT"], PP["RAT"], PP["RKT"]):
            k.op("pool", "memset", ap=t_.v, constant=0.0)
        Mst = [[k.sb("rM%d%d" % (hh, i), [128, 128]) for i in range(2)] for hh in range(2)]
        Upad = [k.sb("rUp%d" % hh, [128, 128]) for hh in range(2)]
        for hh in range(2):
            k.op("pool", "memset", ap=Upad[hh].v, constant=0.0)
        g1r = Rot([k.sb("rg1", [64, 64]) for _ in range(2)])
        u_r = Rot([k.sb("ru", [64, 64]) for _ in range(2)])
        tmr = Rot([k.sb("rtm", [128, 64]) for _ in range(2)])
        s0pad = k.sb("rs0", [64, 128])
        sor = Rot([k.sb("rso", [64, 64]) for _ in range(2)])
        assert BW_ == NB
        t5 = Rot([bt["rt"], bt["at"], bt["kt"], bt["kp"]])
        o_rw = E["o_rw"].re("b (l d h v k) -> b l d h v k", l=L, d=2, h=4, v=64)
        c3 = lambda t_: t_.v.re("p (c s) -> p c s", s=CH)

        def shift(dst, row0, cix):
            k.dma(zt, zT_d[O_RW + row0:O_RW + row0 + 128, s0:s0 + T])
            k.op("dve", "tensor_scalar", out=dst.v, in0=zt.v, scalar1=mid[:, cix:cix + 1], scalar2=None, op0=ALU.mult)
            k.op("dve", "scalar_tensor_tensor", out=dst[:, 1:T], in0=zt[:, 0:T - 1], scalar=muT[:, cix:cix + 1],
                 in1=dst[:, 1:T], op0=ALU.mult, op1=ALU.add)
            k.op("dve", "scalar_tensor_tensor", out=dst[:, 0:T - 1], in0=zt[:, 1:T], scalar=muT[:, 7 + cix:8 + cix],
                 in1=dst[:, 0:T - 1], op0=ALU.mult, op1=ALU.add)

        for sj, (s0, T_) in enumerate(seqs):
            shift(x6, 768, 6)
            k.op("act", "activation", out=x6[0:32, :], in_=x6[0:32, :], func=AF.Sigmoid, scale=2.0)
            k.op("dve", "tensor_scalar", out=x6[0:32, :], in0=x6[0:32, :], scalar1=2.0, scalar2=-1.0, op0=ALU.mult, op1=ALU.add)
            k.op("act", "activation", out=x6[64:128, :], in_=x6[64:128, :], func=AF.Sigmoid)
            for hp in range(2):
                shift(r_, hp * 128, hp)
                shift(kmod, 256 + hp * 128, 2 + hp)
                shift(v_, 512 + hp * 128, 4 + hp)
                for n0 in range(0, T, NB):
                    ps = k.ps()
                    k.op("pe", "matmul", out=ps[:, 0:NB], lhsT=lora[32:64, 2, hp * 128:(hp + 1) * 128],
                         rhs=x6[32:64, n0:n0 + NB], start=True, stop=True)
                    k.op("act", "activation", out=alpha[:, n0:n0 + NB], in_=ps[:, 0:NB], func=AF.Sigmoid,
                         bias=a0T[:, hp:hp + 1], scale=1.0)
                k.op("dve", "tensor_scalar", out=kkn.v, in0=kmod.v, scalar1=kvT[:, 0 + hp:1 + hp], scalar2=None, op0=ALU.mult)
                for n0 in range(0, T, NB):
                    for hh in range(2):
                        sl = slice(hh * 64, hh * 64 + 64)
                        sq, rs = t5.next(), t5.next()
                        k.op("act", "activation", out=sq[sl, :], in_=kkn[sl, n0:n0 + NB], func=AF.Square)
                        pm = k.ps()
                        k.op("pe", "matmul", out=pm[:, 0:NB], lhsT=C["ones"][sl, :], rhs=sq[sl, :], start=True, stop=True)
                        k.op("dve", "tensor_scalar", out=rs[sl, :], in0=pm[sl, 0:NB], scalar1=1e-24, scalar2=None, op0=ALU.max)
                        k.op("act", "activation", out=rs[sl, :], in_=rs[sl, :], func=AF.Ln)
                        k.op("act", "activation", out=rs[sl, :], in_=rs[sl, :], func=AF.Exp, scale=-0.5)
                        k.op("dve", "tensor_tensor", out=kkn[sl, n0:n0 + NB], in0=kkn[sl, n0:n0 + NB], in1=rs[sl, :], op=ALU.mult)
                k.op("dve", "tensor_scalar", out=zt.v, in0=alpha.v, scalar1=kvT[:, 2 + hp:3 + hp], scalar2=omka[:, hp:hp + 1],
                     op0=ALU.mult, op1=ALU.add)
                k.op("dve", "tensor_tensor", out=kmod.v, in0=kmod.v, in1=zt.v, op=ALU.mult)
                k.op("dve", "tensor_tensor", out=alpha.v, in0=alpha.v, in1=kkn.v, op=ALU.mult)
                for d in range(2):
                    if RW_STOP[0] <= 1:
                        break
                    mT_s, mT_i, m_s = MSK[d]
                    for n0 in range(0, T, NB):
                        ps = k.ps()
                        k.op("pe", "matmul", out=ps[:, 0:NB], lhsT=lora[0:32, d, hp * 128:(hp + 1) * 128],
                             rhs=x6[0:32, n0:n0 + NB], start=True, stop=True)
                        k.op("act", "activation", out=logw[:, n0:n0 + NB], in_=ps[:, 0:NB], func=AF.Sigmoid,
                             bias=w0T[:, d * 2 + hp:d * 2 + hp + 1], scale=1.0)
                    k.op("dve", "tensor_scalar", out=logw.v, in0=logw.v, scalar1=-0.6065306597126334, scalar2=None, op0=ALU.mult)
                    k.dma(zt, E["cmask64"][:, 0:T])
                    k.op("dve", "tensor_tensor_scan", out=lpi.v, data0=zt.v, data1=logw.v, initial=0.0, op0=ALU.mult, op1=ALU.add)
                    k.op("dve", "tensor_copy", out=tot.v, in_=c3(lpi)[:, :, CH - 1])
                    if d == 1:
                        k.op("dve", "tensor_tensor", out=c3(lpi), in0=tot.v.re("p (c o) -> p c o", o=1).bc([128, nch, CH]),
                             in1=c3(lpi), op=ALU.subtract)
                        k.op("dve", "tensor_tensor", out=lpi.v, in0=lpi.v, in1=logw.v, op=ALU.add)
                    k.op("act", "activation", out=pC.v, in_=tot.v, func=AF.Exp)
                    cur = [0, 0]
                    for hh in range(2):
                        P0 = hh * 64
                        for i in range(2):
                            k.op("pool", "memset", ap=Mst[hh][i].v, constant=0.0)
                        if is_s:
                            k.op("pool", "memset", ap=s0pad.v, constant=0.0)
                            k.dma(s0pad[:, P0:P0 + 64], E["st_rw"][l, d, hp * 2 + hh])
                            ps = k.ps()
                            k.op("pe", "matmul", out=ps[:, 0:64], lhsT=s0pad.v, rhs=C["ident"][0:64, 0:64], start=True, stop=True)
                            k.op("dve", "tensor_copy", out=Mst[hh][0][P0:P0 + 64, P0:P0 + 64], in_=ps[P0:P0 + 64, 0:64])
                    bats = range(nbat) if d == 0 else range(nbat - 1, -1, -1)
                    if RW_STOP[0] <= 2:
                        bats = []
                    for b in bats:
                        bc_ = slice(b * BW_, (b + 1) * BW_)
                        e = bt["e"]
                        k.op("act", "activation", out=e.v, in_=lpi[:, bc_], func=AF.Exp)
                        k.op("dve", "tensor_tensor", out=bt["rt"].v, in0=r_[:, bc_], in1=e.v, op=ALU.mult)
                        k.op("act", "activation", out=e.v, in_=lpi[:, bc_], func=AF.Exp, scale=-1.0)
                        k.op("dve", "tensor_tensor", out=bt["at"].v, in0=alpha[:, bc_], in1=e.v, op=ALU.mult)
                        k.op("dve", "tensor_tensor", out=bt["kt"].v, in0=kmod[:, bc_], in1=e.v, op=ALU.mult)
                        k.op("dve", "tensor_tensor", out=e.v, in0=lpi[:, bc_], in1=logw[:, bc_], op=ALU.subtract)
                        k.op("act", "activation", out=e.v, in_=e.v, func=AF.Exp)
                        k.op("dve", "tensor_tensor", out=bt["kp"].v, in0=kkn[:, bc_], in1=e.v, op=ALU.mult)
                        for nm, src, off in (("v", v_, b * BW_), ("a", bt["at"], 0), ("k", bt["kt"], 0)):
                            for q0 in range(0, G, 4):
                                ps = k.ps()
                                for qq in range(min(4, G - q0)):
                                    gq = q0 + qq
                                    k.op("pe", "transpose", out=ps[0:64, qq * 128:(qq + 1) * 128],
                                         in_=src[:, off + gq * CH:off + (gq + 1) * CH], identity=C["ident"].v)
                                nq = min(4, G - q0)
                                k.op("act", "copy", out=t64[nm][0:64, q0:q0 + nq, :],
                                     in_=ps[0:64, 0:nq * 128].re("p (q c) -> p q c", q=nq))
                        for hh in range(2):
                            if RW_STOP[0] <= 3:
                                break
                            P0 = hh * 64
                            sl = slice(P0, P0 + 64)
                            GW = G * 64
                            banks = {nm: k.ps() for nm in ("AT", "RAT", "BT", "RKT", "A")}
                            for gq in range(G):
                                cc = slice(gq * CH, (gq + 1) * CH)
                                oc = slice(gq * 64, (gq + 1) * 64)
                                k.op("pe", "matmul", out=banks["AT"][0:64, oc], lhsT=bt["at"][sl, cc], rhs=bt["kp"][sl, cc], start=True, stop=True)
                                k.op("pe", "matmul", out=banks["RAT"][0:64, oc], lhsT=bt["at"][sl, cc], rhs=bt["rt"][sl, cc], start=True, stop=True)
                                k.op("pe", "matmul", out=banks["BT"][0:64, oc], lhsT=bt["kt"][sl, cc], rhs=bt["kp"][sl, cc], start=True, stop=True)
                                k.op("pe", "matmul", out=banks["RKT"][0:64, oc], lhsT=bt["kt"][sl, cc], rhs=bt["rt"][sl, cc], start=True, stop=True)
                                k.op("pe", "matmul", out=banks["A"][0:64, oc], lhsT=bt["kp"][sl, cc], rhs=bt["at"][sl, cc], start=True, stop=True)
                            f2 = lambda t_: t_.v.re("p g c -> p (g c)")
                            Y, YT, Yn, YTn, P = PP["Y0"], PP["YT0"], PP["Y1"], PP["YT1"], PP["P"]
                            k.op("dve", "tensor_tensor", out=Y.v, in0=p3v(banks["AT"]), in1=mb(mT_s), op=ALU.mult)
                            k.op("dve", "tensor_tensor", out=YT.v, in0=p3v(banks["A"]), in1=mb(m_s), op=ALU.mult)
                            k.op("dve", "tensor_tensor", out=PP["RAT"][0:64], in0=p3v(banks["RAT"]), in1=mb(mT_i), op=ALU.mult)
                            k.op("dve", "tensor_tensor", out=PP["BT"][0:64], in0=p3v(banks["BT"]), in1=mb(mT_s), op=ALU.mult)
                            k.op("dve", "tensor_tensor", out=PP["RKT"][0:64], in0=p3v(banks["RKT"]), in1=mb(mT_i), op=ALU.mult)
                            k.op("dve", "tensor_tensor", out=P.v, in0=mb(MK["eye8"]), in1=Y.v, op=ALU.subtract)
                            for lev in range(6 - 1):
                                p1, p2 = k.ps(), k.ps()
                                for gq in range(G):
                                    oc = slice(gq * 64, (gq + 1) * 64)
                                    k.op("pe", "matmul", out=p1[0:64, oc], lhsT=YT[:, gq, :], rhs=Y[:, gq, :], start=True, stop=True)
                                    k.op("pe", "matmul", out=p2[0:64, oc], lhsT=Y[:, gq, :], rhs=YT[:, gq, :], start=True, stop=True)
                                k.op("act", "copy", out=f2(Yn), in_=p1[0:64, 0:GW])
                                k.op("dve", "tensor_copy", out=f2(YTn), in_=p2[0:64, 0:GW])
                                p3 = k.ps()
                                for gq in range(G):
                                    oc = slice(gq * 64, (gq + 1) * 64)
                                    k.op("pe", "matmul", out=p3[0:64, oc], lhsT=YTn[:, gq, :], rhs=P[:, gq, :], start=True, stop=True)
                                k.op("dve", "tensor_tensor", out=f2(P), in0=f2(P), in1=p3[0:64, 0:GW], op=ALU.add)
                                Y, YT, Yn, YTn = Yn, YTn, Y, YT
                            chs = range(G) if d == 0 else range(G - 1, -1, -1)
                            if RW_STOP[0] <= 4:
                                chs = []
                            for gq in chs:
                                ci = b * G + gq
                                cc = slice(gq * CH, (gq + 1) * CH)
                                gcol = slice(b * BW_ + gq * CH, b * BW_ + (gq + 1) * CH)
                                Mc, Mn = Mst[hh][cur[hh]], Mst[hh][1 - cur[hh]]
                                kr = slice(0, 64) if hh == 0 else slice(0, 128)
                                ps1 = k.ps()
                                k.op("pe", "matmul", out=ps1[0:64, 0:64], lhsT=bt["kp"][sl, cc], rhs=Mc[sl, P0:P0 + 64], start=True, stop=False)
                                k.op("pe", "matmul", out=ps1[0:64, 0:64], lhsT=PP["BT"][kr, gq, :], rhs=t64["v"][kr, gq, P0:P0 + 64],
                                     start=False, stop=True)
                                g1 = g1r.next()
                                k.op("act", "mul", out=g1.v, in_=ps1[0:64, 0:64], mul=-1.0)
                                if RW_STOP[0] <= 5:
                                    continue
                                ps2 = k.ps()
                                lh = P
                                if RW_VAR[0] == 5:
                                    lh = PP["RKT"]
                                if RW_VAR[0] == 6:
                                    k.op("dve", "tensor_copy", out=PP["Y0"].v, in_=P.v)
                                    lh = PP["Y0"]
                                k.op("pe", "matmul", out=(ps2[0:64, 64:128] if RW_VAR[0] == 7 else ps2[0:64, 0:64]), lhsT=lh[:, gq, :],
                                     rhs=(PP["RAT"][:, gq, :] if RW_VAR[0] == 3 else g1.v), start=True, stop=True)
                                if RW_VAR[0] == 4:
                                    continue
                                u = u_r.next()
                                if RW_VAR[0] == 2:
                                    k.op("dve", "tensor_copy", out=u.v, in_=ps2[0:64, 0:64])
                                else:
                                    k.op("act", "copy", out=u.v, in_=ps2[0:64, 0:64])
                                if RW_VAR[0] != 1:
                                    k.op("dve", "tensor_copy", out=Upad[hh][0:64, P0:P0 + 64], in_=u.v)
                                if RW_STOP[0] <= 6:
                                    continue
                                psy = k.ps()
                                k.op("pe", "matmul", out=psy[:, 0:64], lhsT=Mc[sl, :], rhs=bt["rt"][sl, cc], start=True, stop=False)
                                k.op("pe", "matmul", out=psy[:, 0:64], lhsT=Upad[hh][kr, :], rhs=PP["RAT"][kr, gq, :], start=False, stop=False)
                                k.op("pe", "matmul", out=psy[:, 0:64], lhsT=t64["v"][kr, gq, :], rhs=PP["RKT"][kr, gq, :], start=False, stop=True)
                                if d == 0:
                                    k.op("act", "copy", out=ysum[sl, gcol], in_=psy[sl, 0:64])
                                else:
                                    k.op("dve", "tensor_tensor", out=ysum[sl, gcol], in0=ysum[sl, gcol], in1=psy[sl, 0:64], op=ALU.add)
                                if RW_STOP[0] <= 7:
                                    continue
                                psm = k.ps()
                                k.op("pe", "matmul", out=psm[:, 0:64], lhsT=t64["a"][:, gq, :], rhs=u.v, start=True, stop=False)
                                k.op("pe", "matmul", out=psm[:, 0:64], lhsT=t64["k"][:, gq, :], rhs=t64["v"][0:64, gq, P0:P0 + 64],
                                     start=False, stop=True)
                                tm = tmr.next()
                                k.op("dve", "tensor_tensor", out=tm[sl, :], in0=psm[sl, 0:64], in1=Mc[sl, P0:P0 + 64], op=ALU.add)
                                k.op("dve", "tensor_scalar", out=Mn[sl, P0:P0 + 64], in0=tm[sl, :], scalar1=pC[sl, ci:ci + 1],
                                     scalar2=None, op0=ALU.mult)
                                cur[hh] = 1 - cur[hh]
                    if not is_s:
                        for hh in range(2):
                            P0 = hh * 64
                            sl = slice(P0, P0 + 64)
                            ps = k.ps()
                            k.op("pe", "matmul", out=ps[0:64, 0:64], lhsT=Mst[hh][cur[hh]][sl, P0:P0 + 64], rhs=C["ident"][sl, sl],
                                 start=True, stop=True)
                            so = sor.next()
                            k.op("act", "copy", out=so.v, in_=ps[0:64, 0:64])
                            k.dma(o_rw[sj, l, d, hp * 2 + hh], so.v)
                for n0 in range(0, T, NB):
                    cs = slice(n0, n0 + NB)
                    pg = k.ps()
                    k.op("pe", "matmul", out=pg[:, 0:NB], lhsT=lora[64:128, 3, hp * 128:(hp + 1) * 128], rhs=x6[64:128, cs],
                         start=True, stop=True)
                    for hh in range(2):
                        sl = slice(hh * 64, hh * 64 + 64)
                        cen, sq, rs, bo = (t5.next() for _ in range(4))
                        pm = k.ps()
                        k.op("pe", "matmul", out=pm[:, 0:NB], lhsT=C["ones"][sl, :], rhs=ysum[sl, cs], start=True, stop=True)
                        k.op("dve", "scalar_tensor_tensor", out=cen[sl, :], in0=pm[sl, 0:NB], scalar=-1.0 / 64, in1=ysum[sl, cs],
                             op0=ALU.mult, op1=ALU.add)
                        k.op("act", "activation", out=sq[sl, :], in_=cen[sl, :], func=AF.Square)
                        pv = k.ps()
                        k.op("pe", "matmul", out=pv[:, 0:NB], lhsT=C["ones"][sl, :], rhs=sq[sl, :], start=True, stop=True)
                        k.op("dve", "tensor_scalar", out=rs[sl, :], in0=pv[sl, 0:NB], scalar1=1.0 / 64, scalar2=64e-5,
                             op0=ALU.mult, op1=ALU.add)
                        k.op("act", "activation", out=rs[sl, :], in_=rs[sl, :], func=AF.Ln)
                        k.op("act", "activation", out=rs[sl, :], in_=rs[sl, :], func=AF.Exp, scale=-0.5)
                        k.op("dve", "tensor_tensor", out=cen[sl, :], in0=cen[sl, :], in1=rs[sl, :], op=ALU.mult)
                        k.op("dve", "tensor_scalar", out=cen[sl, :], in0=cen[sl, :], scalar1=lnT[sl, hp:hp + 1],
                             scalar2=lnT[sl, 2 + hp:3 + hp], op0=ALU.mult, op1=ALU.add)
                        k.op("dve", "scalar_tensor_tensor", out=bo[sl, :], in0=r_[sl, cs], scalar=kvT[sl, 4 + hp:5 + hp],
                             in1=kmod[sl, cs], op0=ALU.mult, op1=ALU.mult)
                        pb = k.ps()
                        k.op("pe", "matmul", out=pb[:, 0:NB], lhsT=C["ones"][sl, :], rhs=bo[sl, :], start=True, stop=True)
                        k.op("dve", "tensor_tensor", out=bo[sl, :], in0=pb[sl, 0:NB], in1=v_[sl, cs], op=ALU.mult)
                        k.op("dve", "tensor_tensor", out=cen[sl, :], in0=cen[sl, :], in1=bo[sl, :], op=ALU.add)
                        k.op("dve", "tensor_tensor", out=zt[sl, cs], in0=cen[sl, :], in1=pg[sl, 0:NB], op=ALU.mult)
                k.dma(yb_d[hp * 128:(hp + 1) * 128, s0:s0 + T], zt[:, 0:T])


def stage_merge(k, E, C, g, l, x, mod_d, zT_d, yb_d):
    NT, TG = g["NT"], g["TG"]
    TB = 256
    with k.scope():
        g1b = bc_param(k, "g1b", mod_d[l, g["cond"]:g["cond"] + 1, 2 * D:3 * D])
        brw = k.sb("brw", [128, 8, D])
        k.dma(brw, E["br_w"][l].re("b (kc p) n -> p (b kc) n", p=128))
        wo = k.sb("wo", [128, 8, D])
        k.dma(wo, E["w_out"][l].re("(kc p) n -> p kc n", p=128))
        ybr = Rot([k.sb("ybb", [128, 8, TB]) for _ in range(2)])
        mTr = Rot([k.sb("mT", [128, 8, TB]) for _ in range(2)])
        gtr = Rot([k.sb("gt", [128, TB]) for _ in range(3)])
        tmr = Rot([k.sb("tm", [128, TB]) for _ in range(2)])
        tor = Rot([k.sb("to", [128, 512]) for _ in range(2)])
        for t0 in range(0, TG, TB):
            yb = ybr.next()
            k.dma(yb, yb_d[:, t0:t0 + TB].re("(c p) t -> p c t", p=128))
            mT = mTr.next()
            for nch in range(8):
                for b in range(4):
                    gt = gtr.next()
                    r0 = O_MG + b * D + nch * 128
                    k.dma(gt, zT_d[r0:r0 + 128, t0:t0 + TB])
                    ps = k.ps()
                    for kc2 in range(2):
                        k.op("pe", "matmul", out=ps[:, 0:TB], lhsT=brw[:, 2 * b + kc2, nch * 128:(nch + 1) * 128],
                             rhs=yb[:, 2 * b + kc2, :], start=(kc2 == 0), stop=(kc2 == 1))
                    if b == 0:
                        k.op("dve", "tensor_tensor", out=mT[:, nch, :], in0=ps[:, 0:TB], in1=gt.v, op=ALU.mult)
                    else:
                        tm = tmr.next()
                        k.op("dve", "tensor_tensor", out=tm.v, in0=ps[:, 0:TB], in1=gt.v, op=ALU.mult)
                        k.op("pool", "tensor_tensor", out=mT[:, nch, :], in0=mT[:, nch, :], in1=tm.v, op=ALU.add)
            for ts in range(TB // 128):
                i = (t0 // 128) + ts
                for eh in range(2):
                    ps = k.ps()
                    for nch in range(8):
                        k.op("pe", "matmul", out=ps.v, lhsT=mT[:, nch, ts * 128:(ts + 1) * 128],
                             rhs=wo[:, nch, eh * 512:(eh + 1) * 512], start=(nch == 0), stop=(nch == 7))
                    to = tor.next()
                    k.op("dve", "tensor_tensor", out=to.v, in0=ps.v, in1=g1b[:, eh * 512:(eh + 1) * 512], op=ALU.mult)
                    k.op("pool", "tensor_tensor", out=x[:, i, eh * 512:(eh + 1) * 512],
                         in0=x[:, i, eh * 512:(eh + 1) * 512], in1=to.v, op=ALU.add)


def stage_moe(k, E, C, g, l, x, mod_d):
    NT, TG = g["NT"], g["TG"]
    seqs = g["seqs"]
    caps = [2 * T // NE for (_, T) in seqs]
    NS = sum(caps)
    slot0 = [sum(caps[:j]) for j in range(len(seqs))]
    with k.scope():
        h2bf = k.sb("h2bf", [128, NT, D], BF16)
        aff = k.sb("aff", [128, NT, NE])
        rsel = k.sb("rsel", [128, NT, NE])
        g2b = bc_param(k, "g2b", mod_d[l, g["cond"]:g["cond"] + 1, 5 * D:6 * D])
        with k.scope():
            A, B = mod_params(k, E, mod_d, l, g["cond"], 1, "norm2_g")
            nm = NormMod(k)
            rt = k.sb("rt", [128, 8, NE])
            k.dma(rt, E["router"][l].re("(kc p) e -> p kc e", p=128))
            htm = Rot([k.sb("htm", [128, D]) for _ in range(2)])
            hTt = Rot([k.sb("hTt", [128, 8, 128]) for _ in range(2)])
            sm = Rot([k.sb("sm", [128, 4]) for _ in range(2)])
            affT = k.sb("affT", [NE, TG])
            for i in range(NT):
                h = htm.next()
                nm(x[:, i, :], A, B, h.v)
                k.op("pool", "tensor_copy", out=h2bf[:, i, :], in_=h.v, free=True)
                hT = hTt.next()
                to_fm(k, C, h, hT, 0, free=False)
                ps = k.ps()
                for kc in range(8):
                    k.op("pe", "matmul", out=ps[:, 0:NE], lhsT=hT[:, kc, :], rhs=rt[:, kc, :],
                         start=(kc == 0), stop=(kc == 7))
                s = sm.next()
                k.op("dve", "reduce_max", out=s[:, 0:1], in_=ps[:, 0:NE], axis=AX.X)
                k.op("dve", "tensor_scalar", out=s[:, 1:2], in0=s[:, 0:1], scalar1=-1.0, scalar2=None, op0=ALU.mult)
                k.op("act", "activation", out=aff[:, i, :], in_=ps[:, 0:NE], func=AF.Exp, bias=s[:, 1:2], scale=1.0,
                     accum_out=s[:, 2:3])
                k.op("dve", "reciprocal", out=s[:, 3:4], in_=s[:, 2:3])
                k.op("dve", "tensor_scalar", out=aff[:, i, :], in0=aff[:, i, :], scalar1=s[:, 3:4], scalar2=None,
                     op0=ALU.mult)
                ps2 = k.ps()
                k.op("pe", "transpose", out=ps2[0:NE, 0:128], in_=aff[:, i, :], identity=C["ident"].v)
                k.op("act", "copy", out=affT[:, i * 128:(i + 1) * 128], in_=ps2[0:NE, 0:128])
            work = k.sb("work", [NE, TG])
            mx = Rot([k.sb("mx", [NE, 8]) for _ in range(2)])
            k.op("dve", "tensor_copy", out=work.v, in_=affT.v)
            for j, (s0, T) in enumerate(seqs):
                for r in range(caps[j] // 8):
                    m8 = mx.next()
                    k.op("dve", "max", out=m8.v, in_=work[:, s0:s0 + T])
                    k.op("dve", "match_replace", out=work[:, s0:s0 + T], in_to_replace=m8.v,
                         in_values=work[:, s0:s0 + T], imm_value=-1.0)
            maskT = k.sb("maskT", [NE, TG])
            k.op("dve", "tensor_scalar", out=maskT.v, in0=work.v, scalar1=-1.0, scalar2=None, op0=ALU.is_equal)
            mtm = k.sb("mtm", [128, NT, NE])
            for i in range(NT):
                ps = k.ps()
                k.op("pe", "transpose", out=ps[:, 0:NE], in_=maskT[:, i * 128:(i + 1) * 128],
                     identity=C["ident"][0:NE, 0:NE])
                k.op("act", "copy", out=mtm[:, i, :], in_=ps[:, 0:NE])
            for j, (s0, T) in enumerate(seqs):
                i0, n = s0 // 128, T // 128
                for ii in range(n):
                    ps = k.ps()
                    for jj in range(ii + 1):
                        k.op("pe", "matmul", out=ps[:, 0:NE], lhsT=(C["triu"] if jj == ii else C["ones"]).v,
                             rhs=mtm[:, i0 + jj, :], start=(jj == 0), stop=(jj == ii))
                    k.op("dve", "scalar_tensor_tensor", out=rsel[:, i0 + ii, :], in0=ps[:, 0:NE], scalar=float(slot0[j]),
                         in1=mtm[:, i0 + ii, :], op0=ALU.add, op1=ALU.mult)
                    k.op("dve", "tensor_scalar", out=rsel[:, i0 + ii, :], in0=rsel[:, i0 + ii, :], scalar1=-1.0,
                         scalar2=None, op0=ALU.add)
        if False:
            with k.scope():
                dt_ = k.sb("dbgt", [128, 2048])
                k.op("dve", "tensor_copy", out=dt_[:, 0:1024], in_=h2bf[:, 0, :])
                k.op("dve", "tensor_copy", out=dt_[:, 1024:2048], in_=h2bf[:, 1, :])
                k.dma(E["dbg"][:, 0:2048], dt_.v)
                k.dma(E["dbg"][:, 2048:2048 + NT * NE], aff.v.re("p n e -> p (n e)"))
                k.dma(E["dbg"][:, 4096:4096 + NT * NE], rsel.v.re("p n e -> p (n e)"))
        with k.scope():
            w1r = Rot([k.sb("w1", [128, 8, 128]) for _ in range(2)])
            w3r = Rot([k.sb("w3", [128, 8, 128]) for _ in range(2)])
            w2r = Rot([k.sb("w2", [128, D]) for _ in range(2)])
            xeT = k.sb("xeT", [128, 8, NS])
            xe = k.sb("xe", [128, (NS + 127) // 128, D])
            hidT = k.sb("hidT", [128, FF // 128, NS])
            NCC = (NS + 127) // 128
            ye = k.sb("ye", [128, NCC, D])
            selr = Rot([k.sb("sel", [128, 256], BF16) for _ in range(3)])
            sgr = Rot([k.sb("sg", [128, 256]) for _ in range(2)])
            sgTr = Rot([k.sb("sgT", [128, 2, 128]) for _ in range(2)])
            s1r = Rot([k.sb("s1", [128, 256]) for _ in range(2)])
            for e in range(NE):
                pss = [[k.ps() for _ in range(2)] for _ in range(NCC)]
                for i in range(NT):
                    sel = selr.next()
                    k.op("dve", "tensor_scalar", out=sel[:, 0:NS], in0=C["iota_f"][:, 0:NS],
                         scalar1=rsel[:, i, e:e + 1], scalar2=None, op0=ALU.is_equal)
                    for cc in range(NCC):
                        cw = min(128, NS - cc * 128)
                        for dh in range(2):
                            k.op("pe", "matmul", out=pss[cc][dh][0:cw, :], lhsT=sel[:, cc * 128:cc * 128 + cw],
                                 rhs=h2bf[:, i, dh * 512:(dh + 1) * 512], start=(i == 0), stop=(i == NT - 1))
                for cc in range(NCC):
                    cw = min(128, NS - cc * 128)
                    for dh in range(2):
                        if dh:
                            k.op("act", "copy", out=xe[0:cw, cc, dh * 512:(dh + 1) * 512], in_=pss[cc][dh][0:cw, :])
                        else:
                            k.op("dve", "tensor_copy", out=xe[0:cw, cc, dh * 512:(dh + 1) * 512], in_=pss[cc][dh][0:cw, :])
                for cc in range(NCC):
                    cw = min(128, NS - cc * 128)
                    for j2 in range(2):
                        pt = k.ps()
                        for a in range(4):
                            kc = 4 * j2 + a
                            k.op("pe", "transpose", out=pt[:, a * 128:a * 128 + cw], in_=xe[0:cw, cc, kc * 128:(kc + 1) * 128],
                                 identity=C["ident"][0:cw, 0:cw])
                        src = pt.v.re("p (a t) -> p a t", a=4)[:, :, 0:cw]
                        dst = xeT[:, 4 * j2:4 * j2 + 4, cc * 128:cc * 128 + cw]
                        if j2:
                            k.op("act", "copy", out=dst, in_=src)
                        else:
                            k.op("dve", "tensor_copy", out=dst, in_=src)
                for fc in range(FF // 128):
                    w1, w3 = w1r.next(), w3r.next()
                    k.dma(w1, E["ex_w1"][l, e][:, fc * 128:(fc + 1) * 128].re("(kc p) n -> p kc n", p=128))
                    k.dma(w3, E["ex_w3"][l, e][:, fc * 128:(fc + 1) * 128].re("(kc p) n -> p kc n", p=128))
                    p1, p3 = k.ps(), k.ps()
                    for kc in range(8):
                        k.op("pe", "matmul", out=p1[:, 0:NS], lhsT=w1[:, kc, :], rhs=xeT[:, kc, :],
                             start=(kc == 0), stop=(kc == 7))
                    for kc in range(8):
                        k.op("pe", "matmul", out=p3[:, 0:NS], lhsT=w3[:, kc, :], rhs=xeT[:, kc, :],
                             start=(kc == 0), stop=(kc == 7))
                    s1 = s1r.next()
                    k.op("act", "activation", out=s1[:, 0:NS], in_=p1[:, 0:NS], func=AF.Sigmoid)
                    k.op("dve", "tensor_tensor", out=s1[:, 0:NS], in0=s1[:, 0:NS], in1=p1[:, 0:NS], op=ALU.mult)
                    k.op("dve", "tensor_tensor", out=hidT[:, fc, :], in0=p3[:, 0:NS], in1=s1[:, 0:NS], op=ALU.mult)
                pacc = [[k.ps() for _ in range(2)] for _ in range(NCC)]
                for fc in range(FF // 128):
                    w2 = w2r.next()
                    k.dma(w2, E["ex_w2"][l, e][fc * 128:(fc + 1) * 128, :])
                    for cc in range(NCC):
                        cw = min(128, NS - cc * 128)
                        for dh in range(2):
                            k.op("pe", "matmul", out=pacc[cc][dh][0:cw, :], lhsT=hidT[:, fc, cc * 128:cc * 128 + cw],
                                 rhs=w2[:, dh * 512:(dh + 1) * 512], start=(fc == 0), stop=(fc == FF // 128 - 1))
                for cc in range(NCC):
                    cw = min(128, NS - cc * 128)
                    for dh in range(2):
                        k.op("dve", "tensor_tensor", out=ye[0:cw, cc, dh * 512:(dh + 1) * 512],
                             in0=pacc[cc][dh][0:cw, :], in1=g2b[0:cw, dh * 512:(dh + 1) * 512], op=ALU.mult)
                for i in range(NT):
                    sg = sgr.next()
                    k.op("dve", "tensor_scalar", out=sg[:, 0:NS], in0=C["iota_f"][:, 0:NS],
                         scalar1=rsel[:, i, e:e + 1], scalar2=aff[:, i, e:e + 1], op0=ALU.is_equal, op1=ALU.mult)
                    sgT = sgTr.next()
                    pst = k.ps()
                    for cc in range(NCC):
                        k.op("pe", "transpose", out=pst[:, cc * 128:(cc + 1) * 128],
                             in_=sg[:, cc * 128:(cc + 1) * 128], identity=C["ident"].v)
                    k.op("act", "copy", out=sgT[:, 0:NCC, :], in_=pst[:, 0:NCC * 128].re("p (c t) -> p c t", c=NCC))
                    for dh in range(2):
                        pso = k.ps()
                        for cc in range(NCC):
                            k.op("pe", "matmul", out=pso.v, lhsT=sgT[:, cc, :],
                                 rhs=ye[:, cc, dh * 512:(dh + 1) * 512],
                                 start=(cc == 0), stop=(cc == NCC - 1))
                        k.op("dve", "tensor_tensor", out=x[:, i, dh * 512:(dh + 1) * 512],
                             in0=x[:, i, dh * 512:(dh + 1) * 512], in1=pso.v, op=ALU.add)


def final_norm(k, x, NT, fg_b, yv):
    nm = NormMod(k)
    op_ = Rot([k.sb("o", [128, D]) for _ in range(2)])
    for i in range(NT):
        o = op_.next()
        nm(x[:, i, :], fg_b, None, o.v)
        k.dma(yv[:, i, :], o.v)


_PROG = None
W_NAMES = ("ada_w", "ada_b", "norm1_g", "norm2_g", "w_in", "br_w", "w_out", "router", "ex_w1", "ex_w3", "ex_w2")


def kernel(**inp):
    global _PROG
    if _PROG is None:
        _PROG = build_program()
    nc = _PROG
    cn = consts_np()
    f32 = lambda a: np.ascontiguousarray(np.asarray(a, dtype=np.float32))
    shared = {n: f32(inp[n][:NLAYERS_RUN]) for n in W_NAMES if (ENABLE["moe"] or not n.startswith("ex_"))}
    shared["final_g"] = f32(inp["final_g"]).reshape(1, D)
    shared["c_ctx"] = f32(inp["c_ctx"]).reshape(1, D)
    shared["ret_rate"] = f32(inp["ret_rate"]).reshape(L, 8)
    shared["ret_gn"] = f32(inp["ret_gn"])
    shared["hg_lb"] = f32(inp["hg_lb"]); shared["hg_norm"] = f32(inp["hg_norm"])
    for nm in ("rw_mu", "rw_w0", "rw_w_up", "rw_a0", "rw_a_up", "rw_g_up", "rw_kvec", "rw_ln", "hy_conv", "hy_ffn1", "hy_ffn1_b", "hy_ffn2", "hy_ffn2_b", "hy_ffn3", "hy_freq", "hy_decay", "hy_skip"):
        shared[nm] = f32(inp[nm])
    shared.update(cn)
    in_maps = []
    for i in range(NC_RUN):
        m = dict(shared)
        m["xs"] = f32(inp["x_sample"][i])
        m["xp"] = f32(inp["x_prompt"][NPR * i:NPR * (i + 1)]).reshape(NPR * TP, D)
        m["c_s"] = f32(inp["c"][i]).reshape(1, D)
        m["st_ret"] = f32(inp["state_ret"][i])
        m["st_hg"] = f32(inp["state_hgrn"][i])
        m["st_rw"] = f32(inp["state_rwkv"][i])
        in_maps.append(m)
    res = run_bass_kernel_spmd(nc, in_maps, core_ids=list(range(NC_RUN)))
    R = res.results
    y_prompt = np.concatenate([R[i]["yp"].reshape(NPR, TP, D) for i in range(NC_RUN)], axis=0)
    y_sample = np.stack([R[i]["ys"] for i in range(NC_RUN)], axis=0)
    sts = []
    for nm in ("o_rw", "o_ret", "o_hg"):
        sts.append(np.concatenate([R[i][nm].reshape(NPR, L, 2, 4, 64, 64) for i in range(NC_RUN)], axis=0))
    return (y_prompt, y_sample, sts[0], sts[1], sts[2])
```

```python
import contextlib
import numpy as np
import concourse.bass as bass
import concourse.mybir as mybir

F32 = mybir.dt.float32
BF16 = mybir.dt.bfloat16
I32 = mybir.dt.int32
AF = mybir.ActivationFunctionType
ALU = mybir.AluOpType
AX = mybir.AxisListType

WRITE_KEYS = ("out", "accum_out", "ap")
N_DMA_SEMS = 96


class Eng:
    def __init__(self, name, h, sem):
        self.name, self.h, self.sem = name, h, sem
        self.cnt = 0
        self.seen = {}


class DSem:
    def __init__(self, sem):
        self.sem = sem
        self.cnt = 0


class Tile:
    def __init__(self, k, handle, space):
        self.k, self.h, self.space = k, handle, space
        self.w = {}
        self.r = {}
        self.dsem = None

    def _ap(self):
        return self.h.ap() if self.space == "dram" else self.h[:]

    @property
    def v(self):
        return View(self, self._ap())

    def __getitem__(self, idx):
        return View(self, self._ap()[idx])


class View:
    def __init__(self, tile, ap):
        self.tile, self.ap = tile, ap

    def __getitem__(self, idx):
        return View(self.tile, self.ap[idx])

    def re(self, pat, **kw):
        return View(self.tile, self.ap.rearrange(pat, **kw))

    def bc(self, shape):
        return View(self.tile, self.ap.to_broadcast(shape))

    @property
    def shape(self):
        return self.ap.shape


class Ext:
    def __init__(self, ap):
        self.ap = ap

    def __getitem__(self, idx):
        return Ext(self.ap[idx])

    def re(self, pat, **kw):
        return Ext(self.ap.rearrange(pat, **kw))

    def bc(self, shape):
        return Ext(self.ap.to_broadcast(shape))


class K:
    def __init__(self, nc):
        self.nc = nc
        self.root = contextlib.ExitStack()
        self.stacks = [self.root]
        self.scope_tiles = [[]]
        self.eng = {}
        for name, h in (("pe", nc.tensor), ("act", nc.scalar), ("dve", nc.vector),
                        ("pool", nc.gpsimd), ("sp", nc.sync)):
            sem = self.root.enter_context(nc.semaphore("sem_" + name))
            self.eng[name] = Eng(name, h, sem)
        self.dsems = [DSem(self.root.enter_context(nc.semaphore("dsem%d" % i))) for i in range(N_DMA_SEMS)]
        self.free_dsems = list(self.dsems)
        self.psums = []
        self.ps_i = 0
        self.n_ins = 0
        self.uid = 0

    def name(self, n):
        self.uid += 1
        return "%s_%d" % (n, self.uid)

    def sb(self, name, shape, dt=F32):
        h = self.stacks[-1].enter_context(self.nc.sbuf_tensor(self.name(name), list(shape), dt))
        t = Tile(self, h, "sb")
        self.scope_tiles[-1].append(t)
        return t

    def dram(self, name, shape, dt=F32):
        h = self.nc.dram_tensor(self.name(name), list(shape), dt, kind="Internal")
        t = Tile(self, h, "dram")
        self.scope_tiles[0].append(t)
        return t

    def init_psum(self, n=8):
        for i in range(n):
            h = self.root.enter_context(self.nc.psum_tensor("ps%d" % i, [128, 512], F32))
            self.psums.append(Tile(self, h, "ps"))

    def ps(self):
        while True:
            t = self.psums[self.ps_i % len(self.psums)]
            self.ps_i += 1
            if not getattr(t, "reserved", False):
                return t

    def ps_reserve(self, n):
        out = []
        for _ in range(n):
            t = self.ps()
            t.reserved = True
            out.append(t)
        return out

    def ps_release(self, tiles):
        for t in tiles:
            t.reserved = False

    @contextlib.contextmanager
    def scope(self):
        st = contextlib.ExitStack()
        self.stacks.append(st)
        self.scope_tiles.append([])
        try:
            with st:
                yield
                self.barrier()
                for t in self.scope_tiles[-1]:
                    if t.dsem is not None:
                        self.free_dsems.append(t.dsem)
                        t.dsem = None
        finally:
            self.stacks.pop()
            self.scope_tiles.pop()

    def _wait(self, e, deps):
        for d in deps:
            if d is None:
                continue
            semobj, val, owner = d
            if owner is e and e.name in ("pe", "sp"):
                continue
            key = id(semobj)
            if e.seen.get(key, 0) < val:
                e.h.wait_ge(semobj.sem, val)
                e.seen[key] = val

    def barrier(self):
        engs = list(self.eng.values())
        for e in engs:
            deps = [(x, x.cnt, x) for x in engs if x is not e and x.cnt > 0]
            deps += [(d, d.cnt, None) for d in self.dsems if d.cnt > 0]
            self._wait(e, deps)

    @staticmethod
    def _merge(d, tag):
        key = id(tag[0])
        if key not in d or d[key][1] < tag[1]:
            d[key] = tag

    def _deps(self, reads, writes, free):
        deps = []
        for t in reads:
            deps.extend(t.w.values())
        for t in writes:
            if not free:
                deps.extend(t.w.values())
                deps.extend(t.r.values())
        return deps

    def _record(self, reads, writes, tag, free):
        for t in writes:
            if free:
                self._merge(t.w, tag)
            else:
                t.w = {id(tag[0]): tag}
                t.r = {}
        for t in reads:
            if t not in writes:
                self._merge(t.r, tag)

    def op(self, engname, meth, R=(), W=(), free=False, **kw):
        e = self.eng[engname]
        reads, writes = list(R), list(W)
        args = {}
        for key, val in kw.items():
            if isinstance(val, Tile):
                val = val.v
            if isinstance(val, View):
                (writes if key in WRITE_KEYS else reads).append(val.tile)
                args[key] = val.ap
            elif isinstance(val, Ext):
                args[key] = val.ap
            else:
                args[key] = val
        self._wait(e, self._deps(reads, writes, free))
        ins = getattr(e.h, meth)(**args)
        e.cnt += 1
        ins.then_inc(e.sem, 1)
        self._record(reads, writes, (e, e.cnt, e), free)
        self.n_ins += 1
        return ins

    def dma(self, out, in_, q="sp", free=False, **kw):
        e = self.eng[q]
        if isinstance(out, Tile):
            out = out.v
        if isinstance(in_, Tile):
            in_ = in_.v
        reads = [in_.tile] if isinstance(in_, View) else []
        writes = [out.tile] if isinstance(out, View) else []
        tracked = None
        if reads:
            tracked = reads[0]
        if writes and (tracked is None or writes[0].space == "sb"):
            tracked = writes[0]
        if tracked is None:
            raise ValueError("dma needs a tracked side")
        self._wait(e, self._deps(reads, writes, free))
        if tracked.dsem is None:
            if not self.free_dsems:
                raise RuntimeError("out of dma sems")
            tracked.dsem = self.free_dsems.pop(0)
        ds = tracked.dsem
        ins = e.h.dma_start(out=out.ap, in_=in_.ap, **kw)
        ds.cnt += 16
        ins.then_inc(ds.sem, 16)
        self._record(reads, writes, (ds, ds.cnt, None), free)
        self.n_ins += 1
        return ins

    def finish(self):
        e = self.eng["sp"]
        deps = [(d, d.cnt, None) for d in self.dsems if d.cnt > 0]
        deps += [(x, x.cnt, x) for x in self.eng.values() if x is not e and x.cnt > 0]
        self._wait(e, deps)
        self.root.close()

from concourse.bass_utils import run_bass_kernel_spmd

D = 1024
L = 4
NLAYERS_RUN = 4
TS = 2048
TP = 256
NPR = 4
NCORES = 8
NC_RUN = 8
EPS = 1e-6
ST_ELEMS = L * 2 * 4 * 64 * 64
NE = 16
FF = 2048
N_IN = 8064
O_RW, O_HY, O_RET, O_HG, O_MG = 0, 896, 896 + 768, 896 + 768 + 1024, 896 + 768 + 1024 + 1280
ENABLE = dict(rw=True, hy=True, ret=True, hg=True, moe=True)
RW_STOP = [9]
PROJ_BF16 = True
RW_VAR = [0]
GROUPS_RUN = ['s', 'p']


def consts_np():
    c = {}
    c["ident"] = np.eye(128, dtype=np.float32)
    c["iota_f"] = np.tile(np.arange(256, dtype=np.float32)[None, :], (128, 1))
    c["triu"] = np.triu(np.ones((128, 128), dtype=np.float32))
    c["ones"] = np.ones((128, 128), dtype=np.float32)
    up = np.arange(2 * TS - 128, dtype=np.float32)[None, :]
    mp = np.arange(128, dtype=np.float32)[:, None]
    c["xtab"] = (up - (TS - 128) - mp).astype(np.float32)
    c["iota_n"] = np.tile(np.arange(TS, dtype=np.float32)[None, :], (128, 1))
    n = np.arange(TS)
    row = (n // 64).astype(np.float32); col = (n % 64).astype(np.float32)
    inv = (10000.0 ** (-np.arange(16, dtype=np.float32) / 16)).astype(np.float32)
    ang = np.concatenate([row[:, None] * inv, col[:, None] * inv], axis=-1).astype(np.float32)
    cosT, sinT = np.cos(ang).T.astype(np.float32), np.sin(ang).T.astype(np.float32)
    c["cos2"] = np.concatenate([cosT, cosT, cosT, cosT], axis=0)
    c["sin2"] = np.concatenate([-sinT, sinT, -sinT, sinT], axis=0)
    pm = np.zeros((128, 128), dtype=np.float32)
    for p in range(128):
        q = (p // 64) * 64 + ((p % 64) + 32) % 64
        pm[q, p] = 1.0
    c["perm"] = pm
    ev = np.zeros((128, 4), dtype=np.float32)
    for blk in range(2):
        m = blk * 128 + np.arange(128)
        ev[:, blk * 2 + 0] = TP - 1 - m
        ev[:, blk * 2 + 1] = m
    c["evals"] = ev
    t = np.arange(TS)
    c["cmask"] = np.tile((t % 32 != 0).astype(np.float32)[None, :], (128, 1))
    a = np.arange(128)
    same = (a[:, None] // 32) == (a[None, :] // 32)
    c["mk_f"] = (same & (a[:, None] <= a[None, :])).astype(np.float32)
    c["mk_b"] = (same & (a[:, None] >= a[None, :])).astype(np.float32)
    for nm, T in (("feats_s", TS), ("feats_p", TP)):
        tt = np.linspace(0.0, 1.0, T, dtype=np.float32)[:, None]
        w = ((2.0 * np.pi / T) * np.arange(T, dtype=np.float32))[:, None].astype(np.float32)
        bands = np.linspace(1e-4, 15.0, 16, dtype=np.float32)[None, :]
        feats = np.concatenate([tt, np.cos(bands * w), -np.sin(bands * w)], axis=-1).astype(np.float32)
        c[nm] = np.ascontiguousarray(feats.T)
    c["negpi"] = np.full((128, 1), -np.pi, dtype=np.float32)
    c["fcs_s"], c["gcs_s"] = dft_consts(TS)
    c["fcs_p"], c["gcs_p"] = dft_consts(TP)
    c["cmask64"] = np.tile((t % 64 != 0).astype(np.float32)[None, :], (128, 1))
    pp = np.arange(64)[:, None]; ff = np.arange(64)[None, :]
    for nm, m in (("m_lt", pp < ff), ("m_gt", pp > ff), ("m_le", pp <= ff), ("m_ge", pp >= ff)):
        c[nm] = np.ascontiguousarray(m.astype(np.float32))
    c["eye8"] = np.eye(64, dtype=np.float32)
    return c


class Rot:
    def __init__(self, tiles):
        self.t, self.i = tiles, 0

    def next(self):
        x = self.t[self.i % len(self.t)]
        self.i += 1
        return x


def build_program():
    nc = bass.Bass("TRN2", target_bir_lowering=False)
    k = K(nc)
    k.init_psum(8)
    E = {}

    def ein(name, shape, dt=F32):
        E[name] = Ext(nc.dram_tensor(name, list(shape), dt, kind="ExternalInput").ap())
        return E[name]

    def eout(name, shape, dt=F32):
        E[name] = Ext(nc.dram_tensor(name, list(shape), dt, kind="ExternalOutput").ap())
        return E[name]

    ein("xs", [TS, D]); ein("xp", [NPR * TP, D])
    ein("c_s", [1, D]); ein("c_ctx", [1, D])
    ein("final_g", [1, D])
    ein("ada_w", [NLAYERS_RUN, D, 6 * D]); ein("ada_b", [NLAYERS_RUN, 6 * D])
    ein("norm1_g", [NLAYERS_RUN, D]); ein("norm2_g", [NLAYERS_RUN, D])
    ein("w_in", [NLAYERS_RUN, D, N_IN])
    ein("br_w", [NLAYERS_RUN, 4, 256, D]); ein("w_out", [NLAYERS_RUN, D, D])
    ein("router", [NLAYERS_RUN, D, NE])
    if ENABLE["moe"]:
        ein("ex_w1", [NLAYERS_RUN, NE, D, FF]); ein("ex_w3", [NLAYERS_RUN, NE, D, FF]); ein("ex_w2", [NLAYERS_RUN, NE, FF, D])
    for nm, shp in (("ident", [128, 128]), ("iota_f", [128, 256]), ("triu", [128, 128]), ("ones", [128, 128])):
        ein(nm, shp)
    ein("st_ret", [L, 2, 4, 64, 64]); ein("ret_rate", [L, 8]); ein("ret_gn", [L, 2, 256])
    for nm, shp in (("xtab", [128, 2 * TS - 128]), ("iota_n", [128, TS]), ("cos2", [128, TS]), ("sin2", [128, TS]),
                    ("perm", [128, 128]), ("evals", [128, 4])):
        ein(nm, shp)
    ein("st_hg", [L, 2, 4, 64, 64]); ein("hg_lb", [L, 2, 256]); ein("hg_norm", [L, 256])
    for nm, shp in (("cmask", [128, TS]), ("mk_f", [128, 128]), ("mk_b", [128, 128])):
        ein(nm, shp)
    for nm, shp in (("hy_conv", [L, 3, 768]), ("hy_ffn1", [L, 33, 64]), ("hy_ffn1_b", [L, 64]), ("hy_ffn2", [L, 64, 64]),
                    ("hy_ffn2_b", [L, 64]), ("hy_ffn3", [L, 64, 1024]), ("hy_freq", [L, 2, 64]), ("hy_decay", [L, 1024]),
                    ("hy_skip", [L, 2, 256]), ("feats_s", [33, TS]), ("feats_p", [33, TP]), ("negpi", [128, 1]),
                    ("fcs_s", [2, TS // 128 + 1, 128, TS]), ("gcs_s", [2, TS // 128 + 1, 128, TS]),
                    ("fcs_p", [2, TP // 128 + 1, 128, TP]), ("gcs_p", [2, TP // 128 + 1, 128, TP])):
        ein(nm, shp)
    for nm, shp in (("st_rw", [L, 2, 4, 64, 64]), ("rw_mu", [L, 2, 896]), ("rw_w0", [L, 2, 256]), ("rw_w_up", [L, 2, 32, 256]),
                    ("rw_a0", [L, 256]), ("rw_a_up", [L, 32, 256]), ("rw_g_up", [L, 64, 256]), ("rw_kvec", [L, 3, 256]),
                    ("rw_ln", [L, 2, 256]), ("cmask64", [128, TS]), ("m_lt", [64, 64]), ("m_gt", [64, 64]),
                    ("m_le", [64, 64]), ("m_ge", [64, 64]), ("eye8", [64, 64])):
        ein(nm, shp)
    eout("ys", [TS, D]); eout("yp", [NPR * TP, D])
    eout("o_rw", [NPR, ST_ELEMS]); eout("o_ret", [NPR, ST_ELEMS]); eout("o_hg", [NPR, ST_ELEMS])

    C = {}
    for nm, shp in (("ident", [128, 128]), ("iota_f", [128, 256]), ("triu", [128, 128]), ("ones", [128, 128])):
        C[nm] = k.sb(nm, shp)
        k.dma(C[nm], E[nm])
    fg_b = k.sb("fg_b", [128, D])
    k.dma(fg_b, E["final_g"].bc([128, D]))

    with k.scope():
        zt = k.sb("zt", [128, 4096])
        k.op("pool", "memset", ap=zt.v, constant=0.0)
        for nm, en in (("o_rw", "rw"), ("o_ret", "ret"), ("o_hg", "hg")):
            if ENABLE[en] and NLAYERS_RUN == L:
                continue
            ov = E[nm].re("b (p f) -> b p f", p=128)
            for b in range(NPR):
                k.dma(ov[b], zt[:, 0:ST_ELEMS // 128])

    mod_d = k.dram("mod_d", [L, 2, 6 * D])
    adaln(k, E, C, mod_d)

    groups = (
        dict(name="s", xin=E["xs"], yout=E["ys"], NT=TS // 128, seqs=[(0, TS)], cond=0),
        dict(name="p", xin=E["xp"], yout=E["yp"], NT=NPR * TP // 128,
             seqs=[(j * TP, TP) for j in range(NPR)], cond=1),
    )
    for g in groups:
        if g['name'] not in GROUPS_RUN:
            continue
        TG = g["NT"] * 128
        g["TG"] = TG
        zT_d = k.dram("zT_" + g["name"], [N_IN, TG])
        yb_d = k.dram("yb_" + g["name"], [4 * 256, TG])
        with k.scope():
            NT = g["NT"]
            x = k.sb("x", [128, NT, D])
            xv = g["xin"].re("(n p) d -> p n d", p=128)
            yv = g["yout"].re("(n p) d -> p n d", p=128)
            for i in range(NT):
                k.dma(x[:, i, :], xv[:, i, :], free=True)
            for l in range(NLAYERS_RUN):
                stage_proj(k, E, C, g, l, x, mod_d, zT_d)
                stage_mixers(k, E, C, g, l, x, zT_d, yb_d)
                if ENABLE["ret"]:
                    stage_ret(k, E, C, g, l, zT_d, yb_d)
                if ENABLE["hg"]:
                    stage_hg(k, E, C, g, l, zT_d, yb_d)
                if ENABLE["hy"]:
                    stage_hy(k, E, C, g, l, zT_d, yb_d)
                if ENABLE["rw"]:
                    stage_rw(k, E, C, g, l, zT_d, yb_d)
                stage_merge(k, E, C, g, l, x, mod_d, zT_d, yb_d)
                if ENABLE["moe"]:
                    stage_moe(k, E, C, g, l, x, mod_d)
            with k.scope():
                final_norm(k, x, NT, fg_b, yv)
    k.finish()
    print("instructions:", k.n_ins)
    return nc


def adaln(k, E, C, mod_d):
    with k.scope():
        cc = k.sb("cc", [16, 128])
        k.dma(cc[0:8, :], E["c_s"].re("o (kc p) -> (o kc) p", p=128))
        k.dma(cc[8:16, :], E["c_ctx"].re("o (kc p) -> (o kc) p", p=128))
        cs = k.sb("cs", [16, 128])
        k.op("act", "activation", out=cs.v, in_=cc.v, func=AF.Sigmoid)
        k.op("dve", "tensor_tensor", out=cs.v, in0=cs.v, in1=cc.v, op=ALU.mult)
        ps = k.ps()
        k.op("pe", "transpose", out=ps[:, 0:16], in_=cs.v, identity=C["ident"][0:16, 0:16])
        scT = k.sb("scT", [128, 16])
        k.op("dve", "tensor_copy", out=scT.v, in_=ps[:, 0:16])
        scv = scT.v.re("p (b kc) -> p kc b", b=2)
        wb = Rot([k.sb("adaw", [128, 8, 512]) for _ in range(2)])
        bb = Rot([k.sb("adab", [2, 512]) for _ in range(2)])
        ob = Rot([k.sb("adao", [2, 512]) for _ in range(2)])
        for l in range(NLAYERS_RUN):
            for c0 in range(0, 6 * D, 512):
                w, b_, o = wb.next(), bb.next(), ob.next()
                k.dma(w, E["ada_w"][l][:, c0:c0 + 512].re("(kc p) n -> p kc n", p=128))
                k.dma(b_, E["ada_b"][l:l + 1, c0:c0 + 512].bc([2, 512]))
                ps = k.ps()
                for kc in range(8):
                    k.op("pe", "matmul", out=ps[0:2, :], lhsT=scv[:, kc, :], rhs=w[:, kc, :],
                         start=(kc == 0), stop=(kc == 7))
                k.op("dve", "tensor_tensor", out=o.v, in0=ps[0:2, :], in1=b_.v, op=ALU.add)
                k.dma(mod_d[l, :, c0:c0 + 512], o.v, free=True)


def bc_param(k, name, src_row):
    t = k.sb(name, [128, D])
    k.dma(t, src_row.bc([128, D]))
    return t


def mod_params(k, E, mod_d, l, cond, which, gname):
    off = 3 * D * which
    sh = bc_param(k, "sh", mod_d[l, cond:cond + 1, off:off + D])
    sc = bc_param(k, "sc", mod_d[l, cond:cond + 1, off + D:off + 2 * D])
    ng = bc_param(k, "ng", E[gname][l:l + 1, :])
    k.op("dve", "scalar_tensor_tensor", out=sc.v, in0=sc.v, scalar=1.0, in1=ng.v, op0=ALU.add, op1=ALU.mult)
    return sc, sh


class NormMod:
    def __init__(self, k):
        self.k = k
        self.junk = k.sb("junk", [128, D])
        self.ss = Rot([k.sb("ss", [128, 1]) for _ in range(2)])

    def __call__(self, xview, A, B, out):
        k = self.k
        ss = self.ss.next()
        k.op("act", "activation", out=self.junk.v, in_=xview, func=AF.Square, accum_out=ss.v)
        k.op("dve", "tensor_scalar", out=ss.v, in0=ss.v, scalar1=1.0 / D, scalar2=EPS, op0=ALU.mult, op1=ALU.add)
        k.op("act", "activation", out=ss.v, in_=ss.v, func=AF.Ln)
        k.op("act", "activation", out=ss.v, in_=ss.v, func=AF.Exp, scale=-0.5)
        k.op("dve", "scalar_tensor_tensor", out=out, in0=xview, scalar=ss[:, 0:1], in1=A.v,
             op0=ALU.mult, op1=ALU.mult)
        if B is not None:
            k.op("dve", "tensor_tensor", out=out, in0=out, in1=B.v, op=ALU.add)


def to_fm(k, C, h_tm, hT, i, free=True, evi=[0]):
    for j in range(2):
        ps = k.ps()
        for a in range(4):
            kc = 4 * j + a
            k.op("pe", "transpose", out=ps[:, a * 128:(a + 1) * 128], in_=h_tm[:, kc * 128:(kc + 1) * 128],
                 identity=C["ident"].v)
        eng = "act" if (evi[0] % 2) else "dve"
        evi[0] += 1
        dst = hT[:, 4 * j:4 * j + 4, i * 128:(i + 1) * 128]
        src = ps.v.re("p (a t) -> p a t", a=4)
        if eng == "act":
            k.op("act", "copy", out=dst, in_=src, free=free)
        else:
            k.op("dve", "tensor_copy", out=dst, in_=src, free=free)


def linear_fm(k, w_ext, col0, ncols, inT, KC, T, evac, wrot, WB=256, TB=512, bfrot=None):
    for c0 in range(col0, col0 + ncols, WB):
        cw = min(WB, col0 + ncols - c0)
        wb = wrot.next()
        k.dma(wb[:, :, 0:cw], w_ext[:, c0:c0 + cw].re("(kc p) n -> p kc n", p=128))
        if bfrot is not None:
            wbb = bfrot.next()
            k.op("pool", "tensor_copy", out=wbb[:, :, 0:cw], in_=wb[:, :, 0:cw])
            wb = wbb
        for n0 in range(0, cw, 128):
            nw = min(128, cw - n0)
            for t0 in range(0, T, TB):
                tw = min(TB, T - t0)
                ps = k.ps()
                for kc in range(KC):
                    k.op("pe", "matmul", out=ps[0:nw, 0:tw], lhsT=wb[:, kc, n0:n0 + nw],
                         rhs=inT[:, kc, t0:t0 + tw], start=(kc == 0), stop=(kc == KC - 1))
                evac(ps, c0 + n0, nw, t0, tw)


def stage_proj(k, E, C, g, l, x, mod_d, zT_d):
    NT, TG = g["NT"], g["TG"]
    with k.scope():
        A, B = mod_params(k, E, mod_d, l, g["cond"], 0, "norm1_g")
        nm = NormMod(k)
        hT = k.sb("hT", [128, 8, TG], BF16 if PROJ_BF16 else F32)
        htm = Rot([k.sb("htm", [128, D]) for _ in range(2)])
        for i in range(NT):
            h = htm.next()
            nm(x[:, i, :], A, B, h.v)
            to_fm(k, C, h, hT, i)
        wrot = Rot([k.sb("wb", [128, 8, 256]) for _ in range(2)])
        bfrot = Rot([k.sb("wbb", [128, 8, 256], BF16) for _ in range(2)]) if PROJ_BF16 else None
        stg = Rot([k.sb("stg", [128, 512]) for _ in range(3)])
        cnt = [0]

        def evac(ps, row0, nw, t0, tw):
            s = stg.next()
            if row0 >= O_MG:
                k.op("act", "activation", out=s[0:nw, 0:tw], in_=ps[0:nw, 0:tw], func=AF.Sigmoid)
            elif cnt[0] % 2:
                k.op("act", "copy", out=s[0:nw, 0:tw], in_=ps[0:nw, 0:tw])
            else:
                k.op("dve", "tensor_copy", out=s[0:nw, 0:tw], in_=ps[0:nw, 0:tw])
            cnt[0] += 1
            k.dma(zT_d[row0:row0 + nw, t0:t0 + tw], s[0:nw, 0:tw], free=True)

        ranges = []
        if ENABLE["rw"]:
            ranges.append((O_RW, 896))
        if ENABLE["hy"]:
            ranges.append((O_HY, 768))
        if ENABLE["ret"]:
            ranges.append((O_RET, 1024))
        if ENABLE["hg"]:
            ranges.append((O_HG, 1280))
        ranges.append((O_MG, 4096))
        for (c0, n) in ranges:
            linear_fm(k, E["w_in"][l], c0, n, hT, 8, TG, evac, wrot, bfrot=bfrot)


def stage_mixers(k, E, C, g, l, x, zT_d, yb_d):
    TG = g["TG"]
    with k.scope():
        z = k.sb("zz", [128, 2048])
        k.op("pool", "memset", ap=z.v, constant=0.0)
        for b, nm in enumerate(("rw", "hy", "ret", "hg")):
            if not ENABLE[nm]:
                for c in range(2):
                    k.dma(yb_d[b * 256 + c * 128:b * 256 + (c + 1) * 128, :], z[:, 0:TG], free=True)


def stage_ret(k, E, C, g, l, zT_d, yb_d):
    TG, seqs = g["TG"], g["seqs"]
    is_s = g["name"] == "s"
    zq, zk, zv, zg = O_RET, O_RET + 256, O_RET + 512, O_RET + 768
    if is_s:
        with k.scope():
            cos2 = k.sb("cos2", [128, TS]); k.dma(cos2, E["cos2"])
            sin2 = k.sb("sin2", [128, TS]); k.dma(sin2, E["sin2"])
            perm = k.sb("perm", [128, 128]); k.dma(perm, E["perm"])
            tq = Rot([k.sb("rq", [128, TS]) for _ in range(2)])
            to = Rot([k.sb("ro", [128, TS]) for _ in range(2)])
            tmp = Rot([k.sb("rt", [128, 512]) for _ in range(2)])
            for r0 in (zq, zq + 128, zk, zk + 128):
                t, o = tq.next(), to.next()
                k.dma(t, zT_d[r0:r0 + 128, :])
                for nb in range(0, TS, 512):
                    ps = k.ps()
                    k.op("pe", "matmul", out=ps.v, lhsT=perm.v, rhs=t[:, nb:nb + 512], start=True, stop=True)
                    tm = tmp.next()
                    k.op("dve", "tensor_tensor", out=tm.v, in0=ps.v, in1=sin2[:, nb:nb + 512], op=ALU.mult)
                    k.op("pool", "tensor_tensor", out=o[:, nb:nb + 512], in0=t[:, nb:nb + 512],
                         in1=cos2[:, nb:nb + 512], op=ALU.mult)
                    k.op("dve", "tensor_tensor", out=o[:, nb:nb + 512], in0=o[:, nb:nb + 512], in1=tm.v, op=ALU.add)
                k.dma(zT_d[r0:r0 + 128, :], o.v)
    with k.scope():
        lg = k.sb("lg", [128, 8])
        nlg = k.sb("nlg", [128, 8])
        lgT = k.sb("lgT", [128, 8])
        k.dma(lg, E["ret_rate"][l:l + 1, :].bc([128, 8]))
        k.op("act", "activation", out=nlg.v, in_=lg.v, func=AF.Exp)
        k.op("dve", "tensor_scalar", out=lg.v, in0=nlg.v, scalar1=-1.0, scalar2=None, op0=ALU.mult)
        gn4 = k.sb("gn4", [4, 128])
        k.dma(gn4, E["ret_gn"][l].re("g (hp c) -> (g hp) c", c=128))
        psg = k.ps()
        k.op("pe", "transpose", out=psg[:, 0:4], in_=gn4.v, identity=C["ident"][0:4, 0:4])
        gnT = k.sb("gnT", [128, 4])
        k.op("dve", "tensor_copy", out=gnT.v, in_=psg[:, 0:4])
        Tmax = max(T for (_, T) in seqs)
        k.op("dve", "tensor_scalar", out=lgT.v, in0=lg.v, scalar1=float(Tmax), scalar2=None, op0=ALU.mult)
        WW = 2 * Tmax - 128
        xoff = (TS - 128) - (Tmax - 128)
        Xt = k.sb("Xt", [128, WW]); k.dma(Xt, E["xtab"][:, xoff:xoff + WW])
        W = k.sb("W", [128, WW])
        HW = WW // 2
        tmpW = k.sb("tmpW", [128, HW])
        NB = min(512, Tmax)
        nnb = Tmax // NB
        nblk = Tmax // 128
        qTt, kTt, vTt, gTt = (k.sb(nm, [128, Tmax]) for nm in ("qT", "kT", "vT", "gT"))
        v_tm = k.sb("v_tm", [128, nblk, 128])
        yo = k.sb("yo", [128, Tmax])
        atr = Rot([k.sb("at", [128, Tmax]) for _ in range(2)])
        t64 = Rot([k.sb("t64", [128, NB]) for _ in range(6)])
        if is_s:
            iota_n = k.sb("iota_n", [128, TS]); k.dma(iota_n, E["iota_n"])
            s0f = k.sb("s0f", [128, 128]); s0b = k.sb("s0b", [128, 128])
        else:
            k_tm = k.sb("k_tm", [128, nblk, 128])
            evals = k.sb("evals", [128, 4]); k.dma(evals, E["evals"])
            tokdec = k.sb("tokdec", [128, 2, 8])
            for blk in range(2):
                for d in range(2):
                    for h in range(4):
                        k.op("act", "activation", out=tokdec[:, blk, d * 4 + h:d * 4 + h + 1],
                             in_=evals[:, blk * 2 + d:blk * 2 + d + 1], func=AF.Exp, scale=lg[:, d * 4 + h:d * 4 + h + 1])
            kdr = Rot([k.sb("kd", [128, 64]) for _ in range(2)])
            sor = Rot([k.sb("so", [64, 64]) for _ in range(2)])
            o_ret = E["o_ret"].re("b (l d h k v) -> b l d h k v", l=L, d=2, h=4, k=64)
        for sj, (s0, T) in enumerate(seqs):
            for hp in range(2):
                k.dma(qTt, zT_d[zq + hp * 128:zq + (hp + 1) * 128, s0:s0 + T])
                k.dma(kTt, zT_d[zk + hp * 128:zk + (hp + 1) * 128, s0:s0 + T])
                k.dma(vTt, zT_d[zv + hp * 128:zv + (hp + 1) * 128, s0:s0 + T])
                k.dma(gTt, zT_d[zg + hp * 128:zg + (hp + 1) * 128, s0:s0 + T])
                k.op("act", "mul", out=qTt.v, in_=qTt.v, mul=0.125)
                for blk in range(nblk):
                    ps = k.ps()
                    k.op("pe", "transpose", out=ps[:, 0:128], in_=vTt[:, blk * 128:(blk + 1) * 128], identity=C["ident"].v)
                    k.op("act", "copy", out=v_tm[:, blk, :], in_=ps[:, 0:128])
                    if not is_s:
                        ps2 = k.ps()
                        k.op("pe", "transpose", out=ps2[:, 0:128], in_=kTt[:, blk * 128:(blk + 1) * 128],
                             identity=C["ident"].v)
                        k.op("dve", "tensor_copy", out=k_tm[:, blk, :], in_=ps2[:, 0:128])
                if is_s:
                    k.op("pool", "memset", ap=s0f.v, constant=0.0)
                    k.op("pool", "memset", ap=s0b.v, constant=0.0)
                    for hh in range(2):
                        P0 = hh * 64
                        k.dma(s0f[P0:P0 + 64, P0:P0 + 64], E["st_ret"][l, 0, hp * 2 + hh])
                        k.dma(s0b[P0:P0 + 64, P0:P0 + 64], E["st_ret"][l, 1, hp * 2 + hh])
                for hh in range(2):
                    h = hp * 2 + hh
                    P0 = hh * 64
                    sl = slice(P0, P0 + 64)
                    cf, cb = h, 4 + h
                    for half in range(2):
                        c0, c1 = half * HW, (half + 1) * HW
                        k.op("act", "activation", out=tmpW.v, in_=Xt[:, c0:c1], func=AF.Exp, scale=lg[:, cf:cf + 1])
                        k.op("dve", "scalar_tensor_tensor", out=W[:, c0:c1], in0=Xt[:, c0:c1], scalar=0.0, in1=tmpW.v,
                             op0=ALU.is_ge, op1=ALU.mult)
                        k.op("act", "activation", out=tmpW.v, in_=Xt[:, c0:c1], func=AF.Exp, scale=nlg[:, cb:cb + 1])
                        k.op("dve", "scalar_tensor_tensor", out=tmpW.v, in0=Xt[:, c0:c1], scalar=0.0, in1=tmpW.v,
                             op0=ALU.is_le, op1=ALU.mult)
                        k.op("pool", "tensor_tensor", out=W[:, c0:c1], in0=W[:, c0:c1], in1=tmpW.v, op=ALU.add)
                    acc = k.ps_reserve(nnb)
                    if is_s:
                        for nb in range(nnb):
                            cs = slice(nb * NB, (nb + 1) * NB)
                            d1, d2 = t64.next(), t64.next()
                            k.op("act", "activation", out=d1[sl, :], in_=iota_n[sl, cs], func=AF.Exp,
                                 scale=lg[sl, cf:cf + 1], bias=lg[sl, cf:cf + 1])
                            k.op("dve", "tensor_tensor", out=d1[sl, :], in0=d1[sl, :], in1=qTt[sl, cs], op=ALU.mult)
                            k.op("pe", "matmul", out=acc[nb][:, 0:NB], lhsT=s0f[sl, :], rhs=d1[sl, :], start=True, stop=False)
                            k.op("act", "activation", out=d2[sl, :], in_=iota_n[sl, cs], func=AF.Exp,
                                 scale=nlg[sl, cb:cb + 1], bias=lgT[sl, cb:cb + 1])
                            k.op("dve", "tensor_tensor", out=d2[sl, :], in0=d2[sl, :], in1=qTt[sl, cs], op=ALU.mult)
                            k.op("pe", "matmul", out=acc[nb][:, 0:NB], lhsT=s0b[sl, :], rhs=d2[sl, :], start=False, stop=False)
                    for j in range(nblk):
                        at = atr.next()
                        for nb in range(nnb):
                            ps = k.ps()
                            k.op("pe", "matmul", out=ps[:, 0:NB], lhsT=kTt[sl, j * 128:(j + 1) * 128],
                                 rhs=qTt[sl, nb * NB:(nb + 1) * NB], start=True, stop=True)
                            wc = (T - 128 - 128 * j) + nb * NB
                            k.op("dve", "tensor_tensor", out=at[:, nb * NB:(nb + 1) * NB], in0=ps[:, 0:NB],
                                 in1=W[:, wc:wc + NB], op=ALU.mult)
                        for nb in range(nnb):
                            k.op("pe", "matmul", out=acc[nb][:, 0:NB], lhsT=v_tm[:, j, :], rhs=at[:, nb * NB:(nb + 1) * NB],
                                 start=(j == 0 and not is_s), stop=(j == nblk - 1))
                    for nb in range(nnb):
                        cs = slice(nb * NB, (nb + 1) * NB)
                        ysb, cen, sq, rs, sg = (t64.next() for _ in range(5))
                        k.op("act", "copy", out=ysb[sl, :], in_=acc[nb][sl, 0:NB])
                        pm = k.ps()
                        k.op("pe", "matmul", out=pm[:, 0:NB], lhsT=C["ones"][sl, :], rhs=ysb[sl, :], start=True, stop=True)
                        k.op("dve", "scalar_tensor_tensor", out=cen[sl, :], in0=pm[sl, 0:NB], scalar=-1.0 / 64, in1=ysb[sl, :],
                             op0=ALU.mult, op1=ALU.add)
                        k.op("act", "activation", out=sq[sl, :], in_=cen[sl, :], func=AF.Square)
                        pv = k.ps()
                        k.op("pe", "matmul", out=pv[:, 0:NB], lhsT=C["ones"][sl, :], rhs=sq[sl, :], start=True, stop=True)
                        k.op("dve", "tensor_scalar", out=rs[sl, :], in0=pv[sl, 0:NB], scalar1=1.0 / 64, scalar2=1e-5,
                             op0=ALU.mult, op1=ALU.add)
                        k.op("act", "activation", out=rs[sl, :], in_=rs[sl, :], func=AF.Ln)
                        k.op("act", "activation", out=rs[sl, :], in_=rs[sl, :], func=AF.Exp, scale=-0.5)
                        k.op("dve", "tensor_tensor", out=cen[sl, :], in0=cen[sl, :], in1=rs[sl, :], op=ALU.mult)
                        k.op("dve", "tensor_scalar", out=cen[sl, :], in0=cen[sl, :], scalar1=gnT[sl, hp:hp + 1],
                             scalar2=gnT[sl, 2 + hp:3 + hp], op0=ALU.mult, op1=ALU.add)
                        k.op("act", "activation", out=sg[sl, :], in_=gTt[sl, cs], func=AF.Sigmoid)
                        k.op("dve", "tensor_tensor", out=sg[sl, :], in0=sg[sl, :], in1=gTt[sl, cs], op=ALU.mult)
                        k.op("dve", "tensor_tensor", out=yo[sl, cs], in0=cen[sl, :], in1=sg[sl, :], op=ALU.mult)
                    k.ps_release(acc)
                    if not is_s:
                        b = sj
                        for d in range(2):
                            pS = k.ps()
                            for blk in range(nblk):
                                kd = kdr.next()
                                k.op("dve", "tensor_scalar", out=kd.v, in0=k_tm[:, blk, P0:P0 + 64],
                                     scalar1=tokdec[:, blk, d * 4 + h:d * 4 + h + 1], scalar2=None, op0=ALU.mult)
                                k.op("pe", "matmul", out=pS[0:64, 0:64], lhsT=kd.v, rhs=v_tm[:, blk, P0:P0 + 64],
                                     start=(blk == 0), stop=(blk == nblk - 1))
                            so = sor.next()
                            k.op("act", "copy", out=so.v, in_=pS[0:64, 0:64])
                            k.dma(o_ret[b, l, d, h], so.v)
                k.dma(yb_d[512 + hp * 128:512 + (hp + 1) * 128, s0:s0 + T], yo[:, 0:T])


def stage_hg(k, E, C, g, l, zT_d, yb_d):
    TG, seqs = g["TG"], g["seqs"]
    is_s = g["name"] == "s"
    z0 = O_HG
    with k.scope():
        lb16 = k.sb("lb16", [16, 128])
        k.dma(lb16, E["hg_lb"].re("l d (hp c) -> (l d hp) c", c=128))
        ps = k.ps()
        k.op("pe", "transpose", out=ps[:, 0:16], in_=lb16.v, identity=C["ident"][0:16, 0:16])
        lbT = k.sb("lbT", [128, 4, 4])
        k.op("dve", "tensor_copy", out=lbT.v.re("p l j -> p (l j)"), in_=ps[:, 0:16])
        mx = k.sb("mx4", [128, 4]); sm4 = k.sb("sm4", [128, 4])
        k.op("dve", "tensor_tensor", out=mx.v, in0=lbT[:, 0, :], in1=lbT[:, 1, :], op=ALU.max)
        k.op("dve", "tensor_tensor", out=mx.v, in0=mx.v, in1=lbT[:, 2, :], op=ALU.max)
        k.op("dve", "tensor_tensor", out=mx.v, in0=mx.v, in1=lbT[:, 3, :], op=ALU.max)
        for ll in range(4):
            k.op("dve", "tensor_tensor", out=lbT[:, ll, :], in0=lbT[:, ll, :], in1=mx.v, op=ALU.subtract)
        k.op("act", "activation", out=lbT.v, in_=lbT.v, func=AF.Exp)
        k.op("dve", "tensor_tensor", out=sm4.v, in0=lbT[:, 0, :], in1=lbT[:, 1, :], op=ALU.add)
        k.op("dve", "tensor_tensor", out=sm4.v, in0=sm4.v, in1=lbT[:, 2, :], op=ALU.add)
        k.op("dve", "tensor_tensor", out=sm4.v, in0=sm4.v, in1=lbT[:, 3, :], op=ALU.add)
        k.op("dve", "reciprocal", out=sm4.v, in_=sm4.v)
        lbl = k.sb("lbl", [128, 4]); oml = k.sb("oml", [128, 4]); noml = k.sb("noml", [128, 4]); lbf = k.sb("lbf", [128, 4])
        k.op("pool", "memset", ap=lbl.v, constant=0.0)
        for ll in range(1, l + 1):
            k.op("dve", "tensor_tensor", out=mx.v, in0=lbT[:, ll, :], in1=sm4.v, op=ALU.mult)
            k.op("dve", "tensor_tensor", out=lbl.v, in0=lbl.v, in1=mx.v, op=ALU.add)
        k.op("dve", "tensor_scalar", out=oml.v, in0=lbl.v, scalar1=-1.0, scalar2=1.0, op0=ALU.mult, op1=ALU.add)
        k.op("dve", "tensor_scalar", out=noml.v, in0=oml.v, scalar1=-1.0, scalar2=None, op0=ALU.mult)
        k.op("dve", "tensor_scalar", out=lbf.v, in0=lbl.v, scalar1=1e-30, scalar2=None, op0=ALU.max)
        hn2 = k.sb("hn2", [2, 128])
        k.dma(hn2, E["hg_norm"][l:l + 1, :].re("o (hp c) -> (o hp) c", c=128))
        ps = k.ps()
        k.op("pe", "transpose", out=ps[:, 0:2], in_=hn2.v, identity=C["ident"][0:2, 0:2])
        hnT = k.sb("hnT", [128, 2])
        k.op("dve", "tensor_copy", out=hnT.v, in_=ps[:, 0:2])
        Tm = max(T for (_, T) in seqs)
        nblk, nch = Tm // 128, Tm // 32
        NB = min(512, Tm)
        cm = k.sb("cm", [128, Tm]); k.dma(cm, E["cmask"][:, 0:Tm])
        mk = [k.sb("mkf", [128, 128]), k.sb("mkb", [128, 128])]
        k.dma(mk[0], E["mk_f"]); k.dma(mk[1], E["mk_b"])
        qh, zf, vT, gh = (k.sb(nm, [128, Tm]) for nm in ("qh", "zf", "vT", "gh"))
        qt, kt, t1 = (k.sb(nm, [128, Tm]) for nm in ("qt", "kt", "t1"))
        kin, bb = zf, vT
        bend = k.sb("bend", [128, nch]); ebend = k.sb("ebend", [128, nch])
        v_tm = k.sb("v_tm", [128, nblk, 128]); kh_tm = k.sb("kh_tm", [64, 2 * nblk, 128])
        v_t64 = k.sb("v_t64", [64, 2 * nblk, 128])
        yd = [k.sb("yf", [128, Tm]), k.sb("ybw", [128, Tm])]
        S = [[k.sb("S%d%d" % (hh, i), [128, 128]) for i in range(2)] for hh in range(2)]
        atr = [Rot([k.sb("at%d" % hh, [128, 128]) for _ in range(2)]) for hh in range(2)]
        t64 = Rot([k.sb("h64", [128, NB]) for _ in range(4)])
        sor = Rot([k.sb("hso", [128, 64]) for _ in range(2)])
        o_hg = E["o_hg"].re("b (l d h k v) -> b l d h k v", l=L, d=2, h=4, k=64)
        c3 = lambda t: t.v.re("p (c s) -> p c s", s=32)
        for sj, (s0, T) in enumerate(seqs):
            for hp in range(2):
                r = lambda o: zT_d[z0 + o + hp * 128:z0 + o + (hp + 1) * 128, s0:s0 + T]
                k.dma(qh, r(0)); k.dma(vT, r(768)); k.dma(gh, r(1024))
                k.op("act", "activation", out=t1.v, in_=qh.v, func=AF.Sigmoid)
                k.op("dve", "tensor_tensor", out=qh.v, in0=qh.v, in1=t1.v, op=ALU.mult)
                for blk in range(nblk):
                    ps = k.ps()
                    k.op("pe", "transpose", out=ps[:, 0:128], in_=vT[:, blk * 128:(blk + 1) * 128], identity=C["ident"].v)
                    k.op("act", "copy", out=v_tm[:, blk, :], in_=ps[:, 0:128])
                for hb in range(2 * nblk):
                    ps = k.ps()
                    k.op("pe", "transpose", out=ps[0:64, 0:128], in_=vT[:, hb * 64:(hb + 1) * 64], identity=C["ident"].v)
                    k.op("dve", "tensor_copy", out=v_t64[:, hb, :], in_=ps[0:64, 0:128])
                for d in range(2):
                    j = d * 2 + hp
                    k.dma(zf, r(256 + 256 * d))
                    k.op("act", "activation", out=t1.v, in_=zf.v, func=AF.Sigmoid)
                    k.op("dve", "tensor_scalar", out=kin.v, in0=t1.v, scalar1=noml[:, j:j + 1], scalar2=oml[:, j:j + 1],
                         op0=ALU.mult, op1=ALU.add)
                    k.op("dve", "tensor_scalar", out=t1.v, in0=t1.v, scalar1=oml[:, j:j + 1], scalar2=lbf[:, j:j + 1],
                         op0=ALU.mult, op1=ALU.add)
                    k.op("act", "activation", out=t1.v, in_=t1.v, func=AF.Ln)
                    k.op("dve", "tensor_tensor_scan", out=bb.v, data0=cm.v, data1=t1.v, initial=0.0,
                         op0=ALU.mult, op1=ALU.add)
                    k.op("dve", "tensor_copy", out=bend.v, in_=c3(bb)[:, :, 31])
                    if d == 1:
                        k.op("dve", "tensor_tensor", out=c3(bb), in0=bend.v.re("p (c o) -> p c o", o=1).bc([128, nch, 32]),
                             in1=c3(bb), op=ALU.subtract)
                        k.op("dve", "tensor_tensor", out=bb.v, in0=bb.v, in1=t1.v, op=ALU.add)
                    k.op("act", "activation", out=t1.v, in_=bb.v, func=AF.Exp)
                    k.op("dve", "tensor_tensor", out=qt.v, in0=qh.v, in1=t1.v, op=ALU.mult)
                    k.op("act", "activation", out=t1.v, in_=bb.v, func=AF.Exp, scale=-1.0)
                    k.op("dve", "tensor_tensor", out=kt.v, in0=kin.v, in1=t1.v, op=ALU.mult)
                    k.op("act", "activation", out=ebend.v, in_=bend.v, func=AF.Exp)
                    k.op("dve", "tensor_tensor", out=c3(t1), in0=c3(kt),
                         in1=ebend.v.re("p (c o) -> p c o", o=1).bc([128, nch, 32]), op=ALU.mult)
                    for hb in range(2 * nblk):
                        ps = k.ps()
                        k.op("pe", "transpose", out=ps[0:64, 0:128], in_=t1[:, hb * 64:(hb + 1) * 64], identity=C["ident"].v)
                        k.op("act", "copy", out=kh_tm[:, hb, :], in_=ps[0:64, 0:128])
                    cur = [0, 0]
                    for hh in range(2):
                        P0 = hh * 64
                        for i in range(2):
                            k.op("pool", "memset", ap=S[hh][i].v, constant=0.0)
                        if is_s:
                            k.dma(S[hh][0][P0:P0 + 64, P0:P0 + 64], E["st_hg"][l, d, hp * 2 + hh])
                    blks = range(nblk) if d == 0 else range(nblk - 1, -1, -1)
                    chs = range(4) if d == 0 else range(3, -1, -1)
                    for blk in blks:
                        bc_ = slice(blk * 128, (blk + 1) * 128)
                        ats, accs = [], []
                        for hh in range(2):
                            sl = slice(hh * 64, hh * 64 + 64)
                            ps = k.ps()
                            k.op("pe", "matmul", out=ps[:, 0:128], lhsT=kt[sl, bc_], rhs=qt[sl, bc_], start=True, stop=True)
                            at = atr[hh].next()
                            k.op("dve", "tensor_tensor", out=at.v, in0=ps[:, 0:128], in1=mk[d].v, op=ALU.mult)
                            ats.append(at)
                        accs = k.ps_reserve(2)
                        for c in chs:
                            lc = slice(c * 32, (c + 1) * 32)
                            gc = slice(blk * 128 + c * 32, blk * 128 + (c + 1) * 32)
                            ci = blk * 4 + c
                            for hh in range(2):
                                P0 = hh * 64
                                sl = slice(P0, P0 + 64)
                                Sc, Sn = S[hh][cur[hh]], S[hh][1 - cur[hh]]
                                k.op("pe", "matmul", out=accs[hh][:, lc], lhsT=Sc[sl, :], rhs=qt[sl, gc], start=True, stop=False)
                                k.op("pe", "matmul", out=accs[hh][:, lc], lhsT=v_tm[:, blk, :], rhs=ats[hh][:, lc],
                                     start=False, stop=True)
                                pu = k.ps()
                                hb, pb = blk * 2 + c // 2, (c % 2) * 32
                                k.op("pe", "matmul", out=pu[:, 0:64], lhsT=kh_tm[pb:pb + 32, hb, :],
                                     rhs=v_t64[pb:pb + 32, hb, P0:P0 + 64], start=True, stop=True)
                                k.op("dve", "scalar_tensor_tensor", out=Sn[sl, P0:P0 + 64], in0=Sc[sl, P0:P0 + 64],
                                     scalar=ebend[sl, ci:ci + 1], in1=pu[sl, 0:64], op0=ALU.mult, op1=ALU.add)
                                cur[hh] = 1 - cur[hh]
                        for hh in range(2):
                            sl = slice(hh * 64, hh * 64 + 64)
                            k.op("act", "copy", out=yd[d][sl, bc_], in_=accs[hh][sl, 0:128])
                        k.ps_release(accs)
                    if not is_s:
                        for hh in range(2):
                            P0 = hh * 64
                            so = sor.next()
                            k.op("dve", "tensor_copy", out=so[P0:P0 + 64, :], in_=S[hh][cur[hh]][P0:P0 + 64, P0:P0 + 64])
                            k.dma(o_hg[sj, l, d, hp * 2 + hh], so[P0:P0 + 64, :])
                k.op("dve", "tensor_tensor", out=yd[0].v, in0=yd[0].v, in1=yd[1].v, op=ALU.add)
                k.op("act", "activation", out=t1.v, in_=gh.v, func=AF.Sigmoid)
                k.op("dve", "tensor_tensor", out=gh.v, in0=gh.v, in1=t1.v, op=ALU.mult)
                for nb in range(T // NB):
                    cs = slice(nb * NB, (nb + 1) * NB)
                    for hh in range(2):
                        sl = slice(hh * 64, hh * 64 + 64)
                        sq, rs = t64.next(), t64.next()
                        k.op("act", "activation", out=sq[sl, :], in_=yd[0][sl, cs], func=AF.Square)
                        pm = k.ps()
                        k.op("pe", "matmul", out=pm[:, 0:NB], lhsT=C["ones"][sl, :], rhs=sq[sl, :], start=True, stop=True)
                        k.op("dve", "tensor_scalar", out=rs[sl, :], in0=pm[sl, 0:NB], scalar1=1.0 / 64, scalar2=EPS,
                             op0=ALU.mult, op1=ALU.add)
                        k.op("act", "activation", out=rs[sl, :], in_=rs[sl, :], func=AF.Ln)
                        k.op("act", "activation", out=rs[sl, :], in_=rs[sl, :], func=AF.Exp, scale=-0.5)
                        k.op("dve", "scalar_tensor_tensor", out=rs[sl, :], in0=rs[sl, :], scalar=hnT[sl, hp:hp + 1],
                             in1=yd[0][sl, cs], op0=ALU.mult, op1=ALU.mult)
                        k.op("dve", "tensor_tensor", out=yd[1][sl, cs], in0=rs[sl, :], in1=gh[sl, cs], op=ALU.mult)
                k.dma(yb_d[768 + hp * 128:768 + (hp + 1) * 128, s0:s0 + T], yd[1][:, 0:T])


def load_T(k, C, src, n, name):
    st = k.sb(name + "_st", [n, 128])
    k.dma(st, src)
    ps = k.ps()
    k.op("pe", "transpose", out=ps[:, 0:n], in_=st.v, identity=C["ident"][0:n, 0:n])
    t = k.sb(name, [128, n])
    k.op("dve", "tensor_copy", out=t.v, in_=ps[:, 0:n])
    return t


def dft_consts(T):
    nblk, NFC = T // 128, T // 128 + 1
    N2 = 2 * T
    f = np.arange(NFC * 128, dtype=np.float64)
    t = np.arange(T, dtype=np.float64)
    th = 2.0 * np.pi * np.outer(f, t) / N2
    valid = (f <= T)[:, None]
    c = np.where(valid, np.cos(th), 0.0)
    s_ = np.where(valid, np.sin(th), 0.0)
    def fw(m):
        a = m.reshape(NFC, 128, nblk, 128)
        return np.ascontiguousarray(a.transpose(0, 3, 2, 1).reshape(NFC, 128, nblk * 128))
    fcs = np.stack([fw(c), fw(s_)]).astype(np.float32)
    wf = np.where((f == 0) | (f == T), 1.0, 2.0)[:, None] * valid
    gc = (wf * c / N2).reshape(NFC, 128, T)
    gs = (-wf * s_ / N2).reshape(NFC, 128, T)
    gcs = np.stack([gc, gs]).astype(np.float32)
    return fcs, gcs


def stage_hy(k, E, C, g, l, zT_d, yb_d):
    TG, seqs = g["TG"], g["seqs"]
    T = seqs[0][1]
    sfx = "s" if T == TS else "p"
    FCS, GCS = E["fcs_" + sfx], E["gcs_" + sfx]
    nblk, NFC = T // 128, T // 128 + 1
    z0 = O_HY
    PI = float(np.pi)
    NB = min(512, T)
    with k.scope():
        skT = load_T(k, C, E["hy_skip"][l].re("o (c p) -> (o c) p", p=128), 4, "skT")
        hcT = load_T(k, C, E["hy_conv"][l].re("w (c p) -> (w c) p", p=128), 18, "hcT")
        decT = load_T(k, C, E["hy_decay"][l:l + 1, :].re("o (c p) -> (o c) p", p=128), 8, "decT")
        dneg = k.sb("dneg", [128, 8])
        k.op("dve", "tensor_scalar", out=dneg.v, in0=decT.v, scalar1=-1.0, scalar2=None, op0=ALU.mult)
        k.op("dve", "tensor_tensor", out=decT.v, in0=decT.v, in1=dneg.v, op=ALU.max)
        k.op("dve", "tensor_scalar", out=decT.v, in0=decT.v, scalar1=-1.0 / (T - 1), scalar2=None, op0=ALU.mult)
        w3 = k.sb("hw3", [64, 1024]); k.dma(w3, E["hy_ffn3"][l])
        hid2 = k.sb("hid2", [64, T])
        with k.scope():
            featsT = k.sb("featsT", [33, T]); k.dma(featsT, E["feats_s" if T == TS else "feats_p"])
            w1 = k.sb("hw1", [33, 64]); k.dma(w1, E["hy_ffn1"][l])
            w2 = k.sb("hw2", [64, 64]); k.dma(w2, E["hy_ffn2"][l])
            pr = k.sb("hpr", [64, 4])
            k.dma(pr[:, 0:1], E["hy_ffn1_b"][l:l + 1, :].re("o (p q) -> (o p) q", q=1))
            k.dma(pr[:, 1:2], E["hy_ffn2_b"][l:l + 1, :].re("o (p q) -> (o p) q", q=1))
            k.dma(pr[:, 2:3], E["hy_freq"][l, 0:1, :].re("o (p q) -> (o p) q", q=1))
            k.dma(pr[:, 3:4], E["hy_freq"][l, 1:2, :].re("o (p q) -> (o p) q", q=1))
            hid1 = k.sb("hid1", [64, T])
            m1 = k.sb("hm1", [64, NB])

            def sin_layer(wt, rhs, bcol, fcol, dst):
                for n0 in range(0, T, NB):
                    ps = k.ps()
                    k.op("pe", "matmul", out=ps[0:64, 0:NB], lhsT=wt, rhs=rhs[:, n0:n0 + NB], start=True, stop=True)
                    a = dst[:, n0:n0 + NB]
                    k.op("dve", "tensor_scalar", out=a, in0=ps[0:64, 0:NB], scalar1=pr[:, bcol:bcol + 1],
                         scalar2=pr[:, fcol:fcol + 1], op0=ALU.add, op1=ALU.mult)
                    k.op("dve", "tensor_scalar", out=m1.v, in0=a, scalar1=PI, scalar2=-2.0 * PI, op0=ALU.is_ge, op1=ALU.mult)
                    k.op("dve", "tensor_tensor", out=a, in0=a, in1=m1.v, op=ALU.add)
                    k.op("dve", "tensor_scalar", out=m1.v, in0=a, scalar1=-PI, scalar2=2.0 * PI, op0=ALU.is_le, op1=ALU.mult)
                    k.op("dve", "tensor_tensor", out=a, in0=a, in1=m1.v, op=ALU.add)
                    k.op("act", "activation", out=a, in_=a, func=AF.Sin)

            sin_layer(w1.v, featsT, 0, 2, hid1)
            sin_layer(w2.v, hid1, 1, 3, hid2)
        frot = Rot([k.sb("hF", [128, nblk, 128]) for _ in range(2)])
        grot = Rot([k.sb("hG", [128, NB]) for _ in range(3)])
        tmpc = Rot([k.sb("htc", [128, 128]) for _ in range(2)])

        def fwd_dft(x_tm, ncol, sink):
            for fc in range(NFC):
                for part in range(2):
                    F = frot.next()
                    k.dma(F.v.re("p b f -> p (b f)"), FCS[part, fc])
                    ps = k.ps()
                    for blk in range(nblk):
                        k.op("pe", "matmul", out=ps[:, 0:ncol], lhsT=F[:, blk, :], rhs=x_tm[:, blk, 0:ncol],
                             start=(blk == 0), stop=(blk == nblk - 1))
                    sink(part, fc, ps)

        for chh in range(2):
            with k.scope():
                Cre = [k.sb("Cre%d" % o, [128, NFC, 128]) for o in range(2)]
                Cim = [k.sb("Cim%d" % o, [128, NFC, 128]) for o in range(2)]
                with k.scope():
                    iota_n = k.sb("iota_n", [128, T]); k.dma(iota_n, E["iota_n"][:, 0:T])
                    ed = k.sb("hed", [128, T])
                    junk = k.sb("hjunk", [128, T])
                    hfb = [k.sb("hfb%d" % i, [128, T]) for i in range(2)]
                    asum = k.sb("asum", [128, 2])
                    f_tm = k.sb("f_tm", [128, nblk, 256])
                    for o in range(2):
                        for di in range(2):
                            c = o * 4 + di * 2 + chh
                            k.op("act", "activation", out=ed.v, in_=iota_n.v, func=AF.Exp, scale=decT[:, c:c + 1])
                            for n0 in range(0, T, NB):
                                ps = k.ps()
                                k.op("pe", "matmul", out=ps[:, 0:NB], lhsT=w3[:, c * 128:(c + 1) * 128], rhs=hid2[:, n0:n0 + NB],
                                     start=True, stop=True)
                                k.op("dve", "tensor_tensor", out=hfb[di][:, n0:n0 + NB], in0=ps[:, 0:NB], in1=ed[:, n0:n0 + NB],
                                     op=ALU.mult)
                            k.op("dve", "tensor_scalar", out=junk.v, in0=hfb[di].v, scalar1=-1.0, scalar2=None, op0=ALU.mult)
                            k.op("dve", "tensor_tensor", out=junk.v, in0=junk.v, in1=hfb[di].v, op=ALU.max)
                            k.op("dve", "reduce_sum", out=asum[:, di:di + 1], in_=junk.v, axis=AX.X)
                        k.op("dve", "tensor_tensor", out=asum[:, 0:1], in0=asum[:, 0:1], in1=asum[:, 1:2], op=ALU.add)
                        k.op("dve", "reciprocal", out=asum[:, 0:1], in_=asum[:, 0:1])
                        for di in range(2):
                            k.op("dve", "tensor_scalar", out=hfb[di].v, in0=hfb[di].v, scalar1=asum[:, 0:1], scalar2=None, op0=ALU.mult)
                        k.op("pool", "memset", ap=hfb[1][:, 0:1], constant=0.0)
                        for blk in range(nblk):
                            ps = k.ps()
                            for di in range(2):
                                k.op("pe", "transpose", out=ps[:, di * 128:(di + 1) * 128], in_=hfb[di][:, blk * 128:(blk + 1) * 128],
                                     identity=C["ident"].v)
                            k.op("act", "copy", out=f_tm[:, blk, :], in_=ps[:, 0:256])

                        def sink_f(part, fc, ps, o=o):
                            tc_ = tmpc.next()
                            k.op("act", "copy", out=tc_.v, in_=ps[:, 0:128])
                            if part == 0:
                                k.op("dve", "tensor_tensor", out=Cre[o][:, fc, :], in0=ps[:, 128:256], in1=tc_.v, op=ALU.add)
                            else:
                                k.op("dve", "tensor_tensor", out=Cim[o][:, fc, :], in0=ps[:, 128:256], in1=tc_.v, op=ALU.subtract)

                        fwd_dft(f_tm, 256, sink_f)
                hx = [k.sb("hx%d" % i, [128, T]) for i in range(3)]
                Xc, Xs, tA, tB = (k.sb(nm, [128, NFC, 128]) for nm in ("hXc", "hXs", "htA", "htB"))
                cv = k.sb("hcv", [128, T])
                zt = cv
                u_tm = cv.v.re("p (b c) -> p b c", c=128)
                for (s0, T_) in seqs:
                    for part in range(3):
                        cix = part * 2 + chh
                        k.dma(zt, zT_d[z0 + part * 256 + chh * 128:z0 + part * 256 + (chh + 1) * 128, s0:s0 + T])
                        h = hx[part]
                        k.op("dve", "tensor_scalar", out=h.v, in0=zt.v, scalar1=hcT[:, 6 + cix:7 + cix], scalar2=None, op0=ALU.mult)
                        k.op("dve", "scalar_tensor_tensor", out=h[:, 1:T], in0=zt[:, 0:T - 1], scalar=hcT[:, cix:cix + 1],
                             in1=h[:, 1:T], op0=ALU.mult, op1=ALU.add)
                        k.op("dve", "scalar_tensor_tensor", out=h[:, 0:T - 1], in0=zt[:, 1:T], scalar=hcT[:, 12 + cix:13 + cix],
                             in1=h[:, 0:T - 1], op0=ALU.mult, op1=ALU.add)
                    cur = hx[0]
                    for o in range(2):
                        for blk in range(nblk):
                            ps = k.ps()
                            k.op("pe", "transpose", out=ps[:, 0:128], in_=cur[:, blk * 128:(blk + 1) * 128], identity=C["ident"].v)
                            k.op("act", "copy", out=u_tm[:, blk, :], in_=ps[:, 0:128])

                        def sink_x(part, fc, ps):
                            dst = Xc if part == 0 else Xs
                            if fc % 2:
                                k.op("act", "copy", out=dst[:, fc, :], in_=ps[:, 0:128])
                            else:
                                k.op("dve", "tensor_copy", out=dst[:, fc, :], in_=ps[:, 0:128])

                        fwd_dft(u_tm, 128, sink_x)
                        k.op("dve", "tensor_tensor", out=tA.v, in0=Xs.v, in1=Cim[o].v, op=ALU.mult)
                        k.op("pool", "tensor_tensor", out=tB.v, in0=Xs.v, in1=Cre[o].v, op=ALU.mult)
                        k.op("dve", "tensor_tensor", out=Xs.v, in0=Xc.v, in1=Cim[o].v, op=ALU.mult)
                        k.op("dve", "tensor_tensor", out=Xs.v, in0=Xs.v, in1=tB.v, op=ALU.subtract)
                        k.op("pool", "tensor_tensor", out=Xc.v, in0=Xc.v, in1=Cre[o].v, op=ALU.mult)
                        k.op("dve", "tensor_tensor", out=Xc.v, in0=Xc.v, in1=tA.v, op=ALU.add)
                        for tb in range(T // NB):
                            ps = k.ps()
                            n = 0
                            for fc in range(NFC):
                                for part in range(2):
                                    G = grot.next()
                                    k.dma(G, GCS[part, fc][:, tb * NB:(tb + 1) * NB])
                                    k.op("pe", "matmul", out=ps[:, 0:NB], lhsT=(Xc if part == 0 else Xs)[:, fc, :], rhs=G.v,
                                         start=(n == 0), stop=(n == 2 * NFC - 1))
                                    n += 1
                            k.op("dve", "scalar_tensor_tensor", out=cv[:, tb * NB:(tb + 1) * NB], in0=cur[:, tb * NB:(tb + 1) * NB],
                                 scalar=skT[:, o * 2 + chh:o * 2 + chh + 1], in1=ps[:, 0:NB], op0=ALU.mult, op1=ALU.add)
                        k.op("dve", "tensor_tensor", out=hx[o + 1].v, in0=hx[o + 1].v, in1=cv.v, op=ALU.mult)
                        cur = hx[o + 1]
                    k.dma(yb_d[256 + chh * 128:256 + (chh + 1) * 128, s0:s0 + T], hx[2].v)


def stage_rw(k, E, C, g, l, zT_d, yb_d):
    TG, seqs = g["TG"], g["seqs"]
    is_s = g["name"] == "s"
    T = seqs[0][1]
    CH = 64
    nch = T // CH
    G = min(8, nch)
    BW_ = G * CH
    nbat = nch // G
    NB = min(512, T)
    with k.scope():
        muT = load_T(k, C, E["rw_mu"][l].re("w (c p) -> (w c) p", p=128), 14, "muT")
        mid = k.sb("rwmid", [128, 7])
        k.op("dve", "tensor_tensor", out=mid.v, in0=muT[:, 0:7], in1=muT[:, 7:14], op=ALU.add)
        k.op("dve", "tensor_scalar", out=mid.v, in0=mid.v, scalar1=-1.0, scalar2=1.0, op0=ALU.mult, op1=ALU.add)
        w0T = load_T(k, C, E["rw_w0"][l].re("d (c p) -> (d c) p", p=128), 4, "w0T")
        a0T = load_T(k, C, E["rw_a0"][l:l + 1, :].re("o (c p) -> (o c) p", p=128), 2, "a0T")
        kvT = load_T(k, C, E["rw_kvec"][l].re("j (c p) -> (j c) p", p=128), 6, "kvT")
        lnT = load_T(k, C, E["rw_ln"][l].re("j (c p) -> (j c) p", p=128), 4, "lnT")
        omka = k.sb("omka", [128, 2])
        k.op("dve", "tensor_scalar", out=omka.v, in0=kvT[:, 2:4], scalar1=-1.0, scalar2=1.0, op0=ALU.mult, op1=ALU.add)
        lora = k.sb("lora", [128, 4, 256])
        k.dma(lora[0:32, 0, :], E["rw_w_up"][l, 0]); k.dma(lora[0:32, 1, :], E["rw_w_up"][l, 1])
        k.dma(lora[32:64, 2, :], E["rw_a_up"][l]); k.dma(lora[64:128, 3, :], E["rw_g_up"][l])
        cm = zt_alias = None
        MK = {}
        for nm in ("m_lt", "m_gt", "m_le", "m_ge", "eye8"):
            MK[nm] = k.sb(nm, [64, 64]); k.dma(MK[nm], E[nm])
        mb = lambda t_: t_.v.re("p (o c) -> p o c", o=1).bc([64, G, 64])
        p3v = lambda ps_: ps_[0:64, 0:G * 64].re("p (g c) -> p g c", g=G)
        MSK = [(MK["m_lt"], MK["m_le"], MK["m_gt"]), (MK["m_gt"], MK["m_ge"], MK["m_lt"])]
        zt = k.sb("rzt", [128, T])
        r_, kmod, v_, kkn, alpha, x6 = (k.sb(nm, [128, T]) for nm in ("rr", "rkmod", "rv", "rkkn", "ralpha", "rx6"))
        logw, lpi, ysum = (k.sb(nm, [128, T]) for nm in ("rlogw", "rlpi", "rysum"))
        tot = k.sb("rtot", [128, nch]); pC = k.sb("rpC", [128, nch])
        bt = {nm: k.sb("rb_" + nm, [128, BW_]) for nm in ("rt", "at", "kt", "kp", "e")}
        t64 = {nm: k.sb("r64_" + nm, [128 if nm == "v" else 64, G, 128]) for nm in ("v", "a", "k")}
        PP = {nm: k.sb("rP_" + nm, [128 if nm in ("BT", "RAT", "RKT") else 64, G, 64])
              for nm in ("Y0", "Y1", "YT0", "YT1", "P", "BT", "RAT", "RKT")}
        for t_ in (t64["v"], PP["BT"], PP["RAT"], PP["RKT"]):
            k.op("pool", "memset", ap=t_.v, constant=0.0)
        Mst = [[k.sb("rM%d%d" % (hh, i), [128, 128]) for i in range(2)] for hh in range(2)]
        Upad = [k.sb("rUp%d" % hh, [128, 128]) for hh in range(2)]
        for hh in range(2):
            k.op("pool", "memset", ap=Upad[hh].v, constant=0.0)
        g1r = Rot([k.sb("rg1", [64, 64]) for _ in range(2)])
        u_r = Rot([k.sb("ru", [64, 64]) for _ in range(2)])
        tmr = Rot([k.sb("rtm", [128, 64]) for _ in range(2)])
        s0pad = k.sb("rs0", [64, 128])
        sor = Rot([k.sb("rso", [64, 64]) for _ in range(2)])
        assert BW_ == NB
        t5 = Rot([bt["rt"], bt["at"], bt["kt"], bt["kp"]])
        o_rw = E["o_rw"].re("b (l d h v k) -> b l d h v k", l=L, d=2, h=4, v=64)
        c3 = lambda t_: t_.v.re("p (c s) -> p c s", s=CH)

        def shift(dst, row0, cix):
            k.dma(zt, zT_d[O_RW + row0:O_RW + row0 + 128, s0:s0 + T])
            k.op("dve", "tensor_scalar", out=dst.v, in0=zt.v, scalar1=mid[:, cix:cix + 1], scalar2=None, op0=ALU.mult)
            k.op("dve", "scalar_tensor_tensor", out=dst[:, 1:T], in0=zt[:, 0:T - 1], scalar=muT[:, cix:cix + 1],
                 in1=dst[:, 1:T], op0=ALU.mult, op1=ALU.add)
            k.op("dve", "scalar_tensor_tensor", out=dst[:, 0:T - 1], in0=zt[:, 1:T], scalar=muT[:, 7 + cix:8 + cix],
                 in1=dst[:, 0:T - 1], op0=ALU.mult, op1=ALU.add)

        for sj, (s0, T_) in enumerate(seqs):
            shift(x6, 768, 6)
            k.op("act", "activation", out=x6[0:32, :], in_=x6[0:32, :], func=AF.Sigmoid, scale=2.0)
            k.op("dve", "tensor_scalar", out=x6[0:32, :], in0=x6[0:32, :], scalar1=2.0, scalar2=-1.0, op0=ALU.mult, op1=ALU.add)
            k.op("act", "activation", out=x6[64:128, :], in_=x6[64:128, :], func=AF.Sigmoid)
            for hp in range(2):
                shift(r_, hp * 128, hp)
                shift(kmod, 256 + hp * 128, 2 + hp)
                shift(v_, 512 + hp * 128, 4 + hp)
                for n0 in range(0, T, NB):
                    ps = k.ps()
                    k.op("pe", "matmul", out=ps[:, 0:NB], lhsT=lora[32:64, 2, hp * 128:(hp + 1) * 128],
                         rhs=x6[32:64, n0:n0 + NB], start=True, stop=True)
                    k.op("act", "activation", out=alpha[:, n0:n0 + NB], in_=ps[:, 0:NB], func=AF.Sigmoid,
                         bias=a0T[:, hp:hp + 1], scale=1.0)
                k.op("dve", "tensor_scalar", out=kkn.v, in0=kmod.v, scalar1=kvT[:, 0 + hp:1 + hp], scalar2=None, op0=ALU.mult)
                for n0 in range(0, T, NB):
                    for hh in range(2):
                        sl = slice(hh * 64, hh * 64 + 64)
                        sq, rs = t5.next(), t5.next()
                        k.op("act", "activation", out=sq[sl, :], in_=kkn[sl, n0:n0 + NB], func=AF.Square)
                        pm = k.ps()
                        k.op("pe", "matmul", out=pm[:, 0:NB], lhsT=C["ones"][sl, :], rhs=sq[sl, :], start=True, stop=True)
                        k.op("dve", "tensor_scalar", out=rs[sl, :], in0=pm[sl, 0:NB], scalar1=1e-24, scalar2=None, op0=ALU.max)
                        k.op("act", "activation", out=rs[sl, :], in_=rs[sl, :], func=AF.Ln)
                        k.op("act", "activation", out=rs[sl, :], in_=rs[sl, :], func=AF.Exp, scale=-0.5)
                        k.op("dve", "tensor_tensor", out=kkn[sl, n0:n0 + NB], in0=kkn[sl, n0:n0 + NB], in1=rs[sl, :], op=ALU.mult)
                k.op("dve", "tensor_scalar", out=zt.v, in0=alpha.v, scalar1=kvT[:, 2 + hp:3 + hp], scalar2=omka[:, hp:hp + 1],
                     op0=ALU.mult, op1=ALU.add)
                k.op("dve", "tensor_tensor", out=kmod.v, in0=kmod.v, in1=zt.v, op=ALU.mult)
                k.op("dve", "tensor_tensor", out=alpha.v, in0=alpha.v, in1=kkn.v, op=ALU.mult)
                for d in range(2):
                    if RW_STOP[0] <= 1:
                        break
                    mT_s, mT_i, m_s = MSK[d]
                    for n0 in range(0, T, NB):
                        ps = k.ps()
                        k.op("pe", "matmul", out=ps[:, 0:NB], lhsT=lora[0:32, d, hp * 128:(hp + 1) * 128],
                             rhs=x6[0:32, n0:n0 + NB], start=True, stop=True)
                        k.op("act", "activation", out=logw[:, n0:n0 + NB], in_=ps[:, 0:NB], func=AF.Sigmoid,
                             bias=w0T[:, d * 2 + hp:d * 2 + hp + 1], scale=1.0)
                    k.op("dve", "tensor_scalar", out=logw.v, in0=logw.v, scalar1=-0.6065306597126334, scalar2=None, op0=ALU.mult)
                    k.dma(zt, E["cmask64"][:, 0:T])
                    k.op("dve", "tensor_tensor_scan", out=lpi.v, data0=zt.v, data1=logw.v, initial=0.0, op0=ALU.mult, op1=ALU.add)
                    k.op("dve", "tensor_copy", out=tot.v, in_=c3(lpi)[:, :, CH - 1])
                    if d == 1:
                        k.op("dve", "tensor_tensor", out=c3(lpi), in0=tot.v.re("p (c o) -> p c o", o=1).bc([128, nch, CH]),
                             in1=c3(lpi), op=ALU.subtract)
                        k.op("dve", "tensor_tensor", out=lpi.v, in0=lpi.v, in1=logw.v, op=ALU.add)
                    k.op("act", "activation", out=pC.v, in_=tot.v, func=AF.Exp)
                    cur = [0, 0]
                    for hh in range(2):
                        P0 = hh * 64
                        for i in range(2):
                            k.op("pool", "memset", ap=Mst[hh][i].v, constant=0.0)
                        if is_s:
                            k.op("pool", "memset", ap=s0pad.v, constant=0.0)
                            k.dma(s0pad[:, P0:P0 + 64], E["st_rw"][l, d, hp * 2 + hh])
                            ps = k.ps()
                            k.op("pe", "matmul", out=ps[:, 0:64], lhsT=s0pad.v, rhs=C["ident"][0:64, 0:64], start=True, stop=True)
                            k.op("dve", "tensor_copy", out=Mst[hh][0][P0:P0 + 64, P0:P0 + 64], in_=ps[P0:P0 + 64, 0:64])
                    bats = range(nbat) if d == 0 else range(nbat - 1, -1, -1)
                    if RW_STOP[0] <= 2:
                        bats = []
                    for b in bats:
                        bc_ = slice(b * BW_, (b + 1) * BW_)
                        e = bt["e"]
                        k.op("act", "activation", out=e.v, in_=lpi[:, bc_], func=AF.Exp)
                        k.op("dve", "tensor_tensor", out=bt["rt"].v, in0=r_[:, bc_], in1=e.v, op=ALU.mult)
                        k.op("act", "activation", out=e.v, in_=lpi[:, bc_], func=AF.Exp, scale=-1.0)
                        k.op("dve", "tensor_tensor", out=bt["at"].v, in0=alpha[:, bc_], in1=e.v, op=ALU.mult)
                        k.op("dve", "tensor_tensor", out=bt["kt"].v, in0=kmod[:, bc_], in1=e.v, op=ALU.mult)
                        k.op("dve", "tensor_tensor", out=e.v, in0=lpi[:, bc_], in1=logw[:, bc_], op=ALU.subtract)
                        k.op("act", "activation", out=e.v, in_=e.v, func=AF.Exp)
                        k.op("dve", "tensor_tensor", out=bt["kp"].v, in0=kkn[:, bc_], in1=e.v, op=ALU.mult)
                        for nm, src, off in (("v", v_, b * BW_), ("a", bt["at"], 0), ("k", bt["kt"], 0)):
                            for q0 in range(0, G, 4):
                                ps = k.ps()
                                for qq in range(min(4, G - q0)):
                                    gq = q0 + qq
                                    k.op("pe", "transpose", out=ps[0:64, qq * 128:(qq + 1) * 128],
                                         in_=src[:, off + gq * CH:off + (gq + 1) * CH], identity=C["ident"].v)
                                nq = min(4, G - q0)
                                k.op("act", "copy", out=t64[nm][0:64, q0:q0 + nq, :],
                                     in_=ps[0:64, 0:nq * 128].re("p (q c) -> p q c", q=nq))
                        for hh in range(2):
                            if RW_STOP[0] <= 3:
                                break
                            P0 = hh * 64
                            sl = slice(P0, P0 + 64)
                            GW = G * 64
                            banks = {nm: k.ps() for nm in ("AT", "RAT", "BT", "RKT", "A")}
                            for gq in range(G):
                                cc = slice(gq * CH, (gq + 1) * CH)
                                oc = slice(gq * 64, (gq + 1) * 64)
                                k.op("pe", "matmul", out=banks["AT"][0:64, oc], lhsT=bt["at"][sl, cc], rhs=bt["kp"][sl, cc], start=True, stop=True)
                                k.op("pe", "matmul", out=banks["RAT"][0:64, oc], lhsT=bt["at"][sl, cc], rhs=bt["rt"][sl, cc], start=True, stop=True)
                                k.op("pe", "matmul", out=banks["BT"][0:64, oc], lhsT=bt["kt"][sl, cc], rhs=bt["kp"][sl, cc], start=True, stop=True)
                                k.op("pe", "matmul", out=banks["RKT"][0:64, oc], lhsT=bt["kt"][sl, cc], rhs=bt["rt"][sl, cc], start=True, stop=True)
                                k.op("pe", "matmul", out=banks["A"][0:64, oc], lhsT=bt["kp"][sl, cc], rhs=bt["at"][sl, cc], start=True, stop=True)
                            f2 = lambda t_: t_.v.re("p g c -> p (g c)")
                            Y, YT, Yn, YTn, P = PP["Y0"], PP["YT0"], PP["Y1"], PP["YT1"], PP["P"]
                            k.op("dve", "tensor_tensor", out=Y.v, in0=p3v(banks["AT"]), in1=mb(mT_s), op=ALU.mult)
                            k.op("dve", "tensor_tensor", out=YT.v, in0=p3v(banks["A"]), in1=mb(m_s), op=ALU.mult)
                            k.op("dve", "tensor_tensor", out=PP["RAT"][0:64], in0=p3v(banks["RAT"]), in1=mb(mT_i), op=ALU.mult)
                            k.op("dve", "tensor_tensor", out=PP["BT"][0:64], in0=p3v(banks["BT"]), in1=mb(mT_s), op=ALU.mult)
                            k.op("dve", "tensor_tensor", out=PP["RKT"][0:64], in0=p3v(banks["RKT"]), in1=mb(mT_i), op=ALU.mult)
                            k.op("dve", "tensor_tensor", out=P.v, in0=mb(MK["eye8"]), in1=Y.v, op=ALU.subtract)
                            for lev in range(6 - 1):
                                p1, p2 = k.ps(), k.ps()
                                for gq in range(G):
                                    oc = slice(gq * 64, (gq + 1) * 64)
                                    k.op("pe", "matmul", out=p1[0:64, oc], lhsT=YT[:, gq, :], rhs=Y[:, gq, :], start=True, stop=True)
                                    k.op("pe", "matmul", out=p2[0:64, oc], lhsT=Y[:, gq, :], rhs=YT[:, gq, :], start=True, stop=True)
                                k.op("act", "copy", out=f2(Yn), in_=p1[0:64, 0:GW])
                                k.op("dve", "tensor_copy", out=f2(YTn), in_=p2[0:64, 0:GW])
                                p3 = k.ps()
                                for gq in range(G):
                                    oc = slice(gq * 64, (gq + 1) * 64)
                                    k.op("pe", "matmul", out=p3[0:64, oc], lhsT=YTn[:, gq, :], rhs=P[:, gq, :], start=True, stop=True)
                                k.op("dve", "tensor_tensor", out=f2(P), in0=f2(P), in1=p3[0:64, 0:GW], op=ALU.add)
                                Y, YT, Yn, YTn = Yn, YTn, Y, YT
                            chs = range(G) if d == 0 else range(G - 1, -1, -1)
                            if RW_STOP[0] <= 4:
                                chs = []
                            for gq in chs:
                                ci = b * G + gq
                                cc = slice(gq * CH, (gq + 1) * CH)
                                gcol = slice(b * BW_ + gq * CH, b * BW_ + (gq + 1) * CH)
                                Mc, Mn = Mst[hh][cur[hh]], Mst[hh][1 - cur[hh]]
                                kr = slice(0, 64) if hh == 0 else slice(0, 128)
                                ps1 = k.ps()
                                k.op("pe", "matmul", out=ps1[0:64, 0:64], lhsT=bt["kp"][sl, cc], rhs=Mc[sl, P0:P0 + 64], start=True, stop=False)
                                k.op("pe", "matmul", out=ps1[0:64, 0:64], lhsT=PP["BT"][kr, gq, :], rhs=t64["v"][kr, gq, P0:P0 + 64],
                                     start=False, stop=True)
                                g1 = g1r.next()
                                k.op("act", "mul", out=g1.v, in_=ps1[0:64, 0:64], mul=-1.0)
                                if RW_STOP[0] <= 5:
                                    continue
                                ps2 = k.ps()
                                lh = P
                                if RW_VAR[0] == 5:
                                    lh = PP["RKT"]
                                if RW_VAR[0] == 6:
                                    k.op("dve", "tensor_copy", out=PP["Y0"].v, in_=P.v)
                                    lh = PP["Y0"]
                                k.op("pe", "matmul", out=(ps2[0:64, 64:128] if RW_VAR[0] == 7 else ps2[0:64, 0:64]), lhsT=lh[:, gq, :],
                                     rhs=(PP["RAT"][:, gq, :] if RW_VAR[0] == 3 else g1.v), start=True, stop=True)
                                if RW_VAR[0] == 4:
                                    continue
                                u = u_r.next()
                                if RW_VAR[0] == 2:
                                    k.op("dve", "tensor_copy", out=u.v, in_=ps2[0:64, 0:64])
                                else:
                                    k.op("act", "copy", out=u.v, in_=ps2[0:64, 0:64])
                                if RW_VAR[0] != 1:
                                    k.op("dve", "tensor_copy", out=Upad[hh][0:64, P0:P0 + 64], in_=u.v)
                                if RW_STOP[0] <= 6:
                                    continue
                                psy = k.ps()
                                k.op("pe", "matmul", out=psy[:, 0:64], lhsT=Mc[sl, :], rhs=bt["rt"][sl, cc], start=True, stop=False)
                                k.op("pe", "matmul", out=psy[:, 0:64], lhsT=Upad[hh][kr, :], rhs=PP["RAT"][kr, gq, :], start=False, stop=False)
                                k.op("pe", "matmul", out=psy[:, 0:64], lhsT=t64["v"][kr, gq, :], rhs=PP["RKT"][kr, gq, :], start=False, stop=True)
                                if d == 0:
                                    k.op("act", "copy", out=ysum[sl, gcol], in_=psy[sl, 0:64])
                                else:
                                    k.op("dve", "tensor_tensor", out=ysum[sl, gcol], in0=ysum[sl, gcol], in1=psy[sl, 0:64], op=ALU.add)
                                if RW_STOP[0] <= 7:
                                    continue
                                psm = k.ps()
                                k.op("pe", "matmul", out=psm[:, 0:64], lhsT=t64["a"][:, gq, :], rhs=u.v, start=True, stop=False)
                                k.op("pe", "matmul", out=psm[:, 0:64], lhsT=t64["k"][:, gq, :], rhs=t64["v"][0:64, gq, P0:P0 + 64],
                                     start=False, stop=True)
                                tm = tmr.next()
                                k.op("dve", "tensor_tensor", out=tm[sl, :], in0=psm[sl, 0:64], in1=Mc[sl, P0:P0 + 64], op=ALU.add)
                                k.op("dve", "tensor_scalar", out=Mn[sl, P0:P0 + 64], in0=tm[sl, :], scalar1=pC[sl, ci:ci + 1],
                                     scalar2=None, op0=ALU.mult)
                                cur[hh] = 1 - cur[hh]
                    if not is_s:
                        for hh in range(2):
                            P0 = hh * 64
                            sl = slice(P0, P0 + 64)
                            ps = k.ps()
                            k.op("pe", "matmul", out=ps[0:64, 0:64], lhsT=Mst[hh][cur[hh]][sl, P0:P0 + 64], rhs=C["ident"][sl, sl],
                                 start=True, stop=True)
                            so = sor.next()
                            k.op("act", "copy", out=so.v, in_=ps[0:64, 0:64])
                            k.dma(o_rw[sj, l, d, hp * 2 + hh], so.v)
                for n0 in range(0, T, NB):
                    cs = slice(n0, n0 + NB)
                    pg = k.ps()
                    k.op("pe", "matmul", out=pg[:, 0:NB], lhsT=lora[64:128, 3, hp * 128:(hp + 1) * 128], rhs=x6[64:128, cs],
                         start=True, stop=True)
                    for hh in range(2):
                        sl = slice(hh * 64, hh * 64 + 64)
                        cen, sq, rs, bo = (t5.next() for _ in range(4))
                        pm = k.ps()
                        k.op("pe", "matmul", out=pm[:, 0:NB], lhsT=C["ones"][sl, :], rhs=ysum[sl, cs], start=True, stop=True)
                        k.op("dve", "scalar_tensor_tensor", out=cen[sl, :], in0=pm[sl, 0:NB], scalar=-1.0 / 64, in1=ysum[sl, cs],
                             op0=ALU.mult, op1=ALU.add)
                        k.op("act", "activation", out=sq[sl, :], in_=cen[sl, :], func=AF.Square)
                        pv = k.ps()
                        k.op("pe", "matmul", out=pv[:, 0:NB], lhsT=C["ones"][sl, :], rhs=sq[sl, :], start=True, stop=True)
                        k.op("dve", "tensor_scalar", out=rs[sl, :], in0=pv[sl, 0:NB], scalar1=1.0 / 64, scalar2=64e-5,
                             op0=ALU.mult, op1=ALU.add)
                        k.op("act", "activation", out=rs[sl, :], in_=rs[sl, :], func=AF.Ln)
                        k.op("act", "activation", out=rs[sl, :], in_=rs[sl, :], func=AF.Exp, scale=-0.5)
                        k.op("dve", "tensor_tensor", out=cen[sl, :], in0=cen[sl, :], in1=rs[sl, :], op=ALU.mult)
                        k.op("dve", "tensor_scalar", out=cen[sl, :], in0=cen[sl, :], scalar1=lnT[sl, hp:hp + 1],
                             scalar2=lnT[sl, 2 + hp:3 + hp], op0=ALU.mult, op1=ALU.add)
                        k.op("dve", "scalar_tensor_tensor", out=bo[sl, :], in0=r_[sl, cs], scalar=kvT[sl, 4 + hp:5 + hp],
                             in1=kmod[sl, cs], op0=ALU.mult, op1=ALU.mult)
                        pb = k.ps()
                        k.op("pe", "matmul", out=pb[:, 0:NB], lhsT=C["ones"][sl, :], rhs=bo[sl, :], start=True, stop=True)
                        k.op("dve", "tensor_tensor", out=bo[sl, :], in0=pb[sl, 0:NB], in1=v_[sl, cs], op=ALU.mult)
                        k.op("dve", "tensor_tensor", out=cen[sl, :], in0=cen[sl, :], in1=bo[sl, :], op=ALU.add)
                        k.op("dve", "tensor_tensor", out=zt[sl, cs], in0=cen[sl, :], in1=pg[sl, 0:NB], op=ALU.mult)
                k.dma(yb_d[hp * 128:(hp + 1) * 128, s0:s0 + T], zt[:, 0:T])


def stage_merge(k, E, C, g, l, x, mod_d, zT_d, yb_d):
    NT, TG = g["NT"], g["TG"]
    TB = 256
    with k.scope():
        g1b = bc_param(k, "g1b", mod_d[l, g["cond"]:g["cond"] + 1, 2 * D:3 * D])
        brw = k.sb("brw", [128, 8, D])
        k.dma(brw, E["br_w"][l].re("b (kc p) n -> p (b kc) n", p=128))
        wo = k.sb("wo", [128, 8, D])
        k.dma(wo, E["w_out"][l].re("(kc p) n -> p kc n", p=128))
        ybr = Rot([k.sb("ybb", [128, 8, TB]) for _ in range(2)])
        mTr = Rot([k.sb("mT", [128, 8, TB]) for _ in range(2)])
        gtr = Rot([k.sb("gt", [128, TB]) for _ in range(3)])
        tmr = Rot([k.sb("tm", [128, TB]) for _ in range(2)])
        tor = Rot([k.sb("to", [128, 512]) for _ in range(2)])
        for t0 in range(0, TG, TB):
            yb = ybr.next()
            k.dma(yb, yb_d[:, t0:t0 + TB].re("(c p) t -> p c t", p=128))
            mT = mTr.next()
            for nch in range(8):
                for b in range(4):
                    gt = gtr.next()
                    r0 = O_MG + b * D + nch * 128
                    k.dma(gt, zT_d[r0:r0 + 128, t0:t0 + TB])
                    ps = k.ps()
                    for kc2 in range(2):
                        k.op("pe", "matmul", out=ps[:, 0:TB], lhsT=brw[:, 2 * b + kc2, nch * 128:(nch + 1) * 128],
                             rhs=yb[:, 2 * b + kc2, :], start=(kc2 == 0), stop=(kc2 == 1))
                    if b == 0:
                        k.op("dve", "tensor_tensor", out=mT[:, nch, :], in0=ps[:, 0:TB], in1=gt.v, op=ALU.mult)
                    else:
                        tm = tmr.next()
                        k.op("dve", "tensor_tensor", out=tm.v, in0=ps[:, 0:TB], in1=gt.v, op=ALU.mult)
                        k.op("pool", "tensor_tensor", out=mT[:, nch, :], in0=mT[:, nch, :], in1=tm.v, op=ALU.add)
            for ts in range(TB // 128):
                i = (t0 // 128) + ts
                for eh in range(2):
                    ps = k.ps()
                    for nch in range(8):
                        k.op("pe", "matmul", out=ps.v, lhsT=mT[:, nch, ts * 128:(ts + 1) * 128],
                             rhs=wo[:, nch, eh * 512:(eh + 1) * 512], start=(nch == 0), stop=(nch == 7))
                    to = tor.next()
                    k.op("dve", "tensor_tensor", out=to.v, in0=ps.v, in1=g1b[:, eh * 512:(eh + 1) * 512], op=ALU.mult)
                    k.op("pool", "tensor_tensor", out=x[:, i, eh * 512:(eh + 1) * 512],
                         in0=x[:, i, eh * 512:(eh + 1) * 512], in1=to.v, op=ALU.add)


def stage_moe(k, E, C, g, l, x, mod_d):
    NT, TG = g["NT"], g["TG"]
    seqs = g["seqs"]
    caps = [2 * T // NE for (_, T) in seqs]
    NS = sum(caps)
    slot0 = [sum(caps[:j]) for j in range(len(seqs))]
    with k.scope():
        h2bf = k.sb("h2bf", [128, NT, D], BF16)
        aff = k.sb("aff", [128, NT, NE])
        rsel = k.sb("rsel", [128, NT, NE])
        g2b = bc_param(k, "g2b", mod_d[l, g["cond"]:g["cond"] + 1, 5 * D:6 * D])
        with k.scope():
            A, B = mod_params(k, E, mod_d, l, g["cond"], 1, "norm2_g")
            nm = NormMod(k)
            rt = k.sb("rt", [128, 8, NE])
            k.dma(rt, E["router"][l].re("(kc p) e -> p kc e", p=128))
            htm = Rot([k.sb("htm", [128, D]) for _ in range(2)])
            hTt = Rot([k.sb("hTt", [128, 8, 128]) for _ in range(2)])
            sm = Rot([k.sb("sm", [128, 4]) for _ in range(2)])
            affT = k.sb("affT", [NE, TG])
            for i in range(NT):
                h = htm.next()
                nm(x[:, i, :], A, B, h.v)
                k.op("pool", "tensor_copy", out=h2bf[:, i, :], in_=h.v, free=True)
                hT = hTt.next()
                to_fm(k, C, h, hT, 0, free=False)
                ps = k.ps()
                for kc in range(8):
                    k.op("pe", "matmul", out=ps[:, 0:NE], lhsT=hT[:, kc, :], rhs=rt[:, kc, :],
                         start=(kc == 0), stop=(kc == 7))
                s = sm.next()
                k.op("dve", "reduce_max", out=s[:, 0:1], in_=ps[:, 0:NE], axis=AX.X)
                k.op("dve", "tensor_scalar", out=s[:, 1:2], in0=s[:, 0:1], scalar1=-1.0, scalar2=None, op0=ALU.mult)
                k.op("act", "activation", out=aff[:, i, :], in_=ps[:, 0:NE], func=AF.Exp, bias=s[:, 1:2], scale=1.0,
                     accum_out=s[:, 2:3])
                k.op("dve", "reciprocal", out=s[:, 3:4], in_=s[:, 2:3])
                k.op("dve", "tensor_scalar", out=aff[:, i, :], in0=aff[:, i, :], scalar1=s[:, 3:4], scalar2=None,
                     op0=ALU.mult)
                ps2 = k.ps()
                k.op("pe", "transpose", out=ps2[0:NE, 0:128], in_=aff[:, i, :], identity=C["ident"].v)
                k.op("act", "copy", out=affT[:, i * 128:(i + 1) * 128], in_=ps2[0:NE, 0:128])
            work = k.sb("work", [NE, TG])
            mx = Rot([k.sb("mx", [NE, 8]) for _ in range(2)])
            k.op("dve", "tensor_copy", out=work.v, in_=affT.v)
            for j, (s0, T) in enumerate(seqs):
                for r in range(caps[j] // 8):
                    m8 = mx.next()
                    k.op("dve", "max", out=m8.v, in_=work[:, s0:s0 + T])
                    k.op("dve", "match_replace", out=work[:, s0:s0 + T], in_to_replace=m8.v,
                         in_values=work[:, s0:s0 + T], imm_value=-1.0)
            maskT = k.sb("maskT", [NE, TG])
            k.op("dve", "tensor_scalar", out=maskT.v, in0=work.v, scalar1=-1.0, scalar2=None, op0=ALU.is_equal)
            mtm = k.sb("mtm", [128, NT, NE])
            for i in range(NT):
                ps = k.ps()
                k.op("pe", "transpose", out=ps[:, 0:NE], in_=maskT[:, i * 128:(i + 1) * 128],
                     identity=C["ident"][0:NE, 0:NE])
                k.op("act", "copy", out=mtm[:, i, :], in_=ps[:, 0:NE])
            for j, (s0, T) in enumerate(seqs):
                i0, n = s0 // 128, T // 128
                for ii in range(n):
                    ps = k.ps()
                    for jj in range(ii + 1):
                        k.op("pe", "matmul", out=ps[:, 0:NE], lhsT=(C["triu"] if jj == ii else C["ones"]).v,
                             rhs=mtm[:, i0 + jj, :], start=(jj == 0), stop=(jj == ii))
                    k.op("dve", "scalar_tensor_tensor", out=rsel[:, i0 + ii, :], in0=ps[:, 0:NE], scalar=float(slot0[j]),
                         in1=mtm[:, i0 + ii, :], op0=ALU.add, op1=ALU.mult)
                    k.op("dve", "tensor_scalar", out=rsel[:, i0 + ii, :], in0=rsel[:, i0 + ii, :], scalar1=-1.0,
                         scalar2=None, op0=ALU.add)
        if False:
            with k.scope():
                dt_ = k.sb("dbgt", [128, 2048])
                k.op("dve", "tensor_copy", out=dt_[:, 0:1024], in_=h2bf[:, 0, :])
                k.op("dve", "tensor_copy", out=dt_[:, 1024:2048], in_=h2bf[:, 1, :])
                k.dma(E["dbg"][:, 0:2048], dt_.v)
                k.dma(E["dbg"][:, 2048:2048 + NT * NE], aff.v.re("p n e -> p (n e)"))
                k.dma(E["dbg"][:, 4096:4096 + NT * NE], rsel.v.re("p n e -> p (n e)"))
        with k.scope():
            w1r = Rot([k.sb("w1", [128, 8, 256]) for _ in range(2)])
            w3r = Rot([k.sb("w3", [128, 8, 256]) for _ in range(2)])
            w2r = Rot([k.sb("w2", [128, D]) for _ in range(2)])
            xeT = k.sb("xeT", [128, 8, NS])
            xe = k.sb("xe", [128, (NS + 127) // 128, D])
            hidT = k.sb("hidT", [128, FF // 128, NS])
            NCC = (NS + 127) // 128
            ye = k.sb("ye", [128, NCC, D])
            selr = Rot([k.sb("sel", [128, 256], BF16) for _ in range(3)])
            sgr = Rot([k.sb("sg", [128, 256]) for _ in range(2)])
            sgTr = Rot([k.sb("sgT", [128, 2, 128]) for _ in range(2)])
            s1r = Rot([k.sb("s1", [128, 256]) for _ in range(2)])
            for e in range(NE):
                pss = [[k.ps() for _ in range(2)] for _ in range(NCC)]
                for i in range(NT):
                    sel = selr.next()
                    k.op("dve", "tensor_scalar", out=sel[:, 0:NS], in0=C["iota_f"][:, 0:NS],
                         scalar1=rsel[:, i, e:e + 1], scalar2=None, op0=ALU.is_equal)
                    for cc in range(NCC):
                        cw = min(128, NS - cc * 128)
                        for dh in range(2):
                            k.op("pe", "matmul", out=pss[cc][dh][0:cw, :], lhsT=sel[:, cc * 128:cc * 128 + cw],
                                 rhs=h2bf[:, i, dh * 512:(dh + 1) * 512], start=(i == 0), stop=(i == NT - 1))
                for cc in range(NCC):
                    cw = min(128, NS - cc * 128)
                    for dh in range(2):
                        if dh:
                            k.op("act", "copy", out=xe[0:cw, cc, dh * 512:(dh + 1) * 512], in_=pss[cc][dh][0:cw, :])
                        else:
                            k.op("dve", "tensor_copy", out=xe[0:cw, cc, dh * 512:(dh + 1) * 512], in_=pss[cc][dh][0:cw, :])
                for cc in range(NCC):
                    cw = min(128, NS - cc * 128)
                    for j2 in range(2):
                        pt = k.ps()
                        for a in range(4):
                            kc = 4 * j2 + a
                            k.op("pe", "transpose", out=pt[:, a * 128:a * 128 + cw], in_=xe[0:cw, cc, kc * 128:(kc + 1) * 128],
                                 identity=C["ident"][0:cw, 0:cw])
                        src = pt.v.re("p (a t) -> p a t", a=4)[:, :, 0:cw]
                        dst = xeT[:, 4 * j2:4 * j2 + 4, cc * 128:cc * 128 + cw]
                        if j2:
                            k.op("act", "copy", out=dst, in_=src)
                        else:
                            k.op("dve", "tensor_copy", out=dst, in_=src)
                for fc2 in range(FF // 256):
                    w1, w3 = w1r.next(), w3r.next()
                    k.dma(w1, E["ex_w1"][l, e][:, fc2 * 256:(fc2 + 1) * 256].re("(kc p) n -> p kc n", p=128))
                    k.dma(w3, E["ex_w3"][l, e][:, fc2 * 256:(fc2 + 1) * 256].re("(kc p) n -> p kc n", p=128), q="pool")
                    for sub in range(2):
                        fc = fc2 * 2 + sub
                        fs = slice(sub * 128, (sub + 1) * 128)
                        p1, p3 = k.ps(), k.ps()
                        for kc in range(8):
                            k.op("pe", "matmul", out=p1[:, 0:NS], lhsT=w1[:, kc, fs], rhs=xeT[:, kc, :],
                                 start=(kc == 0), stop=(kc == 7))
                        for kc in range(8):
                            k.op("pe", "matmul", out=p3[:, 0:NS], lhsT=w3[:, kc, fs], rhs=xeT[:, kc, :],
                                 start=(kc == 0), stop=(kc == 7))
                        s1 = s1r.next()
                        k.op("act", "activation", out=s1[:, 0:NS], in_=p1[:, 0:NS], func=AF.Sigmoid)
                        k.op("dve", "tensor_tensor", out=s1[:, 0:NS], in0=s1[:, 0:NS], in1=p1[:, 0:NS], op=ALU.mult)
                        k.op("dve", "tensor_tensor", out=hidT[:, fc, :], in0=p3[:, 0:NS], in1=s1[:, 0:NS], op=ALU.mult)
                pacc = [[k.ps() for _ in range(2)] for _ in range(NCC)]
                for fc in range(FF // 128):
                    w2 = w2r.next()
                    k.dma(w2, E["ex_w2"][l, e][fc * 128:(fc + 1) * 128, :])
                    for cc in range(NCC):
                        cw = min(128, NS - cc * 128)
                        for dh in range(2):
                            k.op("pe", "matmul", out=pacc[cc][dh][0:cw, :], lhsT=hidT[:, fc, cc * 128:cc * 128 + cw],
                                 rhs=w2[:, dh * 512:(dh + 1) * 512], start=(fc == 0), stop=(fc == FF // 128 - 1))
                for cc in range(NCC):
                    cw = min(128, NS - cc * 128)
                    for dh in range(2):
                        k.op("dve", "tensor_tensor", out=ye[0:cw, cc, dh * 512:(dh + 1) * 512],
                             in0=pacc[cc][dh][0:cw, :], in1=g2b[0:cw, dh * 512:(dh + 1) * 512], op=ALU.mult)
                for i in range(NT):
                    sg = sgr.next()
                    k.op("dve", "tensor_scalar", out=sg[:, 0:NS], in0=C["iota_f"][:, 0:NS],
                         scalar1=rsel[:, i, e:e + 1], scalar2=aff[:, i, e:e + 1], op0=ALU.is_equal, op1=ALU.mult)
                    sgT = sgTr.next()
                    pst = k.ps()
                    for cc in range(NCC):
                        k.op("pe", "transpose", out=pst[:, cc * 128:(cc + 1) * 128],
                             in_=sg[:, cc * 128:(cc + 1) * 128], identity=C["ident"].v)
                    k.op("act", "copy", out=sgT[:, 0:NCC, :], in_=pst[:, 0:NCC * 128].re("p (c t) -> p c t", c=NCC))
                    for dh in range(2):
                        pso = k.ps()
                        for cc in range(NCC):
                            k.op("pe", "matmul", out=pso.v, lhsT=sgT[:, cc, :],
                                 rhs=ye[:, cc, dh * 512:(dh + 1) * 512],
                                 start=(cc == 0), stop=(cc == NCC - 1))
                        k.op("dve", "tensor_tensor", out=x[:, i, dh * 512:(dh + 1) * 512],
                             in0=x[:, i, dh * 512:(dh + 1) * 512], in1=pso.v, op=ALU.add)


def final_norm(k, x, NT, fg_b, yv):
    nm = NormMod(k)
    op_ = Rot([k.sb("o", [128, D]) for _ in range(2)])
    for i in range(NT):
        o = op_.next()
        nm(x[:, i, :], fg_b, None, o.v)
        k.dma(yv[:, i, :], o.v)


_PROG = None
W_NAMES = ("ada_w", "ada_b", "norm1_g", "norm2_g", "w_in", "br_w", "w_out", "router", "ex_w1", "ex_w3", "ex_w2")


def kernel(**inp):
    global _PROG
    if _PROG is None:
        _PROG = build_program()
    nc = _PROG
    cn = consts_np()
    f32 = lambda a: np.ascontiguousarray(np.asarray(a, dtype=np.float32))
    shared = {n: f32(inp[n][:NLAYERS_RUN]) for n in W_NAMES if (ENABLE["moe"] or not n.startswith("ex_"))}
    shared["final_g"] = f32(inp["final_g"]).reshape(1, D)
    shared["c_ctx"] = f32(inp["c_ctx"]).reshape(1, D)
    shared["ret_rate"] = f32(inp["ret_rate"]).reshape(L, 8)
    shared["ret_gn"] = f32(inp["ret_gn"])
    shared["hg_lb"] = f32(inp["hg_lb"]); shared["hg_norm"] = f32(inp["hg_norm"])
    for nm in ("rw_mu", "rw_w0", "rw_w_up", "rw_a0", "rw_a_up", "rw_g_up", "rw_kvec", "rw_ln", "hy_conv", "hy_ffn1", "hy_ffn1_b", "hy_ffn2", "hy_ffn2_b", "hy_ffn3", "hy_freq", "hy_decay", "hy_skip"):
        shared[nm] = f32(inp[nm])
    shared.update(cn)
    in_maps = []
    for i in range(NC_RUN):
        m = dict(shared)
        m["xs"] = f32(inp["x_sample"][i])
        m["xp"] = f32(inp["x_prompt"][NPR * i:NPR * (i + 1)]).reshape(NPR * TP, D)
        m["c_s"] = f32(inp["c"][i]).reshape(1, D)
        m["st_ret"] = f32(inp["state_ret"][i])
        m["st_hg"] = f32(inp["state_hgrn"][i])
        m["st_rw"] = f32(inp["state_rwkv"][i])
        in_maps.append(m)
    res = run_bass_kernel_spmd(nc, in_maps, core_ids=list(range(NC_RUN)))
    R = res.results
    y_prompt = np.concatenate([R[i]["yp"].reshape(NPR, TP, D) for i in range(NC_RUN)], axis=0)
    y_sample = np.stack([R[i]["ys"] for i in range(NC_RUN)], axis=0)
    sts = []
    for nm in ("o_rw", "o_ret", "o_hg"):
        sts.append(np.concatenate([R[i][nm].reshape(NPR, L, 2, 4, 64, 64) for i in range(NC_RUN)], axis=0))
    return (y_prompt, y_sample, sts[0], sts[1], sts[2])
```

```python
import contextlib
import numpy as np
import concourse.bass as bass
import concourse.mybir as mybir

F32 = mybir.dt.float32
BF16 = mybir.dt.bfloat16
I32 = mybir.dt.int32
AF = mybir.ActivationFunctionType
ALU = mybir.AluOpType
AX = mybir.AxisListType

WRITE_KEYS = ("out", "accum_out", "ap")
N_DMA_SEMS = 96


class Eng:
    def __init__(self, name, h, sem):
        self.name, self.h, self.sem = name, h, sem
        self.cnt = 0
        self.seen = {}


class DSem:
    def __init__(self, sem):
        self.sem = sem
        self.cnt = 0


class Tile:
    def __init__(self, k, handle, space):
        self.k, self.h, self.space = k, handle, space
        self.w = {}
        self.r = {}
        self.dsem = None

    def _ap(self):
        return self.h.ap() if self.space == "dram" else self.h[:]

    @property
    def v(self):
        return View(self, self._ap())

    def __getitem__(self, idx):
        return View(self, self._ap()[idx])


class View:
    def __init__(self, tile, ap):
        self.tile, self.ap = tile, ap

    def __getitem__(self, idx):
        return View(self.tile, self.ap[idx])

    def re(self, pat, **kw):
        return View(self.tile, self.ap.rearrange(pat, **kw))

    def bc(self, shape):
        return View(self.tile, self.ap.to_broadcast(shape))

    @property
    def shape(self):
        return self.ap.shape


class Ext:
    def __init__(self, ap):
        self.ap = ap

    def __getitem__(self, idx):
        return Ext(self.ap[idx])

    def re(self, pat, **kw):
        return Ext(self.ap.rearrange(pat, **kw))

    def bc(self, shape):
        return Ext(self.ap.to_broadcast(shape))


class K:
    def __init__(self, nc):
        self.nc = nc
        self.root = contextlib.ExitStack()
        self.stacks = [self.root]
        self.scope_tiles = [[]]
        self.eng = {}
        for name, h in (("pe", nc.tensor), ("act", nc.scalar), ("dve", nc.vector),
                        ("pool", nc.gpsimd), ("sp", nc.sync)):
            sem = self.root.enter_context(nc.semaphore("sem_" + name))
            self.eng[name] = Eng(name, h, sem)
        self.dsems = [DSem(self.root.enter_context(nc.semaphore("dsem%d" % i))) for i in range(N_DMA_SEMS)]
        self.free_dsems = list(self.dsems)
        self.psums = []
        self.ps_i = 0
        self.n_ins = 0
        self.uid = 0

    def name(self, n):
        self.uid += 1
        return "%s_%d" % (n, self.uid)

    def sb(self, name, shape, dt=F32):
        h = self.stacks[-1].enter_context(self.nc.sbuf_tensor(self.name(name), list(shape), dt))
        t = Tile(self, h, "sb")
        self.scope_tiles[-1].append(t)
        return t

    def dram(self, name, shape, dt=F32):
        h = self.nc.dram_tensor(self.name(name), list(shape), dt, kind="Internal")
        t = Tile(self, h, "dram")
        self.scope_tiles[0].append(t)
        return t

    def init_psum(self, n=8):
        for i in range(n):
            h = self.root.enter_context(self.nc.psum_tensor("ps%d" % i, [128, 512], F32))
            self.psums.append(Tile(self, h, "ps"))

    def ps(self):
        while True:
            t = self.psums[self.ps_i % len(self.psums)]
            self.ps_i += 1
            if not getattr(t, "reserved", False):
                return t

    def ps_reserve(self, n):
        out = []
        for _ in range(n):
            t = self.ps()
            t.reserved = True
            out.append(t)
        return out

    def ps_release(self, tiles):
        for t in tiles:
            t.reserved = False

    @contextlib.contextmanager
    def scope(self):
        st = contextlib.ExitStack()
        self.stacks.append(st)
        self.scope_tiles.append([])
        try:
            with st:
                yield
                self.barrier()
                for t in self.scope_tiles[-1]:
                    if t.dsem is not None:
                        self.free_dsems.append(t.dsem)
                        t.dsem = None
        finally:
            self.stacks.pop()
            self.scope_tiles.pop()

    def _wait(self, e, deps):
        for d in deps:
            if d is None:
                continue
            semobj, val, owner = d
            if owner is e and e.name in ("pe", "sp"):
                continue
            key = id(semobj)
            if e.seen.get(key, 0) < val:
                e.h.wait_ge(semobj.sem, val)
                e.seen[key] = val

    def barrier(self):
        engs = list(self.eng.values())
        for e in engs:
            deps = [(x, x.cnt, x) for x in engs if x is not e and x.cnt > 0]
            deps += [(d, d.cnt, None) for d in self.dsems if d.cnt > 0]
            self._wait(e, deps)

    @staticmethod
    def _merge(d, tag):
        key = id(tag[0])
        if key not in d or d[key][1] < tag[1]:
            d[key] = tag

    def _deps(self, reads, writes, free):
        deps = []
        for t in reads:
            deps.extend(t.w.values())
        for t in writes:
            if not free:
                deps.extend(t.w.values())
                deps.extend(t.r.values())
        return deps

    def _record(self, reads, writes, tag, free):
        for t in writes:
            if free:
                self._merge(t.w, tag)
            else:
                t.w = {id(tag[0]): tag}
                t.r = {}
        for t in reads:
            if t not in writes:
                self._merge(t.r, tag)

    def op(self, engname, meth, R=(), W=(), free=False, **kw):
        e = self.eng[engname]
        reads, writes = list(R), list(W)
        args = {}
        for key, val in kw.items():
            if isinstance(val, Tile):
                val = val.v
            if isinstance(val, View):
                (writes if key in WRITE_KEYS else reads).append(val.tile)
                args[key] = val.ap
            elif isinstance(val, Ext):
                args[key] = val.ap
            else:
                args[key] = val
        self._wait(e, self._deps(reads, writes, free))
        ins = getattr(e.h, meth)(**args)
        e.cnt += 1
        ins.then_inc(e.sem, 1)
        self._record(reads, writes, (e, e.cnt, e), free)
        self.n_ins += 1
        return ins

    def dma(self, out, in_, q="sp", free=False, **kw):
        e = self.eng[q]
        if isinstance(out, Tile):
            out = out.v
        if isinstance(in_, Tile):
            in_ = in_.v
        reads = [in_.tile] if isinstance(in_, View) else []
        writes = [out.tile] if isinstance(out, View) else []
        tracked = None
        if reads:
            tracked = reads[0]
        if writes and (tracked is None or writes[0].space == "sb"):
            tracked = writes[0]
        if tracked is None:
            raise ValueError("dma needs a tracked side")
        self._wait(e, self._deps(reads, writes, free))
        if tracked.dsem is None:
            if not self.free_dsems:
                raise RuntimeError("out of dma sems")
            tracked.dsem = self.free_dsems.pop(0)
        ds = tracked.dsem
        ins = e.h.dma_start(out=out.ap, in_=in_.ap, **kw)
        ds.cnt += 16
        ins.then_inc(ds.sem, 16)
        self._record(reads, writes, (ds, ds.cnt, None), free)
        self.n_ins += 1
        return ins

    def finish(self):
        e = self.eng["sp"]
        deps = [(d, d.cnt, None) for d in self.dsems if d.cnt > 0]
        deps += [(x, x.cnt, x) for x in self.eng.values() if x is not e and x.cnt > 0]
        self._wait(e, deps)
        self.root.close()

from concourse.bass_utils import run_bass_kernel_spmd

D = 1024
L = 4
NLAYERS_RUN = 4
TS = 2048
TP = 256
NPR = 4
NCORES = 8
NC_RUN = 8
EPS = 1e-6
ST_ELEMS = L * 2 * 4 * 64 * 64
NE = 16
FF = 2048
N_IN = 8064
O_RW, O_HY, O_RET, O_HG, O_MG = 0, 896, 896 + 768, 896 + 768 + 1024, 896 + 768 + 1024 + 1280
ENABLE = dict(rw=True, hy=True, ret=True, hg=True, moe=True)
RW_STOP = [9]
PROJ_BF16 = True
RW_VAR = [0]
GROUPS_RUN = ['s', 'p']


def consts_np():
    c = {}
    c["ident"] = np.eye(128, dtype=np.float32)
    c["iota_f"] = np.tile(np.arange(256, dtype=np.float32)[None, :], (128, 1))
    c["triu"] = np.triu(np.ones((128, 128), dtype=np.float32))
    c["ones"] = np.ones((128, 128), dtype=np.float32)
    up = np.arange(2 * TS - 128, dtype=np.float32)[None, :]
    mp = np.arange(128, dtype=np.float32)[:, None]
    c["xtab"] = (up - (TS - 128) - mp).astype(np.float32)
    c["iota_n"] = np.tile(np.arange(TS, dtype=np.float32)[None, :], (128, 1))
    n = np.arange(TS)
    row = (n // 64).astype(np.float32); col = (n % 64).astype(np.float32)
    inv = (10000.0 ** (-np.arange(16, dtype=np.float32) / 16)).astype(np.float32)
    ang = np.concatenate([row[:, None] * inv, col[:, None] * inv], axis=-1).astype(np.float32)
    cosT, sinT = np.cos(ang).T.astype(np.float32), np.sin(ang).T.astype(np.float32)
    c["cos2"] = np.concatenate([cosT, cosT, cosT, cosT], axis=0)
    c["sin2"] = np.concatenate([-sinT, sinT, -sinT, sinT], axis=0)
    pm = np.zeros((128, 128), dtype=np.float32)
    for p in range(128):
        q = (p // 64) * 64 + ((p % 64) + 32) % 64
        pm[q, p] = 1.0
    c["perm"] = pm
    ev = np.zeros((128, 4), dtype=np.float32)
    for blk in range(2):
        m = blk * 128 + np.arange(128)
        ev[:, blk * 2 + 0] = TP - 1 - m
        ev[:, blk * 2 + 1] = m
    c["evals"] = ev
    t = np.arange(TS)
    c["cmask"] = np.tile((t % 32 != 0).astype(np.float32)[None, :], (128, 1))
    a = np.arange(128)
    same = (a[:, None] // 32) == (a[None, :] // 32)
    c["mk_f"] = (same & (a[:, None] <= a[None, :])).astype(np.float32)
    c["mk_b"] = (same & (a[:, None] >= a[None, :])).astype(np.float32)
    for nm, T in (("feats_s", TS), ("feats_p", TP)):
        tt = np.linspace(0.0, 1.0, T, dtype=np.float32)[:, None]
        w = ((2.0 * np.pi / T) * np.arange(T, dtype=np.float32))[:, None].astype(np.float32)
        bands = np.linspace(1e-4, 15.0, 16, dtype=np.float32)[None, :]
        feats = np.concatenate([tt, np.cos(bands * w), -np.sin(bands * w)], axis=-1).astype(np.float32)
        c[nm] = np.ascontiguousarray(feats.T)
    c["negpi"] = np.full((128, 1), -np.pi, dtype=np.float32)
    c["fcs_s"], c["gcs_s"] = dft_consts(TS)
    c["fcs_p"], c["gcs_p"] = dft_consts(TP)
    c["cmask64"] = np.tile((t % 64 != 0).astype(np.float32)[None, :], (128, 1))
    pp = np.arange(64)[:, None]; ff = np.arange(64)[None, :]
    for nm, m in (("m_lt", pp < ff), ("m_gt", pp > ff), ("m_le", pp <= ff), ("m_ge", pp >= ff)):
        c[nm] = np.ascontiguousarray(m.astype(np.float32))
    c["eye8"] = np.eye(64, dtype=np.float32)
    return c


class Rot:
    def __init__(self, tiles):
        self.t, self.i = tiles, 0

    def next(self):
        x = self.t[self.i % len(self.t)]
        self.i += 1
        return x


def build_program():
    nc = bass.Bass("TRN2", target_bir_lowering=False)
    k = K(nc)
    k.init_psum(8)
    E = {}

    def ein(name, shape, dt=F32):
        E[name] = Ext(nc.dram_tensor(name, list(shape), dt, kind="ExternalInput").ap())
        return E[name]

    def eout(name, shape, dt=F32):
        E[name] = Ext(nc.dram_tensor(name, list(shape), dt, kind="ExternalOutput").ap())
        return E[name]

    ein("xs", [TS, D]); ein("xp", [NPR * TP, D])
    ein("c_s", [1, D]); ein("c_ctx", [1, D])
    ein("final_g", [1, D])
    ein("ada_w", [NLAYERS_RUN, D, 6 * D]); ein("ada_b", [NLAYERS_RUN, 6 * D])
    ein("norm1_g", [NLAYERS_RUN, D]); ein("norm2_g", [NLAYERS_RUN, D])
    ein("w_in", [NLAYERS_RUN, D, N_IN])
    ein("br_w", [NLAYERS_RUN, 4, 256, D]); ein("w_out", [NLAYERS_RUN, D, D])
    ein("router", [NLAYERS_RUN, D, NE])
    if ENABLE["moe"]:
        ein("ex_w1", [NLAYERS_RUN, NE, D, FF]); ein("ex_w3", [NLAYERS_RUN, NE, D, FF]); ein("ex_w2", [NLAYERS_RUN, NE, FF, D])
    for nm, shp in (("ident", [128, 128]), ("iota_f", [128, 256]), ("triu", [128, 128]), ("ones", [128, 128])):
        ein(nm, shp)
    ein("st_ret", [L, 2, 4, 64, 64]); ein("ret_rate", [L, 8]); ein("ret_gn", [L, 2, 256])
    for nm, shp in (("xtab", [128, 2 * TS - 128]), ("iota_n", [128, TS]), ("cos2", [128, TS]), ("sin2", [128, TS]),
                    ("perm", [128, 128]), ("evals", [128, 4])):
        ein(nm, shp)
    ein("st_hg", [L, 2, 4, 64, 64]); ein("hg_lb", [L, 2, 256]); ein("hg_norm", [L, 256])
    for nm, shp in (("cmask", [128, TS]), ("mk_f", [128, 128]), ("mk_b", [128, 128])):
        ein(nm, shp)
    for nm, shp in (("hy_conv", [L, 3, 768]), ("hy_ffn1", [L, 33, 64]), ("hy_ffn1_b", [L, 64]), ("hy_ffn2", [L, 64, 64]),
                    ("hy_ffn2_b", [L, 64]), ("hy_ffn3", [L, 64, 1024]), ("hy_freq", [L, 2, 64]), ("hy_decay", [L, 1024]),
                    ("hy_skip", [L, 2, 256]), ("feats_s", [33, TS]), ("feats_p", [33, TP]), ("negpi", [128, 1]),
                    ("fcs_s", [2, TS // 128 + 1, 128, TS]), ("gcs_s", [2, TS // 128 + 1, 128, TS]),
                    ("fcs_p", [2, TP // 128 + 1, 128, TP]), ("gcs_p", [2, TP // 128 + 1, 128, TP])):
        ein(nm, shp)
    for nm, shp in (("st_rw", [L, 2, 4, 64, 64]), ("rw_mu", [L, 2, 896]), ("rw_w0", [L, 2, 256]), ("rw_w_up", [L, 2, 32, 256]),
                    ("rw_a0", [L, 256]), ("rw_a_up", [L, 32, 256]), ("rw_g_up", [L, 64, 256]), ("rw_kvec", [L, 3, 256]),
                    ("rw_ln", [L, 2, 256]), ("cmask64", [128, TS]), ("m_lt", [64, 64]), ("m_gt", [64, 64]),
                    ("m_le", [64, 64]), ("m_ge", [64, 64]), ("eye8", [64, 64])):
        ein(nm, shp)
    eout("ys", [TS, D]); eout("yp", [NPR * TP, D])
    eout("o_rw", [NPR, ST_ELEMS]); eout("o_ret", [NPR, ST_ELEMS]); eout("o_hg", [NPR, ST_ELEMS])

    C = {}
    for nm, shp in (("ident", [128, 128]), ("iota_f", [128, 256]), ("triu", [128, 128]), ("ones", [128, 128])):
        C[nm] = k.sb(nm, shp)
        k.dma(C[nm], E[nm])
    fg_b = k.sb("fg_b", [128, D])
    k.dma(fg_b, E["final_g"].bc([128, D]))

    with k.scope():
        zt = k.sb("zt", [128, 4096])
        k.op("pool", "memset", ap=zt.v, constant=0.0)
        for nm, en in (("o_rw", "rw"), ("o_ret", "ret"), ("o_hg", "hg")):
            if ENABLE[en] and NLAYERS_RUN == L:
                continue
            ov = E[nm].re("b (p f) -> b p f", p=128)
            for b in range(NPR):
                k.dma(ov[b], zt[:, 0:ST_ELEMS // 128])

    mod_d = k.dram("mod_d", [L, 2, 6 * D])
    adaln(k, E, C, mod_d)

    groups = (
        dict(name="s", xin=E["xs"], yout=E["ys"], NT=TS // 128, seqs=[(0, TS)], cond=0),
        dict(name="p", xin=E["xp"], yout=E["yp"], NT=NPR * TP // 128,
             seqs=[(j * TP, TP) for j in range(NPR)], cond=1),
    )
    for g in groups:
        if g['name'] not in GROUPS_RUN:
            continue
        TG = g["NT"] * 128
        g["TG"] = TG
        zT_d = k.dram("zT_" + g["name"], [N_IN, TG])
        yb_d = k.dram("yb_" + g["name"], [4 * 256, TG])
        with k.scope():
            NT = g["NT"]
            x = k.sb("x", [128, NT, D])
            xv = g["xin"].re("(n p) d -> p n d", p=128)
            yv = g["yout"].re("(n p) d -> p n d", p=128)
            for i in range(NT):
                k.dma(x[:, i, :], xv[:, i, :], free=True)
            for l in range(NLAYERS_RUN):
                stage_proj(k, E, C, g, l, x, mod_d, zT_d)
                stage_mixers(k, E, C, g, l, x, zT_d, yb_d)
                if ENABLE["ret"]:
                    stage_ret(k, E, C, g, l, zT_d, yb_d)
                if ENABLE["hg"]:
                    stage_hg(k, E, C, g, l, zT_d, yb_d)
                if ENABLE["hy"]:
                    stage_hy(k, E, C, g, l, zT_d, yb_d)
                if ENABLE["rw"]:
                    stage_rw(k, E, C, g, l, zT_d, yb_d)
                stage_merge(k, E, C, g, l, x, mod_d, zT_d, yb_d)
                if ENABLE["moe"]:
                    stage_moe(k, E, C, g, l, x, mod_d)
            with k.scope():
                final_norm(k, x, NT, fg_b, yv)
    k.finish()
    print("instructions:", k.n_ins)
    return nc


def adaln(k, E, C, mod_d):
    with k.scope():
        cc = k.sb("cc", [16, 128])
        k.dma(cc[0:8, :], E["c_s"].re("o (kc p) -> (o kc) p", p=128))
        k.dma(cc[8:16, :], E["c_ctx"].re("o (kc p) -> (o kc) p", p=128))
        cs = k.sb("cs", [16, 128])
        k.op("act", "activation", out=cs.v, in_=cc.v, func=AF.Sigmoid)
        k.op("dve", "tensor_tensor", out=cs.v, in0=cs.v, in1=cc.v, op=ALU.mult)
        ps = k.ps()
        k.op("pe", "transpose", out=ps[:, 0:16], in_=cs.v, identity=C["ident"][0:16, 0:16])
        scT = k.sb("scT", [128, 16])
        k.op("dve", "tensor_copy", out=scT.v, in_=ps[:, 0:16])
        scv = scT.v.re("p (b kc) -> p kc b", b=2)
        wb = Rot([k.sb("adaw", [128, 8, 512]) for _ in range(2)])
        bb = Rot([k.sb("adab", [2, 512]) for _ in range(2)])
        ob = Rot([k.sb("adao", [2, 512]) for _ in range(2)])
        for l in range(NLAYERS_RUN):
            for c0 in range(0, 6 * D, 512):
                w, b_, o = wb.next(), bb.next(), ob.next()
                k.dma(w, E["ada_w"][l][:, c0:c0 + 512].re("(kc p) n -> p kc n", p=128))
                k.dma(b_, E["ada_b"][l:l + 1, c0:c0 + 512].bc([2, 512]))
                ps = k.ps()
                for kc in range(8):
                    k.op("pe", "matmul", out=ps[0:2, :], lhsT=scv[:, kc, :], rhs=w[:, kc, :],
                         start=(kc == 0), stop=(kc == 7))
                k.op("dve", "tensor_tensor", out=o.v, in0=ps[0:2, :], in1=b_.v, op=ALU.add)
                k.dma(mod_d[l, :, c0:c0 + 512], o.v, free=True)


def bc_param(k, name, src_row):
    t = k.sb(name, [128, D])
    k.dma(t, src_row.bc([128, D]))
    return t


def mod_params(k, E, mod_d, l, cond, which, gname):
    off = 3 * D * which
    sh = bc_param(k, "sh", mod_d[l, cond:cond + 1, off:off + D])
    sc = bc_param(k, "sc", mod_d[l, cond:cond + 1, off + D:off + 2 * D])
    ng = bc_param(k, "ng", E[gname][l:l + 1, :])
    k.op("dve", "scalar_tensor_tensor", out=sc.v, in0=sc.v, scalar=1.0, in1=ng.v, op0=ALU.add, op1=ALU.mult)
    return sc, sh


class NormMod:
    def __init__(self, k):
        self.k = k
        self.junk = k.sb("junk", [128, D])
        self.ss = Rot([k.sb("ss", [128, 1]) for _ in range(2)])

    def __call__(self, xview, A, B, out):
        k = self.k
        ss = self.ss.next()
        k.op("act", "activation", out=self.junk.v, in_=xview, func=AF.Square, accum_out=ss.v)
        k.op("dve", "tensor_scalar", out=ss.v, in0=ss.v, scalar1=1.0 / D, scalar2=EPS, op0=ALU.mult, op1=ALU.add)
        k.op("act", "activation", out=ss.v, in_=ss.v, func=AF.Ln)
        k.op("act", "activation", out=ss.v, in_=ss.v, func=AF.Exp, scale=-0.5)
        k.op("dve", "scalar_tensor_tensor", out=out, in0=xview, scalar=ss[:, 0:1], in1=A.v,
             op0=ALU.mult, op1=ALU.mult)
        if B is not None:
            k.op("dve", "tensor_tensor", out=out, in0=out, in1=B.v, op=ALU.add)


def to_fm(k, C, h_tm, hT, i, free=True, evi=[0]):
    for j in range(2):
        ps = k.ps()
        for a in range(4):
            kc = 4 * j + a
            k.op("pe", "transpose", out=ps[:, a * 128:(a + 1) * 128], in_=h_tm[:, kc * 128:(kc + 1) * 128],
                 identity=C["ident"].v)
        eng = "act" if (evi[0] % 2) else "dve"
        evi[0] += 1
        dst = hT[:, 4 * j:4 * j + 4, i * 128:(i + 1) * 128]
        src = ps.v.re("p (a t) -> p a t", a=4)
        if eng == "act":
            k.op("act", "copy", out=dst, in_=src, free=free)
        else:
            k.op("dve", "tensor_copy", out=dst, in_=src, free=free)


def linear_fm(k, w_ext, col0, ncols, inT, KC, T, evac, wrot, WB=256, TB=512, bfrot=None):
    for c0 in range(col0, col0 + ncols, WB):
        cw = min(WB, col0 + ncols - c0)
        wb = wrot.next()
        k.dma(wb[:, :, 0:cw], w_ext[:, c0:c0 + cw].re("(kc p) n -> p kc n", p=128))
        if bfrot is not None:
            wbb = bfrot.next()
            k.op("pool", "tensor_copy", out=wbb[:, :, 0:cw], in_=wb[:, :, 0:cw])
            wb = wbb
        for n0 in range(0, cw, 128):
            nw = min(128, cw - n0)
            for t0 in range(0, T, TB):
                tw = min(TB, T - t0)
                ps = k.ps()
                for kc in range(KC):
                    k.op("pe", "matmul", out=ps[0:nw, 0:tw], lhsT=wb[:, kc, n0:n0 + nw],
                         rhs=inT[:, kc, t0:t0 + tw], start=(kc == 0), stop=(kc == KC - 1))
                evac(ps, c0 + n0, nw, t0, tw)


def stage_proj(k, E, C, g, l, x, mod_d, zT_d):
    NT, TG = g["NT"], g["TG"]
    with k.scope():
        A, B = mod_params(k, E, mod_d, l, g["cond"], 0, "norm1_g")
        nm = NormMod(k)
        hT = k.sb("hT", [128, 8, TG], BF16 if PROJ_BF16 else F32)
        htm = Rot([k.sb("htm", [128, D]) for _ in range(2)])
        for i in range(NT):
            h = htm.next()
            nm(x[:, i, :], A, B, h.v)
            to_fm(k, C, h, hT, i)
        wrot = Rot([k.sb("wb", [128, 8, 256]) for _ in range(2)])
        bfrot = Rot([k.sb("wbb", [128, 8, 256], BF16) for _ in range(2)]) if PROJ_BF16 else None
        stg = Rot([k.sb("stg", [128, 512]) for _ in range(3)])
        cnt = [0]

        def evac(ps, row0, nw, t0, tw):
            s = stg.next()
            if row0 >= O_MG:
                k.op("act", "activation", out=s[0:nw, 0:tw], in_=ps[0:nw, 0:tw], func=AF.Sigmoid)
            elif cnt[0] % 2:
                k.op("act", "copy", out=s[0:nw, 0:tw], in_=ps[0:nw, 0:tw])
            else:
                k.op("dve", "tensor_copy", out=s[0:nw, 0:tw], in_=ps[0:nw, 0:tw])
            cnt[0] += 1
            k.dma(zT_d[row0:row0 + nw, t0:t0 + tw], s[0:nw, 0:tw], free=True)

        ranges = []
        if ENABLE["rw"]:
            ranges.append((O_RW, 896))
        if ENABLE["hy"]:
            ranges.append((O_HY, 768))
        if ENABLE["ret"]:
            ranges.append((O_RET, 1024))
        if ENABLE["hg"]:
            ranges.append((O_HG, 1280))
        ranges.append((O_MG, 4096))
        for (c0, n) in ranges:
            linear_fm(k, E["w_in"][l], c0, n, hT, 8, TG, evac, wrot, bfrot=bfrot)


def stage_mixers(k, E, C, g, l, x, zT_d, yb_d):
    TG = g["TG"]
    with k.scope():
        z = k.sb("zz", [128, 2048])
        k.op("pool", "memset", ap=z.v, constant=0.0)
        for b, nm in enumerate(("rw", "hy", "ret", "hg")):
            if not ENABLE[nm]:
                for c in range(2):
                    k.dma(yb_d[b * 256 + c * 128:b * 256 + (c + 1) * 128, :], z[:, 0:TG], free=True)


def stage_ret(k, E, C, g, l, zT_d, yb_d):
    TG, seqs = g["TG"], g["seqs"]
    is_s = g["name"] == "s"
    zq, zk, zv, zg = O_RET, O_RET + 256, O_RET + 512, O_RET + 768
    if is_s:
        with k.scope():
            cos2 = k.sb("cos2", [128, TS]); k.dma(cos2, E["cos2"])
            sin2 = k.sb("sin2", [128, TS]); k.dma(sin2, E["sin2"])
            perm = k.sb("perm", [128, 128]); k.dma(perm, E["perm"])
            tq = Rot([k.sb("rq", [128, TS]) for _ in range(2)])
            to = Rot([k.sb("ro", [128, TS]) for _ in range(2)])
            tmp = Rot([k.sb("rt", [128, 512]) for _ in range(2)])
            for r0 in (zq, zq + 128, zk, zk + 128):
                t, o = tq.next(), to.next()
                k.dma(t, zT_d[r0:r0 + 128, :])
                for nb in range(0, TS, 512):
                    ps = k.ps()
                    k.op("pe", "matmul", out=ps.v, lhsT=perm.v, rhs=t[:, nb:nb + 512], start=True, stop=True)
                    tm = tmp.next()
                    k.op("dve", "tensor_tensor", out=tm.v, in0=ps.v, in1=sin2[:, nb:nb + 512], op=ALU.mult)
                    k.op("pool", "tensor_tensor", out=o[:, nb:nb + 512], in0=t[:, nb:nb + 512],
                         in1=cos2[:, nb:nb + 512], op=ALU.mult)
                    k.op("dve", "tensor_tensor", out=o[:, nb:nb + 512], in0=o[:, nb:nb + 512], in1=tm.v, op=ALU.add)
                k.dma(zT_d[r0:r0 + 128, :], o.v)
    with k.scope():
        lg = k.sb("lg", [128, 8])
        nlg = k.sb("nlg", [128, 8])
        lgT = k.sb("lgT", [128, 8])
        k.dma(lg, E["ret_rate"][l:l + 1, :].bc([128, 8]))
        k.op("act", "activation", out=nlg.v, in_=lg.v, func=AF.Exp)
        k.op("dve", "tensor_scalar", out=lg.v, in0=nlg.v, scalar1=-1.0, scalar2=None, op0=ALU.mult)
        gn4 = k.sb("gn4", [4, 128])
        k.dma(gn4, E["ret_gn"][l].re("g (hp c) -> (g hp) c", c=128))
        psg = k.ps()
        k.op("pe", "transpose", out=psg[:, 0:4], in_=gn4.v, identity=C["ident"][0:4, 0:4])
        gnT = k.sb("gnT", [128, 4])
        k.op("dve", "tensor_copy", out=gnT.v, in_=psg[:, 0:4])
        Tmax = max(T for (_, T) in seqs)
        k.op("dve", "tensor_scalar", out=lgT.v, in0=lg.v, scalar1=float(Tmax), scalar2=None, op0=ALU.mult)
        WW = 2 * Tmax - 128
        xoff = (TS - 128) - (Tmax - 128)
        Xt = k.sb("Xt", [128, WW]); k.dma(Xt, E["xtab"][:, xoff:xoff + WW])
        W = k.sb("W", [128, WW])
        HW = WW // 2
        tmpW = k.sb("tmpW", [128, HW])
        NB = min(512, Tmax)
        nnb = Tmax // NB
        nblk = Tmax // 128
        qTt, kTt, vTt, gTt = (k.sb(nm, [128, Tmax]) for nm in ("qT", "kT", "vT", "gT"))
        v_tm = k.sb("v_tm", [128, nblk, 128])
        yo = k.sb("yo", [128, Tmax])
        atr = Rot([k.sb("at", [128, Tmax]) for _ in range(2)])
        t64 = Rot([k.sb("t64", [128, NB]) for _ in range(6)])
        if is_s:
            iota_n = k.sb("iota_n", [128, TS]); k.dma(iota_n, E["iota_n"])
            s0f = k.sb("s0f", [128, 128]); s0b = k.sb("s0b", [128, 128])
        else:
            k_tm = k.sb("k_tm", [128, nblk, 128])
            evals = k.sb("evals", [128, 4]); k.dma(evals, E["evals"])
            tokdec = k.sb("tokdec", [128, 2, 8])
            for blk in range(2):
                for d in range(2):
                    for h in range(4):
                        k.op("act", "activation", out=tokdec[:, blk, d * 4 + h:d * 4 + h + 1],
                             in_=evals[:, blk * 2 + d:blk * 2 + d + 1], func=AF.Exp, scale=lg[:, d * 4 + h:d * 4 + h + 1])
            kdr = Rot([k.sb("kd", [128, 64]) for _ in range(2)])
            sor = Rot([k.sb("so", [64, 64]) for _ in range(2)])
            o_ret = E["o_ret"].re("b (l d h k v) -> b l d h k v", l=L, d=2, h=4, k=64)
        for sj, (s0, T) in enumerate(seqs):
            for hp in range(2):
                k.dma(qTt, zT_d[zq + hp * 128:zq + (hp + 1) * 128, s0:s0 + T])
                k.dma(kTt, zT_d[zk + hp * 128:zk + (hp + 1) * 128, s0:s0 + T])
                k.dma(vTt, zT_d[zv + hp * 128:zv + (hp + 1) * 128, s0:s0 + T])
                k.dma(gTt, zT_d[zg + hp * 128:zg + (hp + 1) * 128, s0:s0 + T])
                k.op("act", "mul", out=qTt.v, in_=qTt.v, mul=0.125)
                for blk in range(nblk):
                    ps = k.ps()
                    k.op("pe", "transpose", out=ps[:, 0:128], in_=vTt[:, blk * 128:(blk + 1) * 128], identity=C["ident"].v)
                    k.op("act", "copy", out=v_tm[:, blk, :], in_=ps[:, 0:128])
                    if not is_s:
                        ps2 = k.ps()
                        k.op("pe", "transpose", out=ps2[:, 0:128], in_=kTt[:, blk * 128:(blk + 1) * 128],
                             identity=C["ident"].v)
                        k.op("dve", "tensor_copy", out=k_tm[:, blk, :], in_=ps2[:, 0:128])
                if is_s:
                    k.op("pool", "memset", ap=s0f.v, constant=0.0)
                    k.op("pool", "memset", ap=s0b.v, constant=0.0)
                    for hh in range(2):
                        P0 = hh * 64
                        k.dma(s0f[P0:P0 + 64, P0:P0 + 64], E["st_ret"][l, 0, hp * 2 + hh])
                        k.dma(s0b[P0:P0 + 64, P0:P0 + 64], E["st_ret"][l, 1, hp * 2 + hh])
                for hh in range(2):
                    h = hp * 2 + hh
                    P0 = hh * 64
                    sl = slice(P0, P0 + 64)
                    cf, cb = h, 4 + h
                    for half in range(2):
                        c0, c1 = half * HW, (half + 1) * HW
                        k.op("act", "activation", out=tmpW.v, in_=Xt[:, c0:c1], func=AF.Exp, scale=lg[:, cf:cf + 1])
                        k.op("dve", "scalar_tensor_tensor", out=W[:, c0:c1], in0=Xt[:, c0:c1], scalar=0.0, in1=tmpW.v,
                             op0=ALU.is_ge, op1=ALU.mult)
                        k.op("act", "activation", out=tmpW.v, in_=Xt[:, c0:c1], func=AF.Exp, scale=nlg[:, cb:cb + 1])
                        k.op("dve", "scalar_tensor_tensor", out=tmpW.v, in0=Xt[:, c0:c1], scalar=0.0, in1=tmpW.v,
                             op0=ALU.is_le, op1=ALU.mult)
                        k.op("pool", "tensor_tensor", out=W[:, c0:c1], in0=W[:, c0:c1], in1=tmpW.v, op=ALU.add)
                    acc = k.ps_reserve(nnb)
                    if is_s:
                        for nb in range(nnb):
                            cs = slice(nb * NB, (nb + 1) * NB)
                            d1, d2 = t64.next(), t64.next()
                            k.op("act", "activation", out=d1[sl, :], in_=iota_n[sl, cs], func=AF.Exp,
                                 scale=lg[sl, cf:cf + 1], bias=lg[sl, cf:cf + 1])
                            k.op("dve", "tensor_tensor", out=d1[sl, :], in0=d1[sl, :], in1=qTt[sl, cs], op=ALU.mult)
                            k.op("pe", "matmul", out=acc[nb][:, 0:NB], lhsT=s0f[sl, :], rhs=d1[sl, :], start=True, stop=False)
                            k.op("act", "activation", out=d2[sl, :], in_=iota_n[sl, cs], func=AF.Exp,
                                 scale=nlg[sl, cb:cb + 1], bias=lgT[sl, cb:cb + 1])
                            k.op("dve", "tensor_tensor", out=d2[sl, :], in0=d2[sl, :], in1=qTt[sl, cs], op=ALU.mult)
                            k.op("pe", "matmul", out=acc[nb][:, 0:NB], lhsT=s0b[sl, :], rhs=d2[sl, :], start=False, stop=False)
                    for j in range(nblk):
                        at = atr.next()
                        for nb in range(nnb):
                            ps = k.ps()
                            k.op("pe", "matmul", out=ps[:, 0:NB], lhsT=kTt[sl, j * 128:(j + 1) * 128],
                                 rhs=qTt[sl, nb * NB:(nb + 1) * NB], start=True, stop=True)
                            wc = (T - 128 - 128 * j) + nb * NB
                            k.op("dve", "tensor_tensor", out=at[:, nb * NB:(nb + 1) * NB], in0=ps[:, 0:NB],
                                 in1=W[:, wc:wc + NB], op=ALU.mult)
                        for nb in range(nnb):
                            k.op("pe", "matmul", out=acc[nb][:, 0:NB], lhsT=v_tm[:, j, :], rhs=at[:, nb * NB:(nb + 1) * NB],
                                 start=(j == 0 and not is_s), stop=(j == nblk - 1))
                    for nb in range(nnb):
                        cs = slice(nb * NB, (nb + 1) * NB)
                        ysb, cen, sq, rs, sg = (t64.next() for _ in range(5))
                        k.op("act", "copy", out=ysb[sl, :], in_=acc[nb][sl, 0:NB])
                        pm = k.ps()
                        k.op("pe", "matmul", out=pm[:, 0:NB], lhsT=C["ones"][sl, :], rhs=ysb[sl, :], start=True, stop=True)
                        k.op("dve", "scalar_tensor_tensor", out=cen[sl, :], in0=pm[sl, 0:NB], scalar=-1.0 / 64, in1=ysb[sl, :],
                             op0=ALU.mult, op1=ALU.add)
                        k.op("act", "activation", out=sq[sl, :], in_=cen[sl, :], func=AF.Square)
                        pv = k.ps()
                        k.op("pe", "matmul", out=pv[:, 0:NB], lhsT=C["ones"][sl, :], rhs=sq[sl, :], start=True, stop=True)
                        k.op("dve", "tensor_scalar", out=rs[sl, :], in0=pv[sl, 0:NB], scalar1=1.0 / 64, scalar2=1e-5,
                             op0=ALU.mult, op1=ALU.add)
                        k.op("act", "activation", out=rs[sl, :], in_=rs[sl, :], func=AF.Ln)
                        k.op("act", "activation", out=rs[sl, :], in_=rs[sl, :], func=AF.Exp, scale=-0.5)
                        k.op("dve", "tensor_tensor", out=cen[sl, :], in0=cen[sl, :], in1=rs[sl, :], op=ALU.mult)
                        k.op("dve", "tensor_scalar", out=cen[sl, :], in0=cen[sl, :], scalar1=gnT[sl, hp:hp + 1],
                             scalar2=gnT[sl, 2 + hp:3 + hp], op0=ALU.mult, op1=ALU.add)
                        k.op("act", "activation", out=sg[sl, :], in_=gTt[sl, cs], func=AF.Sigmoid)
                        k.op("dve", "tensor_tensor", out=sg[sl, :], in0=sg[sl, :], in1=gTt[sl, cs], op=ALU.mult)
                        k.op("dve", "tensor_tensor", out=yo[sl, cs], in0=cen[sl, :], in1=sg[sl, :], op=ALU.mult)
                    k.ps_release(acc)
                    if not is_s:
                        b = sj
                        for d in range(2):
                            pS = k.ps()
                            for blk in range(nblk):
                                kd = kdr.next()
                                k.op("dve", "tensor_scalar", out=kd.v, in0=k_tm[:, blk, P0:P0 + 64],
                                     scalar1=tokdec[:, blk, d * 4 + h:d * 4 + h + 1], scalar2=None, op0=ALU.mult)
                                k.op("pe", "matmul", out=pS[0:64, 0:64], lhsT=kd.v, rhs=v_tm[:, blk, P0:P0 + 64],
                                     start=(blk == 0), stop=(blk == nblk - 1))
                            so = sor.next()
                            k.op("act", "copy", out=so.v, in_=pS[0:64, 0:64])
                            k.dma(o_ret[b, l, d, h], so.v)
                k.dma(yb_d[512 + hp * 128:512 + (hp + 1) * 128, s0:s0 + T], yo[:, 0:T])


def stage_hg(k, E, C, g, l, zT_d, yb_d):
    TG, seqs = g["TG"], g["seqs"]
    is_s = g["name"] == "s"
    z0 = O_HG
    with k.scope():
        lb16 = k.sb("lb16", [16, 128])
        k.dma(lb16, E["hg_lb"].re("l d (hp c) -> (l d hp) c", c=128))
        ps = k.ps()
        k.op("pe", "transpose", out=ps[:, 0:16], in_=lb16.v, identity=C["ident"][0:16, 0:16])
        lbT = k.sb("lbT", [128, 4, 4])
        k.op("dve", "tensor_copy", out=lbT.v.re("p l j -> p (l j)"), in_=ps[:, 0:16])
        mx = k.sb("mx4", [128, 4]); sm4 = k.sb("sm4", [128, 4])
        k.op("dve", "tensor_tensor", out=mx.v, in0=lbT[:, 0, :], in1=lbT[:, 1, :], op=ALU.max)
        k.op("dve", "tensor_tensor", out=mx.v, in0=mx.v, in1=lbT[:, 2, :], op=ALU.max)
        k.op("dve", "tensor_tensor", out=mx.v, in0=mx.v, in1=lbT[:, 3, :], op=ALU.max)
        for ll in range(4):
            k.op("dve", "tensor_tensor", out=lbT[:, ll, :], in0=lbT[:, ll, :], in1=mx.v, op=ALU.subtract)
        k.op("act", "activation", out=lbT.v, in_=lbT.v, func=AF.Exp)
        k.op("dve", "tensor_tensor", out=sm4.v, in0=lbT[:, 0, :], in1=lbT[:, 1, :], op=ALU.add)
        k.op("dve", "tensor_tensor", out=sm4.v, in0=sm4.v, in1=lbT[:, 2, :], op=ALU.add)
        k.op("dve", "tensor_tensor", out=sm4.v, in0=sm4.v, in1=lbT[:, 3, :], op=ALU.add)
        k.op("dve", "reciprocal", out=sm4.v, in_=sm4.v)
        lbl = k.sb("lbl", [128, 4]); oml = k.sb("oml", [128, 4]); noml = k.sb("noml", [128, 4]); lbf = k.sb("lbf", [128, 4])
        k.op("pool", "memset", ap=lbl.v, constant=0.0)
        for ll in range(1, l + 1):
            k.op("dve", "tensor_tensor", out=mx.v, in0=lbT[:, ll, :], in1=sm4.v, op=ALU.mult)
            k.op("dve", "tensor_tensor", out=lbl.v, in0=lbl.v, in1=mx.v, op=ALU.add)
        k.op("dve", "tensor_scalar", out=oml.v, in0=lbl.v, scalar1=-1.0, scalar2=1.0, op0=ALU.mult, op1=ALU.add)
        k.op("dve", "tensor_scalar", out=noml.v, in0=oml.v, scalar1=-1.0, scalar2=None, op0=ALU.mult)
        k.op("dve", "tensor_scalar", out=lbf.v, in0=lbl.v, scalar1=1e-30, scalar2=None, op0=ALU.max)
        hn2 = k.sb("hn2", [2, 128])
        k.dma(hn2, E["hg_norm"][l:l + 1, :].re("o (hp c) -> (o hp) c", c=128))
        ps = k.ps()
        k.op("pe", "transpose", out=ps[:, 0:2], in_=hn2.v, identity=C["ident"][0:2, 0:2])
        hnT = k.sb("hnT", [128, 2])
        k.op("dve", "tensor_copy", out=hnT.v, in_=ps[:, 0:2])
        Tm = max(T for (_, T) in seqs)
        nblk, nch = Tm // 128, Tm // 32
        NB = min(512, Tm)
        cm = k.sb("cm", [128, Tm]); k.dma(cm, E["cmask"][:, 0:Tm])
        mk = [k.sb("mkf", [128, 128]), k.sb("mkb", [128, 128])]
        k.dma(mk[0], E["mk_f"]); k.dma(mk[1], E["mk_b"])
        qh, zf, vT, gh = (k.sb(nm, [128, Tm]) for nm in ("qh", "zf", "vT", "gh"))
        qt, kt, t1 = (k.sb(nm, [128, Tm]) for nm in ("qt", "kt", "t1"))
        kin, bb = zf, vT
        bend = k.sb("bend", [128, nch]); ebend = k.sb("ebend", [128, nch])
        v_tm = k.sb("v_tm", [128, nblk, 128]); kh_tm = k.sb("kh_tm", [64, 2 * nblk, 128])
        v_t64 = k.sb("v_t64", [64, 2 * nblk, 128])
        yd = [k.sb("yf", [128, Tm]), k.sb("ybw", [128, Tm])]
        S = [[k.sb("S%d%d" % (hh, i), [128, 128]) for i in range(2)] for hh in range(2)]
        atr = [Rot([k.sb("at%d" % hh, [128, 128]) for _ in range(2)]) for hh in range(2)]
        t64 = Rot([k.sb("h64", [128, NB]) for _ in range(4)])
        sor = Rot([k.sb("hso", [128, 64]) for _ in range(2)])
        o_hg = E["o_hg"].re("b (l d h k v) -> b l d h k v", l=L, d=2, h=4, k=64)
        c3 = lambda t: t.v.re("p (c s) -> p c s", s=32)
        for sj, (s0, T) in enumerate(seqs):
            for hp in range(2):
                r = lambda o: zT_d[z0 + o + hp * 128:z0 + o + (hp + 1) * 128, s0:s0 + T]
                k.dma(qh, r(0)); k.dma(vT, r(768)); k.dma(gh, r(1024))
                k.op("act", "activation", out=t1.v, in_=qh.v, func=AF.Sigmoid)
                k.op("dve", "tensor_tensor", out=qh.v, in0=qh.v, in1=t1.v, op=ALU.mult)
                for blk in range(nblk):
                    ps = k.ps()
                    k.op("pe", "transpose", out=ps[:, 0:128], in_=vT[:, blk * 128:(blk + 1) * 128], identity=C["ident"].v)
                    k.op("act", "copy", out=v_tm[:, blk, :], in_=ps[:, 0:128])
                for hb in range(2 * nblk):
                    ps = k.ps()
                    k.op("pe", "transpose", out=ps[0:64, 0:128], in_=vT[:, hb * 64:(hb + 1) * 64], identity=C["ident"].v)
                    k.op("dve", "tensor_copy", out=v_t64[:, hb, :], in_=ps[0:64, 0:128])
                for d in range(2):
                    j = d * 2 + hp
                    k.dma(zf, r(256 + 256 * d))
                    k.op("act", "activation", out=t1.v, in_=zf.v, func=AF.Sigmoid)
                    k.op("dve", "tensor_scalar", out=kin.v, in0=t1.v, scalar1=noml[:, j:j + 1], scalar2=oml[:, j:j + 1],
                         op0=ALU.mult, op1=ALU.add)
                    k.op("dve", "tensor_scalar", out=t1.v, in0=t1.v, scalar1=oml[:, j:j + 1], scalar2=lbf[:, j:j + 1],
                         op0=ALU.mult, op1=ALU.add)
                    k.op("act", "activation", out=t1.v, in_=t1.v, func=AF.Ln)
                    k.op("dve", "tensor_tensor_scan", out=bb.v, data0=cm.v, data1=t1.v, initial=0.0,
                         op0=ALU.mult, op1=ALU.add)
                    k.op("dve", "tensor_copy", out=bend.v, in_=c3(bb)[:, :, 31])
                    if d == 1:
                        k.op("dve", "tensor_tensor", out=c3(bb), in0=bend.v.re("p (c o) -> p c o", o=1).bc([128, nch, 32]),
                             in1=c3(bb), op=ALU.subtract)
                        k.op("dve", "tensor_tensor", out=bb.v, in0=bb.v, in1=t1.v, op=ALU.add)
                    k.op("act", "activation", out=t1.v, in_=bb.v, func=AF.Exp)
                    k.op("dve", "tensor_tensor", out=qt.v, in0=qh.v, in1=t1.v, op=ALU.mult)
                    k.op("act", "activation", out=t1.v, in_=bb.v, func=AF.Exp, scale=-1.0)
                    k.op("dve", "tensor_tensor", out=kt.v, in0=kin.v, in1=t1.v, op=ALU.mult)
                    k.op("act", "activation", out=ebend.v, in_=bend.v, func=AF.Exp)
                    k.op("dve", "tensor_tensor", out=c3(t1), in0=c3(kt),
                         in1=ebend.v.re("p (c o) -> p c o", o=1).bc([128, nch, 32]), op=ALU.mult)
                    for hb in range(2 * nblk):
                        ps = k.ps()
                        k.op("pe", "transpose", out=ps[0:64, 0:128], in_=t1[:, hb * 64:(hb + 1) * 64], identity=C["ident"].v)
                        k.op("act", "copy", out=kh_tm[:, hb, :], in_=ps[0:64, 0:128])
                    cur = [0, 0]
                    for hh in range(2):
                        P0 = hh * 64
                        for i in range(2):
                            k.op("pool", "memset", ap=S[hh][i].v, constant=0.0)
                        if is_s:
                            k.dma(S[hh][0][P0:P0 + 64, P0:P0 + 64], E["st_hg"][l, d, hp * 2 + hh])
                    blks = range(nblk) if d == 0 else range(nblk - 1, -1, -1)
                    chs = range(4) if d == 0 else range(3, -1, -1)
                    for blk in blks:
                        bc_ = slice(blk * 128, (blk + 1) * 128)
                        ats, accs = [], []
                        for hh in range(2):
                            sl = slice(hh * 64, hh * 64 + 64)
                            ps = k.ps()
                            k.op("pe", "matmul", out=ps[:, 0:128], lhsT=kt[sl, bc_], rhs=qt[sl, bc_], start=True, stop=True)
                            at = atr[hh].next()
                            k.op("dve", "tensor_tensor", out=at.v, in0=ps[:, 0:128], in1=mk[d].v, op=ALU.mult)
                            ats.append(at)
                        accs = k.ps_reserve(2)
                        for c in chs:
                            lc = slice(c * 32, (c + 1) * 32)
                            gc = slice(blk * 128 + c * 32, blk * 128 + (c + 1) * 32)
                            ci = blk * 4 + c
                            for hh in range(2):
                                P0 = hh * 64
                                sl = slice(P0, P0 + 64)
                                Sc, Sn = S[hh][cur[hh]], S[hh][1 - cur[hh]]
                                k.op("pe", "matmul", out=accs[hh][:, lc], lhsT=Sc[sl, :], rhs=qt[sl, gc], start=True, stop=False)
                                k.op("pe", "matmul", out=accs[hh][:, lc], lhsT=v_tm[:, blk, :], rhs=ats[hh][:, lc],
                                     start=False, stop=True)
                                pu = k.ps()
                                hb, pb = blk * 2 + c // 2, (c % 2) * 32
                                k.op("pe", "matmul", out=pu[:, 0:64], lhsT=kh_tm[pb:pb + 32, hb, :],
                                     rhs=v_t64[pb:pb + 32, hb, P0:P0 + 64], start=True, stop=True)
                                k.op("dve", "scalar_tensor_tensor", out=Sn[sl, P0:P0 + 64], in0=Sc[sl, P0:P0 + 64],
                                     scalar=ebend[sl, ci:ci + 1], in1=pu[sl, 0:64], op0=ALU.mult, op1=ALU.add)
                                cur[hh] = 1 - cur[hh]
                        for hh in range(2):
                            sl = slice(hh * 64, hh * 64 + 64)
                            k.op("act", "copy", out=yd[d][sl, bc_], in_=accs[hh][sl, 0:128])
                        k.ps_release(accs)
                    if not is_s:
                        for hh in range(2):
                            P0 = hh * 64
                            so = sor.next()
                            k.op("dve", "tensor_copy", out=so[P0:P0 + 64, :], in_=S[hh][cur[hh]][P0:P0 + 64, P0:P0 + 64])
                            k.dma(o_hg[sj, l, d, hp * 2 + hh], so[P0:P0 + 64, :])
                k.op("dve", "tensor_tensor", out=yd[0].v, in0=yd[0].v, in1=yd[1].v, op=ALU.add)
                k.op("act", "activation", out=t1.v, in_=gh.v, func=AF.Sigmoid)
                k.op("dve", "tensor_tensor", out=gh.v, in0=gh.v, in1=t1.v, op=ALU.mult)
                for nb in range(T // NB):
                    cs = slice(nb * NB, (nb + 1) * NB)
                    for hh in range(2):
                        sl = slice(hh * 64, hh * 64 + 64)
                        sq, rs = t64.next(), t64.next()
                        k.op("act", "activation", out=sq[sl, :], in_=yd[0][sl, cs], func=AF.Square)
                        pm = k.ps()
                        k.op("pe", "matmul", out=pm[:, 0:NB], lhsT=C["ones"][sl, :], rhs=sq[sl, :], start=True, stop=True)
                        k.op("dve", "tensor_scalar", out=rs[sl, :], in0=pm[sl, 0:NB], scalar1=1.0 / 64, scalar2=EPS,
                             op0=ALU.mult, op1=ALU.add)
                        k.op("act", "activation", out=rs[sl, :], in_=rs[sl, :], func=AF.Ln)
                        k.op("act", "activation", out=rs[sl, :], in_=rs[sl, :], func=AF.Exp, scale=-0.5)
                        k.op("dve", "scalar_tensor_tensor", out=rs[sl, :], in0=rs[sl, :], scalar=hnT[sl, hp:hp + 1],
                             in1=yd[0][sl, cs], op0=ALU.mult, op1=ALU.mult)
                        k.op("dve", "tensor_tensor", out=yd[1][sl, cs], in0=rs[sl, :], in1=gh[sl, cs], op=ALU.mult)
                k.dma(yb_d[768 + hp * 128:768 + (hp + 1) * 128, s0:s0 + T], yd[1][:, 0:T])


def load_T(k, C, src, n, name):
    st = k.sb(name + "_st", [n, 128])
    k.dma(st, src)
    ps = k.ps()
    k.op("pe", "transpose", out=ps[:, 0:n], in_=st.v, identity=C["ident"][0:n, 0:n])
    t = k.sb(name, [128, n])
    k.op("dve", "tensor_copy", out=t.v, in_=ps[:, 0:n])
    return t


def dft_consts(T):
    nblk, NFC = T // 128, T // 128 + 1
    N2 = 2 * T
    f = np.arange(NFC * 128, dtype=np.float64)
    t = np.arange(T, dtype=np.float64)
    th = 2.0 * np.pi * np.outer(f, t) / N2
    valid = (f <= T)[:, None]
    c = np.where(valid, np.cos(th), 0.0)
    s_ = np.where(valid, np.sin(th), 0.0)
    def fw(m):
        a = m.reshape(NFC, 128, nblk, 128)
        return np.ascontiguousarray(a.transpose(0, 3, 2, 1).reshape(NFC, 128, nblk * 128))
    fcs = np.stack([fw(c), fw(s_)]).astype(np.float32)
    wf = np.where((f == 0) | (f == T), 1.0, 2.0)[:, None] * valid
    gc = (wf * c / N2).reshape(NFC, 128, T)
    gs = (-wf * s_ / N2).reshape(NFC, 128, T)
    gcs = np.stack([gc, gs]).astype(np.float32)
    return fcs, gcs


def stage_hy(k, E, C, g, l, zT_d, yb_d):
    TG, seqs = g["TG"], g["seqs"]
    T = seqs[0][1]
    sfx = "s" if T == TS else "p"
    FCS, GCS = E["fcs_" + sfx], E["gcs_" + sfx]
    nblk, NFC = T // 128, T // 128 + 1
    z0 = O_HY
    PI = float(np.pi)
    NB = min(512, T)
    with k.scope():
        skT = load_T(k, C, E["hy_skip"][l].re("o (c p) -> (o c) p", p=128), 4, "skT")
        hcT = load_T(k, C, E["hy_conv"][l].re("w (c p) -> (w c) p", p=128), 18, "hcT")
        decT = load_T(k, C, E["hy_decay"][l:l + 1, :].re("o (c p) -> (o c) p", p=128), 8, "decT")
        dneg = k.sb("dneg", [128, 8])
        k.op("dve", "tensor_scalar", out=dneg.v, in0=decT.v, scalar1=-1.0, scalar2=None, op0=ALU.mult)
        k.op("dve", "tensor_tensor", out=decT.v, in0=decT.v, in1=dneg.v, op=ALU.max)
        k.op("dve", "tensor_scalar", out=decT.v, in0=decT.v, scalar1=-1.0 / (T - 1), scalar2=None, op0=ALU.mult)
        w3 = k.sb("hw3", [64, 1024]); k.dma(w3, E["hy_ffn3"][l])
        hid2 = k.sb("hid2", [64, T])
        with k.scope():
            featsT = k.sb("featsT", [33, T]); k.dma(featsT, E["feats_s" if T == TS else "feats_p"])
            w1 = k.sb("hw1", [33, 64]); k.dma(w1, E["hy_ffn1"][l])
            w2 = k.sb("hw2", [64, 64]); k.dma(w2, E["hy_ffn2"][l])
            pr = k.sb("hpr", [64, 4])
            k.dma(pr[:, 0:1], E["hy_ffn1_b"][l:l + 1, :].re("o (p q) -> (o p) q", q=1))
            k.dma(pr[:, 1:2], E["hy_ffn2_b"][l:l + 1, :].re("o (p q) -> (o p) q", q=1))
            k.dma(pr[:, 2:3], E["hy_freq"][l, 0:1, :].re("o (p q) -> (o p) q", q=1))
            k.dma(pr[:, 3:4], E["hy_freq"][l, 1:2, :].re("o (p q) -> (o p) q", q=1))
            hid1 = k.sb("hid1", [64, T])
            m1 = k.sb("hm1", [64, NB])

            def sin_layer(wt, rhs, bcol, fcol, dst):
                for n0 in range(0, T, NB):
                    ps = k.ps()
                    k.op("pe", "matmul", out=ps[0:64, 0:NB], lhsT=wt, rhs=rhs[:, n0:n0 + NB], start=True, stop=True)
                    a = dst[:, n0:n0 + NB]
                    k.op("dve", "tensor_scalar", out=a, in0=ps[0:64, 0:NB], scalar1=pr[:, bcol:bcol + 1],
                         scalar2=pr[:, fcol:fcol + 1], op0=ALU.add, op1=ALU.mult)
                    k.op("dve", "tensor_scalar", out=m1.v, in0=a, scalar1=PI, scalar2=-2.0 * PI, op0=ALU.is_ge, op1=ALU.mult)
                    k.op("dve", "tensor_tensor", out=a, in0=a, in1=m1.v, op=ALU.add)
                    k.op("dve", "tensor_scalar", out=m1.v, in0=a, scalar1=-PI, scalar2=2.0 * PI, op0=ALU.is_le, op1=ALU.mult)
                    k.op("dve", "tensor_tensor", out=a, in0=a, in1=m1.v, op=ALU.add)
                    k.op("act", "activation", out=a, in_=a, func=AF.Sin)

            sin_layer(w1.v, featsT, 0, 2, hid1)
            sin_layer(w2.v, hid1, 1, 3, hid2)
        frot = Rot([k.sb("hF", [128, nblk, 128]) for _ in range(2)])
        grot = Rot([k.sb("hG", [128, NB]) for _ in range(3)])
        tmpc = Rot([k.sb("htc", [128, 128]) for _ in range(2)])

        def fwd_dft(x_tm, ncol, sink):
            for fc in range(NFC):
                for part in range(2):
                    F = frot.next()
                    k.dma(F.v.re("p b f -> p (b f)"), FCS[part, fc])
                    ps = k.ps()
                    for blk in range(nblk):
                        k.op("pe", "matmul", out=ps[:, 0:ncol], lhsT=F[:, blk, :], rhs=x_tm[:, blk, 0:ncol],
                             start=(blk == 0), stop=(blk == nblk - 1))
                    sink(part, fc, ps)

        for chh in range(2):
            with k.scope():
                Cre = [k.sb("Cre%d" % o, [128, NFC, 128]) for o in range(2)]
                Cim = [k.sb("Cim%d" % o, [128, NFC, 128]) for o in range(2)]
                with k.scope():
                    iota_n = k.sb("iota_n", [128, T]); k.dma(iota_n, E["iota_n"][:, 0:T])
                    ed = k.sb("hed", [128, T])
                    junk = k.sb("hjunk", [128, T])
                    hfb = [k.sb("hfb%d" % i, [128, T]) for i in range(2)]
                    asum = k.sb("asum", [128, 2])
                    f_tm = k.sb("f_tm", [128, nblk, 256])
                    for o in range(2):
                        for di in range(2):
                            c = o * 4 + di * 2 + chh
                            k.op("act", "activation", out=ed.v, in_=iota_n.v, func=AF.Exp, scale=decT[:, c:c + 1])
                            for n0 in range(0, T, NB):
                                ps = k.ps()
                                k.op("pe", "matmul", out=ps[:, 0:NB], lhsT=w3[:, c * 128:(c + 1) * 128], rhs=hid2[:, n0:n0 + NB],
                                     start=True, stop=True)
                                k.op("dve", "tensor_tensor", out=hfb[di][:, n0:n0 + NB], in0=ps[:, 0:NB], in1=ed[:, n0:n0 + NB],
                                     op=ALU.mult)
                            k.op("dve", "tensor_scalar", out=junk.v, in0=hfb[di].v, scalar1=-1.0, scalar2=None, op0=ALU.mult)
                            k.op("dve", "tensor_tensor", out=junk.v, in0=junk.v, in1=hfb[di].v, op=ALU.max)
                            k.op("dve", "reduce_sum", out=asum[:, di:di + 1], in_=junk.v, axis=AX.X)
                        k.op("dve", "tensor_tensor", out=asum[:, 0:1], in0=asum[:, 0:1], in1=asum[:, 1:2], op=ALU.add)
                        k.op("dve", "reciprocal", out=asum[:, 0:1], in_=asum[:, 0:1])
                        for di in range(2):
                            k.op("dve", "tensor_scalar", out=hfb[di].v, in0=hfb[di].v, scalar1=asum[:, 0:1], scalar2=None, op0=ALU.mult)
                        k.op("pool", "memset", ap=hfb[1][:, 0:1], constant=0.0)
                        for blk in range(nblk):
                            ps = k.ps()
                            for di in range(2):
                                k.op("pe", "transpose", out=ps[:, di * 128:(di + 1) * 128], in_=hfb[di][:, blk * 128:(blk + 1) * 128],
                                     identity=C["ident"].v)
                            k.op("act", "copy", out=f_tm[:, blk, :], in_=ps[:, 0:256])

                        def sink_f(part, fc, ps, o=o):
                            tc_ = tmpc.next()
                            k.op("act", "copy", out=tc_.v, in_=ps[:, 0:128])
                            if part == 0:
                                k.op("dve", "tensor_tensor", out=Cre[o][:, fc, :], in0=ps[:, 128:256], in1=tc_.v, op=ALU.add)
                            else:
                                k.op("dve", "tensor_tensor", out=Cim[o][:, fc, :], in0=ps[:, 128:256], in1=tc_.v, op=ALU.subtract)

                        fwd_dft(f_tm, 256, sink_f)
                hx = [k.sb("hx%d" % i, [128, T]) for i in range(3)]
                Xc, Xs, tA, tB = (k.sb(nm, [128, NFC, 128]) for nm in ("hXc", "hXs", "htA", "htB"))
                cv = k.sb("hcv", [128, T])
                zt = cv
                u_tm = cv.v.re("p (b c) -> p b c", c=128)
                for (s0, T_) in seqs:
                    for part in range(3):
                        cix = part * 2 + chh
                        k.dma(zt, zT_d[z0 + part * 256 + chh * 128:z0 + part * 256 + (chh + 1) * 128, s0:s0 + T])
                        h = hx[part]
                        k.op("dve", "tensor_scalar", out=h.v, in0=zt.v, scalar1=hcT[:, 6 + cix:7 + cix], scalar2=None, op0=ALU.mult)
                        k.op("dve", "scalar_tensor_tensor", out=h[:, 1:T], in0=zt[:, 0:T - 1], scalar=hcT[:, cix:cix + 1],
                             in1=h[:, 1:T], op0=ALU.mult, op1=ALU.add)
                        k.op("dve", "scalar_tensor_tensor", out=h[:, 0:T - 1], in0=zt[:, 1:T], scalar=hcT[:, 12 + cix:13 + cix],
                             in1=h[:, 0:T - 1], op0=ALU.mult, op1=ALU.add)
                    cur = hx[0]
                    for o in range(2):
                        for blk in range(nblk):
                            ps = k.ps()
                            k.op("pe", "transpose", out=ps[:, 0:128], in_=cur[:, blk * 128:(blk + 1) * 128], identity=C["ident"].v)
                            k.op("act", "copy", out=u_tm[:, blk, :], in_=ps[:, 0:128])

                        def sink_x(part, fc, ps):
                            dst = Xc if part == 0 else Xs
                            if fc % 2:
                                k.op("act", "copy", out=dst[:, fc, :], in_=ps[:, 0:128])
                            else:
                                k.op("dve", "tensor_copy", out=dst[:, fc, :], in_=ps[:, 0:128])

                        fwd_dft(u_tm, 128, sink_x)
                        k.op("dve", "tensor_tensor", out=tA.v, in0=Xs.v, in1=Cim[o].v, op=ALU.mult)
                        k.op("pool", "tensor_tensor", out=tB.v, in0=Xs.v, in1=Cre[o].v, op=ALU.mult)
                        k.op("dve", "tensor_tensor", out=Xs.v, in0=Xc.v, in1=Cim[o].v, op=ALU.mult)
                        k.op("dve", "tensor_tensor", out=Xs.v, in0=Xs.v, in1=tB.v, op=ALU.subtract)
                        k.op("pool", "tensor_tensor", out=Xc.v, in0=Xc.v, in1=Cre[o].v, op=ALU.mult)
                        k.op("dve", "tensor_tensor", out=Xc.v, in0=Xc.v, in1=tA.v, op=ALU.add)
                        for tb in range(T // NB):
                            ps = k.ps()
                            n = 0
                            for fc in range(NFC):
                                for part in range(2):
                                    G = grot.next()
                                    k.dma(G, GCS[part, fc][:, tb * NB:(tb + 1) * NB])
                                    k.op("pe", "matmul", out=ps[:, 0:NB], lhsT=(Xc if part == 0 else Xs)[:, fc, :], rhs=G.v,
                                         start=(n == 0), stop=(n == 2 * NFC - 1))
                                    n += 1
                            k.op("dve", "scalar_tensor_tensor", out=cv[:, tb * NB:(tb + 1) * NB], in0=cur[:, tb * NB:(tb + 1) * NB],
                                 scalar=skT[:, o * 2 + chh:o * 2 + chh + 1], in1=ps[:, 0:NB], op0=ALU.mult, op1=ALU.add)
                        k.op("dve", "tensor_tensor", out=hx[o + 1].v, in0=hx[o + 1].v, in1=cv.v, op=ALU.mult)
                        cur = hx[o + 1]
                    k.dma(yb_d[256 + chh * 128:256 + (chh + 1) * 128, s0:s0 + T], hx[2].v)


def stage_rw(k, E, C, g, l, zT_d, yb_d):
    TG, seqs = g["TG"], g["seqs"]
    is_s = g["name"] == "s"
    T = seqs[0][1]
    CH = 64
    nch = T // CH
    G = min(8, nch)
    BW_ = G * CH
    nbat = nch // G
    NB = min(512, T)
    with k.scope():
        muT = load_T(k, C, E["rw_mu"][l].re("w (c p) -> (w c) p", p=128), 14, "muT")
        mid = k.sb("rwmid", [128, 7])
        k.op("dve", "tensor_tensor", out=mid.v, in0=muT[:, 0:7], in1=muT[:, 7:14], op=ALU.add)
        k.op("dve", "tensor_scalar", out=mid.v, in0=mid.v, scalar1=-1.0, scalar2=1.0, op0=ALU.mult, op1=ALU.add)
        w0T = load_T(k, C, E["rw_w0"][l].re("d (c p) -> (d c) p", p=128), 4, "w0T")
        a0T = load_T(k, C, E["rw_a0"][l:l + 1, :].re("o (c p) -> (o c) p", p=128), 2, "a0T")
        kvT = load_T(k, C, E["rw_kvec"][l].re("j (c p) -> (j c) p", p=128), 6, "kvT")
        lnT = load_T(k, C, E["rw_ln"][l].re("j (c p) -> (j c) p", p=128), 4, "lnT")
        omka = k.sb("omka", [128, 2])
        k.op("dve", "tensor_scalar", out=omka.v, in0=kvT[:, 2:4], scalar1=-1.0, scalar2=1.0, op0=ALU.mult, op1=ALU.add)
        lora = k.sb("lora", [128, 4, 256])
        k.dma(lora[0:32, 0, :], E["rw_w_up"][l, 0]); k.dma(lora[0:32, 1, :], E["rw_w_up"][l, 1])
        k.dma(lora[32:64, 2, :], E["rw_a_up"][l]); k.dma(lora[64:128, 3, :], E["rw_g_up"][l])
        cm = zt_alias = None
        MK = {}
        for nm in ("m_lt", "m_gt", "m_le", "m_ge", "eye8"):
            MK[nm] = k.sb(nm, [64, 64]); k.dma(MK[nm], E[nm])
        mb = lambda t_: t_.v.re("p (o c) -> p o c", o=1).bc([64, G, 64])
        p3v = lambda ps_: ps_[0:64, 0:G * 64].re("p (g c) -> p g c", g=G)
        MSK = [(MK["m_lt"], MK["m_le"], MK["m_gt"]), (MK["m_gt"], MK["m_ge"], MK["m_lt"])]
        zt = k.sb("rzt", [128, T])
        r_, kmod, v_, kkn, alpha, x6 = (k.sb(nm, [128, T]) for nm in ("rr", "rkmod", "rv", "rkkn", "ralpha", "rx6"))
        logw, lpi, ysum = (k.sb(nm, [128, T]) for nm in ("rlogw", "rlpi", "rysum"))
        tot = k.sb("rtot", [128, nch]); pC = k.sb("rpC", [128, nch])
        bt = {nm: k.sb("rb_" + nm, [128, BW_]) for nm in ("rt", "at", "kt", "kp", "e")}
        t64 = {nm: k.sb("r64_" + nm, [128 if nm == "v" else 64, G, 128]) for nm in ("v", "a", "k")}
        PP = {nm: k.sb("rP_" + nm, [128 if nm in ("BT", "RAT", "RKT") else 64, G, 64])
              for nm in ("Y0", "Y1", "YT0", "YT1", "P", "BT", "RAT", "RKT")}
        for t_ in (t64["v"], PP["BT"], PP["RAT"], PP["RKT"]):
            k.op("pool", "memset", ap=t_.v, constant=0.0)
        Mst = [[k.sb("rM%d%d" % (hh, i), [128, 128]) for i in range(2)] for hh in range(2)]
        Upad = [k.sb("rUp%d" % hh, [128, 128]) for hh in range(2)]
        for hh in range(2):
            k.op("pool", "memset", ap=Upad[hh].v, constant=0.0)
        g1r = Rot([k.sb("rg1", [64, 64]) for _ in range(2)])
        u_r = Rot([k.sb("ru", [64, 64]) for _ in range(2)])
        tmr = Rot([k.sb("rtm", [128, 64]) for _ in range(2)])
        s0pad = k.sb("rs0", [64, 128])
        sor = Rot([k.sb("rso", [64, 64]) for _ in range(2)])
        assert BW_ == NB
        t5 = Rot([bt["rt"], bt["at"], bt["kt"], bt["kp"]])
        o_rw = E["o_rw"].re("b (l d h v k) -> b l d h v k", l=L, d=2, h=4, v=64)
        c3 = lambda t_: t_.v.re("p (c s) -> p c s", s=CH)

        def shift(dst, row0, cix):
            k.dma(zt, zT_d[O_RW + row0:O_RW + row0 + 128, s0:s0 + T])
            k.op("dve", "tensor_scalar", out=dst.v, in0=zt.v, scalar1=mid[:, cix:cix + 1], scalar2=None, op0=ALU.mult)
            k.op("dve", "scalar_tensor_tensor", out=dst[:, 1:T], in0=zt[:, 0:T - 1], scalar=muT[:, cix:cix + 1],
                 in1=dst[:, 1:T], op0=ALU.mult, op1=ALU.add)
            k.op("dve", "scalar_tensor_tensor", out=dst[:, 0:T - 1], in0=zt[:, 1:T], scalar=muT[:, 7 + cix:8 + cix],
                 in1=dst[:, 0:T - 1], op0=ALU.mult, op1=ALU.add)

        for sj, (s0, T_) in enumerate(seqs):
            shift(x6, 768, 6)
            k.op("act", "activation", out=x6[0:32, :], in_=x6[0:32, :], func=AF.Sigmoid, scale=2.0)
            k.op("dve", "tensor_scalar", out=x6[0:32, :], in0=x6[0:32, :], scalar1=2.0, scalar2=-1.0, op0=ALU.mult, op1=ALU.add)
            k.op("act", "activation", out=x6[64:128, :], in_=x6[64:128, :], func=AF.Sigmoid)
            for hp in range(2):
                shift(r_, hp * 128, hp)
                shift(kmod, 256 + hp * 128, 2 + hp)
                shift(v_, 512 + hp * 128, 4 + hp)
                for n0 in range(0, T, NB):
                    ps = k.ps()
                    k.op("pe", "matmul", out=ps[:, 0:NB], lhsT=lora[32:64, 2, hp * 128:(hp + 1) * 128],
                         rhs=x6[32:64, n0:n0 + NB], start=True, stop=True)
                    k.op("act", "activation", out=alpha[:, n0:n0 + NB], in_=ps[:, 0:NB], func=AF.Sigmoid,
                         bias=a0T[:, hp:hp + 1], scale=1.0)
                k.op("dve", "tensor_scalar", out=kkn.v, in0=kmod.v, scalar1=kvT[:, 0 + hp:1 + hp], scalar2=None, op0=ALU.mult)
                for n0 in range(0, T, NB):
                    for hh in range(2):
                        sl = slice(hh * 64, hh * 64 + 64)
                        sq, rs = t5.next(), t5.next()
                        k.op("act", "activation", out=sq[sl, :], in_=kkn[sl, n0:n0 + NB], func=AF.Square)
                        pm = k.ps()
                        k.op("pe", "matmul", out=pm[:, 0:NB], lhsT=C["ones"][sl, :], rhs=sq[sl, :], start=True, stop=True)
                        k.op("dve", "tensor_scalar", out=rs[sl, :], in0=pm[sl, 0:NB], scalar1=1e-24, scalar2=None, op0=ALU.max)
                        k.op("act", "activation", out=rs[sl, :], in_=rs[sl, :], func=AF.Ln)
                        k.op("act", "activation", out=rs[sl, :], in_=rs[sl, :], func=AF.Exp, scale=-0.5)
                        k.op("dve", "tensor_tensor", out=kkn[sl, n0:n0 + NB], in0=kkn[sl, n0:n0 + NB], in1=rs[sl, :], op=ALU.mult)
                k.op("dve", "tensor_scalar", out=zt.v, in0=alpha.v, scalar1=kvT[:, 2 + hp:3 + hp], scalar2=omka[:, hp:hp + 1],
                     op0=ALU.mult, op1=ALU.add)
                k.op("dve", "tensor_tensor", out=kmod.v, in0=kmod.v, in1=zt.v, op=ALU.mult)
                k.op("dve", "tensor_tensor", out=alpha.v, in0=alpha.v, in1=kkn.v, op=ALU.mult)
                for d in range(2):
                    if RW_STOP[0] <= 1:
                        break
                    mT_s, mT_i, m_s = MSK[d]
                    for n0 in range(0, T, NB):
                        ps = k.ps()
                        k.op("pe", "matmul", out=ps[:, 0:NB], lhsT=lora[0:32, d, hp * 128:(hp + 1) * 128],
                             rhs=x6[0:32, n0:n0 + NB], start=True, stop=True)
                        k.op("act", "activation", out=logw[:, n0:n0 + NB], in_=ps[:, 0:NB], func=AF.Sigmoid,
                             bias=w0T[:, d * 2 + hp:d * 2 + hp + 1], scale=1.0)
                    k.op("dve", "tensor_scalar", out=logw.v, in0=logw.v, scalar1=-0.6065306597126334, scalar2=None, op0=ALU.mult)
                    k.dma(zt, E["cmask64"][:, 0:T])
                    k.op("dve", "tensor_tensor_scan", out=lpi.v, data0=zt.v, data1=logw.v, initial=0.0, op0=ALU.mult, op1=ALU.add)
                    k.op("dve", "tensor_copy", out=tot.v, in_=c3(lpi)[:, :, CH - 1])
                    if d == 1:
                        k.op("dve", "tensor_tensor", out=c3(lpi), in0=tot.v.re("p (c o) -> p c o", o=1).bc([128, nch, CH]),
                             in1=c3(lpi), op=ALU.subtract)
                        k.op("dve", "tensor_tensor", out=lpi.v, in0=lpi.v, in1=logw.v, op=ALU.add)
                    k.op("act", "activation", out=pC.v, in_=tot.v, func=AF.Exp)
                    cur = [0, 0]
                    for hh in range(2):
                        P0 = hh * 64
                        for i in range(2):
                            k.op("pool", "memset", ap=Mst[hh][i].v, constant=0.0)
                        if is_s:
                            k.op("pool", "memset", ap=s0pad.v, constant=0.0)
                            k.dma(s0pad[:, P0:P0 + 64], E["st_rw"][l, d, hp * 2 + hh])
                            ps = k.ps()
                            k.op("pe", "matmul", out=ps[:, 0:64], lhsT=s0pad.v, rhs=C["ident"][0:64, 0:64], start=True, stop=True)
                            k.op("dve", "tensor_copy", out=Mst[hh][0][P0:P0 + 64, P0:P0 + 64], in_=ps[P0:P0 + 64, 0:64])
                    bats = range(nbat) if d == 0 else range(nbat - 1, -1, -1)
                    if RW_STOP[0] <= 2:
                        bats = []
                    for b in bats:
                        bc_ = slice(b * BW_, (b + 1) * BW_)
                        e = bt["e"]
                        k.op("act", "activation", out=e.v, in_=lpi[:, bc_], func=AF.Exp)
                        k.op("dve", "tensor_tensor", out=bt["rt"].v, in0=r_[:, bc_], in1=e.v, op=ALU.mult)
                        k.op("act", "activation", out=e.v, in_=lpi[:, bc_], func=AF.Exp, scale=-1.0)
                        k.op("dve", "tensor_tensor", out=bt["at"].v, in0=alpha[:, bc_], in1=e.v, op=ALU.mult)
                        k.op("dve", "tensor_tensor", out=bt["kt"].v, in0=kmod[:, bc_], in1=e.v, op=ALU.mult)
                        k.op("dve", "tensor_tensor", out=e.v, in0=lpi[:, bc_], in1=logw[:, bc_], op=ALU.subtract)
                        k.op("act", "activation", out=e.v, in_=e.v, func=AF.Exp)
                        k.op("dve", "tensor_tensor", out=bt["kp"].v, in0=kkn[:, bc_], in1=e.v, op=ALU.mult)
                        for nm, src, off in (("v", v_, b * BW_), ("a", bt["at"], 0), ("k", bt["kt"], 0)):
                            for q0 in range(0, G, 4):
                                ps = k.ps()
                                for qq in range(min(4, G - q0)):
                                    gq = q0 + qq
                                    k.op("pe", "transpose", out=ps[0:64, qq * 128:(qq + 1) * 128],
                                         in_=src[:, off + gq * CH:off + (gq + 1) * CH], identity=C["ident"].v)
                                nq = min(4, G - q0)
                                k.op("act", "copy", out=t64[nm][0:64, q0:q0 + nq, :],
                                     in_=ps[0:64, 0:nq * 128].re("p (q c) -> p q c", q=nq))
                        for hh in range(2):
                            if RW_STOP[0] <= 3:
                                break
                            P0 = hh * 64
                            sl = slice(P0, P0 + 64)
                            GW = G * 64
                            banks = {nm: k.ps() for nm in ("AT", "RAT", "BT", "RKT", "A")}
                            for gq in range(G):
                                cc = slice(gq * CH, (gq + 1) * CH)
                                oc = slice(gq * 64, (gq + 1) * 64)
                                k.op("pe", "matmul", out=banks["AT"][0:64, oc], lhsT=bt["at"][sl, cc], rhs=bt["kp"][sl, cc], start=True, stop=True)
                                k.op("pe", "matmul", out=banks["RAT"][0:64, oc], lhsT=bt["at"][sl, cc], rhs=bt["rt"][sl, cc], start=True, stop=True)
                                k.op("pe", "matmul", out=banks["BT"][0:64, oc], lhsT=bt["kt"][sl, cc], rhs=bt["kp"][sl, cc], start=True, stop=True)
                                k.op("pe", "matmul", out=banks["RKT"][0:64, oc], lhsT=bt["kt"][sl, cc], rhs=bt["rt"][sl, cc], start=True, stop=True)
                                k.op("pe", "matmul", out=banks["A"][0:64, oc], lhsT=bt["kp"][sl, cc], rhs=bt["at"][sl, cc], start=True, stop=True)
                            f2 = lambda t_: t_.v.re("p g c -> p (g c)")
                            Y, YT, Yn, YTn, P = PP["Y0"], PP["YT0"], PP["Y1"], PP["YT1"], PP["P"]
                            k.op("dve", "tensor_tensor", out=Y.v, in0=p3v(banks["AT"]), in1=mb(mT_s), op=ALU.mult)
                            k.op("dve", "tensor_tensor", out=YT.v, in0=p3v(banks["A"]), in1=mb(m_s), op=ALU.mult)
                            k.op("dve", "tensor_tensor", out=PP["RAT"][0:64], in0=p3v(banks["RAT"]), in1=mb(mT_i), op=ALU.mult)
                            k.op("dve", "tensor_tensor", out=PP["BT"][0:64], in0=p3v(banks["BT"]), in1=mb(mT_s), op=ALU.mult)
                            k.op("dve", "tensor_tensor", out=PP["RKT"][0:64], in0=p3v(banks["RKT"]), in1=mb(mT_i), op=ALU.mult)
                            k.op("dve", "tensor_tensor", out=P.v, in0=mb(MK["eye8"]), in1=Y.v, op=ALU.subtract)
                            for lev in range(6 - 1):
                                p1, p2 = k.ps(), k.ps()
                                for gq in range(G):
                                    oc = slice(gq * 64, (gq + 1) * 64)
                                    k.op("pe", "matmul", out=p1[0:64, oc], lhsT=YT[:, gq, :], rhs=Y[:, gq, :], start=True, stop=True)
                                    k.op("pe", "matmul", out=p2[0:64, oc], lhsT=Y[:, gq, :], rhs=YT[:, gq, :], start=True, stop=True)
                                k.op("act", "copy", out=f2(Yn), in_=p1[0:64, 0:GW])
                                k.op("dve", "tensor_copy", out=f2(YTn), in_=p2[0:64, 0:GW])
                                p3 = k.ps()
                                for gq in range(G):
                                    oc = slice(gq * 64, (gq + 1) * 64)
                                    k.op("pe", "matmul", out=p3[0:64, oc], lhsT=YTn[:, gq, :], rhs=P[:, gq, :], start=True, stop=True)
                                k.op("dve", "tensor_tensor", out=f2(P), in0=f2(P), in1=p3[0:64, 0:GW], op=ALU.add)
                                Y, YT, Yn, YTn = Yn, YTn, Y, YT
                            chs = range(G) if d == 0 else range(G - 1, -1, -1)
                            if RW_STOP[0] <= 4:
                                chs = []
                            for gq in chs:
                                ci = b * G + gq
                                cc = slice(gq * CH, (gq + 1) * CH)
                                gcol = slice(b * BW_ + gq * CH, b * BW_ + (gq + 1) * CH)
                                Mc, Mn = Mst[hh][cur[hh]], Mst[hh][1 - cur[hh]]
                                kr = slice(0, 64) if hh == 0 else slice(0, 128)
                                ps1 = k.ps()
                                k.op("pe", "matmul", out=ps1[0:64, 0:64], lhsT=bt["kp"][sl, cc], rhs=Mc[sl, P0:P0 + 64], start=True, stop=False)
                                k.op("pe", "matmul", out=ps1[0:64, 0:64], lhsT=PP["BT"][kr, gq, :], rhs=t64["v"][kr, gq, P0:P0 + 64],
                                     start=False, stop=True)
                                g1 = g1r.next()
                                k.op("act", "mul", out=g1.v, in_=ps1[0:64, 0:64], mul=-1.0)
                                if RW_STOP[0] <= 5:
                                    continue
                                ps2 = k.ps()
                                lh = P
                                if RW_VAR[0] == 5:
                                    lh = PP["RKT"]
                                if RW_VAR[0] == 6:
                                    k.op("dve", "tensor_copy", out=PP["Y0"].v, in_=P.v)
                                    lh = PP["Y0"]
                                k.op("pe", "matmul", out=(ps2[0:64, 64:128] if RW_VAR[0] == 7 else ps2[0:64, 0:64]), lhsT=lh[:, gq, :],
                                     rhs=(PP["RAT"][:, gq, :] if RW_VAR[0] == 3 else g1.v), start=True, stop=True)
                                if RW_VAR[0] == 4:
                                    continue
                                u = u_r.next()
                                if RW_VAR[0] == 2:
                                    k.op("dve", "tensor_copy", out=u.v, in_=ps2[0:64, 0:64])
                                else:
                                    k.op("act", "copy", out=u.v, in_=ps2[0:64, 0:64])
                                if RW_VAR[0] != 1:
                                    k.op("dve", "tensor_copy", out=Upad[hh][0:64, P0:P0 + 64], in_=u.v)
                                if RW_STOP[0] <= 6:
                                    continue
                                psy = k.ps()
                                k.op("pe", "matmul", out=psy[:, 0:64], lhsT=Mc[sl, :], rhs=bt["rt"][sl, cc], start=True, stop=False)
                                k.op("pe", "matmul", out=psy[:, 0:64], lhsT=Upad[hh][kr, :], rhs=PP["RAT"][kr, gq, :], start=False, stop=False)
                                k.op("pe", "matmul", out=psy[:, 0:64], lhsT=t64["v"][kr, gq, :], rhs=PP["RKT"][kr, gq, :], start=False, stop=True)
                                if d == 0:
                                    k.op("act", "copy", out=ysum[sl, gcol], in_=psy[sl, 0:64])
                                else:
                                    k.op("dve", "tensor_tensor", out=ysum[sl, gcol], in0=ysum[sl, gcol], in1=psy[sl, 0:64], op=ALU.add)
                                if RW_STOP[0] <= 7:
                                    continue
                                psm = k.ps()
                                k.op("pe", "matmul", out=psm[:, 0:64], lhsT=t64["a"][:, gq, :], rhs=u.v, start=True, stop=False)
                                k.op("pe", "matmul", out=psm[:, 0:64], lhsT=t64["k"][:, gq, :], rhs=t64["v"][0:64, gq, P0:P0 + 64],
                                     start=False, stop=True)
                                tm = tmr.next()
                                k.op("dve", "tensor_tensor", out=tm[sl, :], in0=psm[sl, 0:64], in1=Mc[sl, P0:P0 + 64], op=ALU.add)
                                k.op("dve", "tensor_scalar", out=Mn[sl, P0:P0 + 64], in0=tm[sl, :], scalar1=pC[sl, ci:ci + 1],
                                     scalar2=None, op0=ALU.mult)
                                cur[hh] = 1 - cur[hh]
                    if not is_s:
                        for hh in range(2):
                            P0 = hh * 64
                            sl = slice(P0, P0 + 64)
                            ps = k.ps()
                            k.op("pe", "matmul", out=ps[0:64, 0:64], lhsT=Mst[hh][cur[hh]][sl, P0:P0 + 64], rhs=C["ident"][sl, sl],
                                 start=True, stop=True)
                            so = sor.next()
                            k.op("act", "copy", out=so.v, in_=ps[0:64, 0:64])
                            k.dma(o_rw[sj, l, d, hp * 2 + hh], so.v)
                for n0 in range(0, T, NB):
                    cs = slice(n0, n0 + NB)
                    pg = k.ps()
                    k.op("pe", "matmul", out=pg[:, 0:NB], lhsT=lora[64:128, 3, hp * 128:(hp + 1) * 128], rhs=x6[64:128, cs],
                         start=True, stop=True)
                    for hh in range(2):
                        sl = slice(hh * 64, hh * 64 + 64)
                        cen, sq, rs, bo = (t5.next() for _ in range(4))
                        pm = k.ps()
                        k.op("pe", "matmul", out=pm[:, 0:NB], lhsT=C["ones"][sl, :], rhs=ysum[sl, cs], start=True, stop=True)
                        k.op("dve", "scalar_tensor_tensor", out=cen[sl, :], in0=pm[sl, 0:NB], scalar=-1.0 / 64, in1=ysum[sl, cs],
                             op0=ALU.mult, op1=ALU.add)
                        k.op("act", "activation", out=sq[sl, :], in_=cen[sl, :], func=AF.Square)
                        pv = k.ps()
                        k.op("pe", "matmul", out=pv[:, 0:NB], lhsT=C["ones"][sl, :], rhs=sq[sl, :], start=True, stop=True)
                        k.op("dve", "tensor_scalar", out=rs[sl, :], in0=pv[sl, 0:NB], scalar1=1.0 / 64, scalar2=64e-5,
                             op0=ALU.mult, op1=ALU.add)
                        k.op("act", "activation", out=rs[sl, :], in_=rs[sl, :], func=AF.Ln)
                        k.op("act", "activation", out=rs[sl, :], in_=rs[sl, :], func=AF.Exp, scale=-0.5)
                        k.op("dve", "tensor_tensor", out=cen[sl, :], in0=cen[sl, :], in1=rs[sl, :], op=ALU.mult)
                        k.op("dve", "tensor_scalar", out=cen[sl, :], in0=cen[sl, :], scalar1=lnT[sl, hp:hp + 1],
                             scalar2=lnT[sl, 2 + hp:3 + hp], op0=ALU.mult, op1=ALU.add)
                        k.op("dve", "scalar_tensor_tensor", out=bo[sl, :], in0=r_[sl, cs], scalar=kvT[sl, 4 + hp:5 + hp],
                             in1=kmod[sl, cs], op0=ALU.mult, op1=ALU.mult)
                        pb = k.ps()
                        k.op("pe", "matmul", out=pb[:, 0:NB], lhsT=C["ones"][sl, :], rhs=bo[sl, :], start=True, stop=True)
                        k.op("dve", "tensor_tensor", out=bo[sl, :], in0=pb[sl, 0:NB], in1=v_[sl, cs], op=ALU.mult)
                        k.op("dve", "tensor_tensor", out=cen[sl, :], in0=cen[sl, :], in1=bo[sl, :], op=ALU.add)
                        k.op("dve", "tensor_tensor", out=zt[sl, cs], in0=cen[sl, :], in1=pg[sl, 0:NB], op=ALU.mult)
                k.dma(yb_d[hp * 128:(hp + 1) * 128, s0:s0 + T], zt[:, 0:T])


def stage_merge(k, E, C, g, l, x, mod_d, zT_d, yb_d):
    NT, TG = g["NT"], g["TG"]
    TB = 256
    with k.scope():
        g1b = bc_param(k, "g1b", mod_d[l, g["cond"]:g["cond"] + 1, 2 * D:3 * D])
        brw = k.sb("brw", [128, 8, D])
        k.dma(brw, E["br_w"][l].re("b (kc p) n -> p (b kc) n", p=128))
        wo = k.sb("wo", [128, 8, D])
        k.dma(wo, E["w_out"][l].re("(kc p) n -> p kc n", p=128))
        ybr = Rot([k.sb("ybb", [128, 8, TB]) for _ in range(2)])
        mTr = Rot([k.sb("mT", [128, 8, TB]) for _ in range(2)])
        gtr = Rot([k.sb("gt", [128, TB]) for _ in range(3)])
        tmr = Rot([k.sb("tm", [128, TB]) for _ in range(2)])
        tor = Rot([k.sb("to", [128, 512]) for _ in range(2)])
        for t0 in range(0, TG, TB):
            yb = ybr.next()
            k.dma(yb, yb_d[:, t0:t0 + TB].re("(c p) t -> p c t", p=128))
            mT = mTr.next()
            for nch in range(8):
                for b in range(4):
                    gt = gtr.next()
                    r0 = O_MG + b * D + nch * 128
                    k.dma(gt, zT_d[r0:r0 + 128, t0:t0 + TB])
                    ps = k.ps()
                    for kc2 in range(2):
                        k.op("pe", "matmul", out=ps[:, 0:TB], lhsT=brw[:, 2 * b + kc2, nch * 128:(nch + 1) * 128],
                             rhs=yb[:, 2 * b + kc2, :], start=(kc2 == 0), stop=(kc2 == 1))
                    if b == 0:
                        k.op("dve", "tensor_tensor", out=mT[:, nch, :], in0=ps[:, 0:TB], in1=gt.v, op=ALU.mult)
                    else:
                        tm = tmr.next()
                        k.op("dve", "tensor_tensor", out=tm.v, in0=ps[:, 0:TB], in1=gt.v, op=ALU.mult)
                        k.op("pool", "tensor_tensor", out=mT[:, nch, :], in0=mT[:, nch, :], in1=tm.v, op=ALU.add)
            for ts in range(TB // 128):
                i = (t0 // 128) + ts
                for eh in range(2):
                    ps = k.ps()
                    for nch in range(8):
                        k.op("pe", "matmul", out=ps.v, lhsT=mT[:, nch, ts * 128:(ts + 1) * 128],
                             rhs=wo[:, nch, eh * 512:(eh + 1) * 512], start=(nch == 0), stop=(nch == 7))
                    to = tor.next()
                    k.op("dve", "tensor_tensor", out=to.v, in0=ps.v, in1=g1b[:, eh * 512:(eh + 1) * 512], op=ALU.mult)
                    k.op("pool", "tensor_tensor", out=x[:, i, eh * 512:(eh + 1) * 512],
                         in0=x[:, i, eh * 512:(eh + 1) * 512], in1=to.v, op=ALU.add)


def stage_moe(k, E, C, g, l, x, mod_d):
    NT, TG = g["NT"], g["TG"]
    seqs = g["seqs"]
    caps = [2 * T // NE for (_, T) in seqs]
    NS = sum(caps)
    slot0 = [sum(caps[:j]) for j in range(len(seqs))]
    with k.scope():
        h2bf = k.sb("h2bf", [128, NT, D], BF16)
        aff = k.sb("aff", [128, NT, NE])
        rsel = k.sb("rsel", [128, NT, NE])
        g2b = bc_param(k, "g2b", mod_d[l, g["cond"]:g["cond"] + 1, 5 * D:6 * D])
        with k.scope():
            A, B = mod_params(k, E, mod_d, l, g["cond"], 1, "norm2_g")
            nm = NormMod(k)
            rt = k.sb("rt", [128, 8, NE])
            k.dma(rt, E["router"][l].re("(kc p) e -> p kc e", p=128))
            htm = Rot([k.sb("htm", [128, D]) for _ in range(2)])
            hTt = Rot([k.sb("hTt", [128, 8, 128]) for _ in range(2)])
            sm = Rot([k.sb("sm", [128, 4]) for _ in range(2)])
            affT = k.sb("affT", [NE, TG])
            for i in range(NT):
                h = htm.next()
                nm(x[:, i, :], A, B, h.v)
                k.op("pool", "tensor_copy", out=h2bf[:, i, :], in_=h.v, free=True)
                hT = hTt.next()
                to_fm(k, C, h, hT, 0, free=False)
                ps = k.ps()
                for kc in range(8):
                    k.op("pe", "matmul", out=ps[:, 0:NE], lhsT=hT[:, kc, :], rhs=rt[:, kc, :],
                         start=(kc == 0), stop=(kc == 7))
                s = sm.next()
                k.op("dve", "reduce_max", out=s[:, 0:1], in_=ps[:, 0:NE], axis=AX.X)
                k.op("dve", "tensor_scalar", out=s[:, 1:2], in0=s[:, 0:1], scalar1=-1.0, scalar2=None, op0=ALU.mult)
                k.op("act", "activation", out=aff[:, i, :], in_=ps[:, 0:NE], func=AF.Exp, bias=s[:, 1:2], scale=1.0,
                     accum_out=s[:, 2:3])
                k.op("dve", "reciprocal", out=s[:, 3:4], in_=s[:, 2:3])
                k.op("dve", "tensor_scalar", out=aff[:, i, :], in0=aff[:, i, :], scalar1=s[:, 3:4], scalar2=None,
                     op0=ALU.mult)
                ps2 = k.ps()
                k.op("pe", "transpose", out=ps2[0:NE, 0:128], in_=aff[:, i, :], identity=C["ident"].v)
                k.op("act", "copy", out=affT[:, i * 128:(i + 1) * 128], in_=ps2[0:NE, 0:128])
            work = k.sb("work", [NE, TG])
            mx = Rot([k.sb("mx", [NE, 8]) for _ in range(2)])
            k.op("dve", "tensor_copy", out=work.v, in_=affT.v)
            for j, (s0, T) in enumerate(seqs):
                for r in range(caps[j] // 8):
                    m8 = mx.next()
                    k.op("dve", "max", out=m8.v, in_=work[:, s0:s0 + T])
                    k.op("dve", "match_replace", out=work[:, s0:s0 + T], in_to_replace=m8.v,
                         in_values=work[:, s0:s0 + T], imm_value=-1.0)
            maskT = k.sb("maskT", [NE, TG])
            k.op("dve", "tensor_scalar", out=maskT.v, in0=work.v, scalar1=-1.0, scalar2=None, op0=ALU.is_equal)
            mtm = k.sb("mtm", [128, NT, NE])
            for i in range(NT):
                ps = k.ps()
                k.op("pe", "transpose", out=ps[:, 0:NE], in_=maskT[:, i * 128:(i + 1) * 128],
                     identity=C["ident"][0:NE, 0:NE])
                k.op("act", "copy", out=mtm[:, i, :], in_=ps[:, 0:NE])
            for j, (s0, T) in enumerate(seqs):
                i0, n = s0 // 128, T // 128
                for ii in range(n):
                    ps = k.ps()
                    for jj in range(ii + 1):
                        k.op("pe", "matmul", out=ps[:, 0:NE], lhsT=(C["triu"] if jj == ii else C["ones"]).v,
                             rhs=mtm[:, i0 + jj, :], start=(jj == 0), stop=(jj == ii))
                    k.op("dve", "scalar_tensor_tensor", out=rsel[:, i0 + ii, :], in0=ps[:, 0:NE], scalar=float(slot0[j]),
                         in1=mtm[:, i0 + ii, :], op0=ALU.add, op1=ALU.mult)
                    k.op("dve", "tensor_scalar", out=rsel[:, i0 + ii, :], in0=rsel[:, i0 + ii, :], scalar1=-1.0,
                         scalar2=None, op0=ALU.add)
        if False:
            with k.scope():
                dt_ = k.sb("dbgt", [128, 2048])
                k.op("dve", "tensor_copy", out=dt_[:, 0:1024], in_=h2bf[:, 0, :])
                k.op("dve", "tensor_copy", out=dt_[:, 1024:2048], in_=h2bf[:, 1, :])
                k.dma(E["dbg"][:, 0:2048], dt_.v)
                k.dma(E["dbg"][:, 2048:2048 + NT * NE], aff.v.re("p n e -> p (n e)"))
                k.dma(E["dbg"][:, 4096:4096 + NT * NE], rsel.v.re("p n e -> p (n e)"))
        with k.scope():
            w1r = Rot([k.sb("w1", [128, 8, 256]) for _ in range(2)])
            w3r = Rot([k.sb("w3", [128, 8, 256]) for _ in range(2)])
            w2r = Rot([k.sb("w2", [128, D]) for _ in range(4)])
            xeT = k.sb("xeT", [128, 8, NS])
            xe = k.sb("xe", [128, (NS + 127) // 128, D])
            hidT = k.sb("hidT", [128, FF // 128, NS])
            NCC = (NS + 127) // 128
            ye = k.sb("ye", [128, NCC, D])
            selr = Rot([k.sb("sel", [128, 256], BF16) for _ in range(3)])
            sgr = Rot([k.sb("sg", [128, 256]) for _ in range(2)])
            sgTr = Rot([k.sb("sgT", [128, 2, 128]) for _ in range(2)])
            s1r = Rot([k.sb("s1", [128, 256]) for _ in range(2)])
            for e in range(NE):
                pss = [[k.ps() for _ in range(2)] for _ in range(NCC)]
                for i in range(NT):
                    sel = selr.next()
                    k.op("dve", "tensor_scalar", out=sel[:, 0:NS], in0=C["iota_f"][:, 0:NS],
                         scalar1=rsel[:, i, e:e + 1], scalar2=None, op0=ALU.is_equal)
                    for cc in range(NCC):
                        cw = min(128, NS - cc * 128)
                        for dh in range(2):
                            k.op("pe", "matmul", out=pss[cc][dh][0:cw, :], lhsT=sel[:, cc * 128:cc * 128 + cw],
                                 rhs=h2bf[:, i, dh * 512:(dh + 1) * 512], start=(i == 0), stop=(i == NT - 1))
                for cc in range(NCC):
                    cw = min(128, NS - cc * 128)
                    for dh in range(2):
                        if dh:
                            k.op("act", "copy", out=xe[0:cw, cc, dh * 512:(dh + 1) * 512], in_=pss[cc][dh][0:cw, :])
                        else:
                            k.op("dve", "tensor_copy", out=xe[0:cw, cc, dh * 512:(dh + 1) * 512], in_=pss[cc][dh][0:cw, :])
                for cc in range(NCC):
                    cw = min(128, NS - cc * 128)
                    for j2 in range(2):
                        pt = k.ps()
                        for a in range(4):
                            kc = 4 * j2 + a
                            k.op("pe", "transpose", out=pt[:, a * 128:a * 128 + cw], in_=xe[0:cw, cc, kc * 128:(kc + 1) * 128],
                                 identity=C["ident"][0:cw, 0:cw])
                        src = pt.v.re("p (a t) -> p a t", a=4)[:, :, 0:cw]
                        dst = xeT[:, 4 * j2:4 * j2 + 4, cc * 128:cc * 128 + cw]
                        if j2:
                            k.op("act", "copy", out=dst, in_=src)
                        else:
                            k.op("dve", "tensor_copy", out=dst, in_=src)
                for fc2 in range(FF // 256):
                    w1, w3 = w1r.next(), w3r.next()
                    k.dma(w1, E["ex_w1"][l, e][:, fc2 * 256:(fc2 + 1) * 256].re("(kc p) n -> p kc n", p=128))
                    k.dma(w3, E["ex_w3"][l, e][:, fc2 * 256:(fc2 + 1) * 256].re("(kc p) n -> p kc n", p=128), q="pool")
                    for sub in range(2):
                        fc = fc2 * 2 + sub
                        fs = slice(sub * 128, (sub + 1) * 128)
                        p1, p3 = k.ps(), k.ps()
                        for kc in range(8):
                            k.op("pe", "matmul", out=p1[:, 0:NS], lhsT=w1[:, kc, fs], rhs=xeT[:, kc, :],
                                 start=(kc == 0), stop=(kc == 7))
                        for kc in range(8):
                            k.op("pe", "matmul", out=p3[:, 0:NS], lhsT=w3[:, kc, fs], rhs=xeT[:, kc, :],
                                 start=(kc == 0), stop=(kc == 7))
                        s1 = s1r.next()
                        k.op("act", "activation", out=s1[:, 0:NS], in_=p1[:, 0:NS], func=AF.Sigmoid)
                        k.op("dve", "tensor_tensor", out=s1[:, 0:NS], in0=s1[:, 0:NS], in1=p1[:, 0:NS], op=ALU.mult)
                        k.op("dve", "tensor_tensor", out=hidT[:, fc, :], in0=p3[:, 0:NS], in1=s1[:, 0:NS], op=ALU.mult)
                pacc = [[k.ps() for _ in range(2)] for _ in range(NCC)]
                for fc in range(FF // 128):
                    w2 = w2r.next()
                    k.dma(w2, E["ex_w2"][l, e][fc * 128:(fc + 1) * 128, :], q=("pool" if fc % 2 else "sp"))
                    for cc in range(NCC):
                        cw = min(128, NS - cc * 128)
                        for dh in range(2):
                            k.op("pe", "matmul", out=pacc[cc][dh][0:cw, :], lhsT=hidT[:, fc, cc * 128:cc * 128 + cw],
                                 rhs=w2[:, dh * 512:(dh + 1) * 512], start=(fc == 0), stop=(fc == FF // 128 - 1))
                for cc in range(NCC):
                    cw = min(128, NS - cc * 128)
                    for dh in range(2):
                        k.op("dve", "tensor_tensor", out=ye[0:cw, cc, dh * 512:(dh + 1) * 512],
                             in0=pacc[cc][dh][0:cw, :], in1=g2b[0:cw, dh * 512:(dh + 1) * 512], op=ALU.mult)
                for i in range(NT):
                    sg = sgr.next()
                    k.op("dve", "tensor_scalar", out=sg[:, 0:NS], in0=C["iota_f"][:, 0:NS],
                         scalar1=rsel[:, i, e:e + 1], scalar2=aff[:, i, e:e + 1], op0=ALU.is_equal, op1=ALU.mult)
                    sgT = sgTr.next()
                    pst = k.ps()
                    for cc in range(NCC):
                        k.op("pe", "transpose", out=pst[:, cc * 128:(cc + 1) * 128],
                             in_=sg[:, cc * 128:(cc + 1) * 128], identity=C["ident"].v)
                    k.op("act", "copy", out=sgT[:, 0:NCC, :], in_=pst[:, 0:NCC * 128].re("p (c t) -> p c t", c=NCC))
                    for dh in range(2):
                        pso = k.ps()
                        for cc in range(NCC):
                            k.op("pe", "matmul", out=pso.v, lhsT=sgT[:, cc, :],
                                 rhs=ye[:, cc, dh * 512:(dh + 1) * 512],
                                 start=(cc == 0), stop=(cc == NCC - 1))
                        k.op("dve", "tensor_tensor", out=x[:, i, dh * 512:(dh + 1) * 512],
                             in0=x[:, i, dh * 512:(dh + 1) * 512], in1=pso.v, op=ALU.add)


def final_norm(k, x, NT, fg_b, yv):
    nm = NormMod(k)
    op_ = Rot([k.sb("o", [128, D]) for _ in range(2)])
    for i in range(NT):
        o = op_.next()
        nm(x[:, i, :], fg_b, None, o.v)
        k.dma(yv[:, i, :], o.v)


_PROG = None
W_NAMES = ("ada_w", "ada_b", "norm1_g", "norm2_g", "w_in", "br_w", "w_out", "router", "ex_w1", "ex_w3", "ex_w2")


def kernel(**inp):
    global _PROG
    if _PROG is None:
        _PROG = build_program()
    nc = _PROG
    cn = consts_np()
    f32 = lambda a: np.ascontiguousarray(np.asarray(a, dtype=np.float32))
    shared = {n: f32(inp[n][:NLAYERS_RUN]) for n in W_NAMES if (ENABLE["moe"] or not n.startswith("ex_"))}
    shared["final_g"] = f32(inp["final_g"]).reshape(1, D)
    shared["c_ctx"] = f32(inp["c_ctx"]).reshape(1, D)
    shared["ret_rate"] = f32(inp["ret_rate"]).reshape(L, 8)
    shared["ret_gn"] = f32(inp["ret_gn"])
    shared["hg_lb"] = f32(inp["hg_lb"]); shared["hg_norm"] = f32(inp["hg_norm"])
    for nm in ("rw_mu", "rw_w0", "rw_w_up", "rw_a0", "rw_a_up", "rw_g_up", "rw_kvec", "rw_ln", "hy_conv", "hy_ffn1", "hy_ffn1_b", "hy_ffn2", "hy_ffn2_b", "hy_ffn3", "hy_freq", "hy_decay", "hy_skip"):
        shared[nm] = f32(inp[nm])
    shared.update(cn)
    in_maps = []
    for i in range(NC_RUN):
        m = dict(shared)
        m["xs"] = f32(inp["x_sample"][i])
        m["xp"] = f32(inp["x_prompt"][NPR * i:NPR * (i + 1)]).reshape(NPR * TP, D)
        m["c_s"] = f32(inp["c"][i]).reshape(1, D)
        m["st_ret"] = f32(inp["state_ret"][i])
        m["st_hg"] = f32(inp["state_hgrn"][i])
        m["st_rw"] = f32(inp["state_rwkv"][i])
        in_maps.append(m)
    res = run_bass_kernel_spmd(nc, in_maps, core_ids=list(range(NC_RUN)))
    R = res.results
    y_prompt = np.concatenate([R[i]["yp"].reshape(NPR, TP, D) for i in range(NC_RUN)], axis=0)
    y_sample = np.stack([R[i]["ys"] for i in range(NC_RUN)], axis=0)
    sts = []
    for nm in ("o_rw", "o_ret", "o_hg"):
        sts.append(np.concatenate([R[i][nm].reshape(NPR, L, 2, 4, 64, 64) for i in range(NC_RUN)], axis=0))
    return (y_prompt, y_sample, sts[0], sts[1], sts[2])
```
